# Optimizing a Trainium2 kernel written in Bass

```python
import jax
import jax.numpy as jnp
from jax import lax
import numpy as np

D_MODEL = 1024
BATCH = 8
SEQ = 4096
DEPTH = 1

HG_HEADS = 4
HG_DK = 128
HG_DV = 128
HG_KW = HG_HEADS * HG_DK
HG_VW = HG_HEADS * HG_DV
HG_CHUNK = 64
CV_WIDTH = 512
CONV_TAPS = 31
N_BRANCH = 2
N_EXPERTS = 32
TOP_K = 4
D_FF = D_MODEL
SWIGLU_LIMIT = 7.0
SWIGLU_ALPHA = 1.702
MOE_BLOCK = 512
N_MOD = 6
EPS = 1e-6
IN_SPLITS = (HG_KW, HG_KW, HG_KW, HG_VW, HG_VW, CV_WIDTH, CV_WIDTH, N_BRANCH * D_MODEL)
IN_COLS = 3 * HG_KW + 2 * HG_VW + 2 * CV_WIDTH + N_BRANCH * D_MODEL

kernel_name = "hybrid_hgrn2_conformer_moe_adaln_encoder"


def _rms_norm(x, g):
    x32 = x.astype(jnp.float32)
    return x32 * lax.rsqrt(jnp.mean(x32 * x32, axis=-1, keepdims=True) + EPS) * g.astype(jnp.float32)


def _layer_norm(x, g, b):
    x32 = x.astype(jnp.float32)
    xc = x32 - jnp.mean(x32, axis=-1, keepdims=True)
    var = jnp.mean(xc * xc, axis=-1, keepdims=True)
    return xc * lax.rsqrt(var + EPS) * g.astype(jnp.float32) + b.astype(jnp.float32)


def _gla_chunk_scan(q, k, v, logf):
    bsz, seq, nh, kd = q.shape
    vd = v.shape[-1]
    n_chunks = seq // HG_CHUNK

    def chunks(t):
        return t.reshape(bsz, n_chunks, HG_CHUNK, nh, t.shape[-1]).transpose(1, 0, 3, 2, 4)

    mask = jnp.tril(jnp.ones((HG_CHUNK, HG_CHUNK), dtype=bool))[:, :, None]

    def step(state, inp):
        qc, kc, vc, gc = inp
        b = jnp.cumsum(gc, axis=2)
        o_inter = jnp.einsum('bhtk,bhkv->bhtv', qc * jnp.exp(b), state)
        diff = b[:, :, :, None, :] - b[:, :, None, :, :]
        decay = jnp.exp(jnp.where(mask, diff, -jnp.inf))
        scores = jnp.einsum('bhtk,bhtsk,bhsk->bhts', qc, decay, kc)
        o_intra = jnp.einsum('bhts,bhsv->bhtv', scores, vc)
        b_last = b[:, :, -1, :]
        k_dec = kc * jnp.exp(b_last[:, :, None, :] - b)
        new_state = jnp.exp(b_last)[..., None] * state + jnp.einsum('bhsk,bhsv->bhkv', k_dec, vc)
        return new_state, o_inter + o_intra

    s0 = jnp.zeros((bsz, nh, kd, vd), jnp.float32)
    _, o = lax.scan(step, s0, (chunks(q), chunks(k), chunks(v), chunks(logf)))
    return o.transpose(1, 0, 3, 2, 4).reshape(bsz, seq, nh, vd)


def _hgrn2_branch(q, z_fwd, z_bwd, i_val, o_gate, lb, norm_g):
    bsz, seq, _ = q.shape
    lb = lb.astype(jnp.float32)
    f_fwd = lb[0] + (1.0 - lb[0]) * jax.nn.sigmoid(z_fwd)
    f_bwd = lb[1] + (1.0 - lb[1]) * jax.nn.sigmoid(z_bwd)

    def hk(t):
        return t.reshape(bsz, seq, HG_HEADS, HG_DK)

    def flip(t):
        return jnp.flip(t, axis=1)

    qh = hk(q)
    vh = i_val.reshape(bsz, seq, HG_HEADS, HG_DV)
    q_all = jnp.concatenate([qh, flip(qh)], axis=2)
    k_all = jnp.concatenate([hk(1.0 - f_fwd), flip(hk(1.0 - f_bwd))], axis=2)
    g_all = jnp.concatenate([hk(jnp.log(f_fwd)), flip(hk(jnp.log(f_bwd)))], axis=2)
    v_all = jnp.concatenate([vh, flip(vh)], axis=2)
    o = _gla_chunk_scan(q_all, k_all, v_all, g_all)
    o = o[:, :, :HG_HEADS] + flip(o[:, :, HG_HEADS:])
    o = _rms_norm(o, norm_g)
    return o.reshape(bsz, seq, HG_VW) * jax.nn.silu(o_gate)


def _conformer_conv_branch(val, gate, dw_w, dw_b, ln_g, ln_b):
    u = val * jax.nn.sigmoid(gate)
    u = lax.conv_general_dilated(
        u, dw_w.astype(jnp.float32)[:, None, :], window_strides=(1,),
        padding=[(CONV_TAPS // 2, CONV_TAPS // 2)],
        dimension_numbers=('NWC', 'WIO', 'NWC'), feature_group_count=CV_WIDTH)
    u = u + dw_b.astype(jnp.float32)
    return jax.nn.silu(_layer_norm(u, ln_g, ln_b))


def _moe_ffn(h, w_r, b_r, w_gu, b_gu, w_dn, b_dn):
    bsz, seq, d = h.shape
    n_tok = bsz * seq
    n_assign = n_tok * TOP_K
    hf = h.reshape(n_tok, d)
    logits = hf @ w_r.astype(jnp.float32) + b_r.astype(jnp.float32)
    top_v, top_i = lax.top_k(logits, TOP_K)
    gate_w = jax.nn.softmax(top_v, axis=-1)
    e_flat = top_i.reshape(-1)
    w_flat = gate_w.reshape(-1)
    tok_flat = jnp.arange(n_assign, dtype=jnp.int32) // TOP_K
    order = jnp.argsort(e_flat)
    e_s, tok_s, w_s = e_flat[order], tok_flat[order], w_flat[order]
    counts = jnp.bincount(e_flat, length=N_EXPERTS)
    padded = (counts + MOE_BLOCK - 1) // MOE_BLOCK * MOE_BLOCK
    start_sorted = jnp.cumsum(counts) - counts
    end_padded = jnp.cumsum(padded)
    start_padded = end_padded - padded
    rank = jnp.arange(n_assign, dtype=jnp.int32) - start_sorted[e_s]
    dest = start_padded[e_s] + rank
    n_blocks = -(-n_assign // MOE_BLOCK) + N_EXPERTS
    n_slots = n_blocks * MOE_BLOCK
    slot_tok = jnp.full((n_slots,), n_tok, jnp.int32).at[dest].set(tok_s)
    slot_w = jnp.zeros((n_slots,), jnp.float32).at[dest].set(w_s)
    block_e = jnp.minimum(
        jnp.searchsorted(end_padded, jnp.arange(n_blocks, dtype=jnp.int32) * MOE_BLOCK, side='right'),
        N_EXPERTS - 1)
    h_pad = jnp.concatenate([hf, jnp.zeros((1, d), jnp.float32)], axis=0)

    def step(acc, blk):
        tok, wt, e = blk
        xb = h_pad[tok]
        gu = xb @ w_gu[e].astype(jnp.float32) + b_gu[e].astype(jnp.float32)
        g, u = gu[:, :D_FF], gu[:, D_FF:]
        g = jnp.minimum(g, SWIGLU_LIMIT)
        u = jnp.clip(u, -SWIGLU_LIMIT, SWIGLU_LIMIT)
        act = (u + 1.0) * (g * jax.nn.sigmoid(SWIGLU_ALPHA * g))
        y = act @ w_dn[e].astype(jnp.float32) + b_dn[e].astype(jnp.float32)
        return acc.at[tok].add(y * wt[:, None]), None

    acc0 = jnp.zeros((n_tok + 1, d), jnp.float32)
    acc, _ = lax.scan(step, acc0, (slot_tok.reshape(n_blocks, MOE_BLOCK),
                                   slot_w.reshape(n_blocks, MOE_BLOCK), block_e))
    return acc[:n_tok].reshape(bsz, seq, d)


def setup_inputs(seed: int = 0) -> dict:
    key = jax.random.key(seed)
    ks = jax.random.split(key, 24)
    f32 = jnp.float32

    def nrm(k, shape, scale):
        return jax.random.normal(k, shape, f32) * scale

    return {
        "x": nrm(ks[0], (BATCH, SEQ, D_MODEL), 1.0),
        "c": nrm(ks[1], (BATCH, D_MODEL), 1.0),
        "ada_w": nrm(ks[2], (DEPTH, D_MODEL, N_MOD * D_MODEL), 0.5 * D_MODEL ** -0.5),
        "ada_b": nrm(ks[3], (DEPTH, N_MOD * D_MODEL), 0.02),
        "norm1_g": 1.0 + nrm(ks[4], (DEPTH, D_MODEL), 0.02),
        "w_in": nrm(ks[5], (DEPTH, D_MODEL, IN_COLS), D_MODEL ** -0.5),
        "lb_table": nrm(ks[6], (DEPTH + 1, 2, HG_KW), 0.5),
        "hg_norm_g": 1.0 + nrm(ks[7], (DEPTH, HG_HEADS, HG_DV), 0.02),
        "w_o_hg": nrm(ks[8], (DEPTH, HG_VW, D_MODEL), HG_VW ** -0.5),
        "dw_w": nrm(ks[9], (DEPTH, CONV_TAPS, CV_WIDTH), CONV_TAPS ** -0.5),
        "dw_b": nrm(ks[10], (DEPTH, CV_WIDTH), 0.02),
        "cv_ln_g": 1.0 + nrm(ks[11], (DEPTH, CV_WIDTH), 0.02),
        "cv_ln_b": nrm(ks[12], (DEPTH, CV_WIDTH), 0.02),
        "w_o_cv": nrm(ks[13], (DEPTH, CV_WIDTH, D_MODEL), CV_WIDTH ** -0.5),
        "b_o_cv": nrm(ks[14], (DEPTH, D_MODEL), 0.02),
        "w_out": nrm(ks[15], (DEPTH, D_MODEL, D_MODEL), D_MODEL ** -0.5),
        "norm2_g": 1.0 + nrm(ks[16], (DEPTH, D_MODEL), 0.02),
        "router_w": nrm(ks[17], (DEPTH, D_MODEL, N_EXPERTS), D_MODEL ** -0.5),
        "router_b": nrm(ks[18], (DEPTH, N_EXPERTS), 0.01),
        "w_gate_up": nrm(ks[19], (DEPTH, N_EXPERTS, D_MODEL, 2 * D_FF), D_MODEL ** -0.5),
        "b_gate_up": nrm(ks[20], (DEPTH, N_EXPERTS, 2 * D_FF), 0.02),
        "w_down": nrm(ks[21], (DEPTH, N_EXPERTS, D_FF, D_MODEL), D_FF ** -0.5),
        "b_down": nrm(ks[22], (DEPTH, N_EXPERTS, D_MODEL), 0.02),
        "final_norm_g": 1.0 + nrm(ks[23], (D_MODEL,), 0.02),
    }


def reference(x, c, ada_w, ada_b, norm1_g, w_in, lb_table, hg_norm_g, w_o_hg, dw_w, dw_b,
              cv_ln_g, cv_ln_b, w_o_cv, b_o_cv, w_out, norm2_g, router_w, router_b,
              w_gate_up, b_gate_up, w_down, b_down, final_norm_g):
    out_dtype = x.dtype
    h_res = x.astype(jnp.float32)
    c_act = jax.nn.silu(c.astype(jnp.float32))
    lb_all = jnp.cumsum(jax.nn.softmax(lb_table.astype(jnp.float32), axis=0), axis=0)
    split_at = np.cumsum(IN_SPLITS)[:-1].tolist()
    for l in range(DEPTH):
        mod = c_act @ ada_w[l].astype(jnp.float32) + ada_b[l].astype(jnp.float32)
        sh1, sc1, g1, sh2, sc2, g2 = jnp.split(mod[:, None, :], N_MOD, axis=-1)

        h = _rms_norm(h_res, norm1_g[l]) * (1.0 + sc1) + sh1
        proj = h @ w_in[l].astype(jnp.float32)
        q, z_f, z_b, i_val, o_gate, cv_val, cv_gate, merge_g = jnp.split(proj, split_at, axis=-1)
        y_hg = _hgrn2_branch(q, z_f, z_b, i_val, o_gate, lb_all[l], hg_norm_g[l]) @ w_o_hg[l].astype(jnp.float32)
        y_cv = (_conformer_conv_branch(cv_val, cv_gate, dw_w[l], dw_b[l], cv_ln_g[l], cv_ln_b[l])
                @ w_o_cv[l].astype(jnp.float32) + b_o_cv[l].astype(jnp.float32))
        gate_hg, gate_cv = jnp.split(merge_g, N_BRANCH, axis=-1)
        merged = jax.nn.sigmoid(gate_hg) * y_hg + jax.nn.sigmoid(gate_cv) * y_cv
        h_res = h_res + g1 * (merged @ w_out[l].astype(jnp.float32))

        h2 = _rms_norm(h_res, norm2_g[l]) * (1.0 + sc2) + sh2
        h_res = h_res + g2 * _moe_ffn(h2, router_w[l], router_b[l], w_gate_up[l], b_gate_up[l],
                                      w_down[l], b_down[l])
    return _rms_norm(h_res, final_norm_g).astype(out_dtype)
```

```python
import os
from contextlib import ExitStack

import numpy as np
import concourse.bass as bass
import concourse.mybir as mybir
from concourse.bass_utils import run_bass_kernel_spmd

F32 = mybir.dt.float32
BF16 = mybir.dt.bfloat16
I32 = mybir.dt.int32
AF = mybir.ActivationFunctionType
ALU = mybir.AluOpType
AX = mybir.AxisListType

S = 4096
D = 1024
NT = 32
NCH = 8
E = 32
NBLK = 64
NSLOT = NBLK * 512
EPS = 1e-6
IN_COLS = 5632

ENGS = ("pe", "act", "dve", "pool", "sp")


class Res:
    __slots__ = ("last_w", "readers")

    def __init__(self):
        self.last_w = None
        self.readers = {}


class Prog:
    def __init__(self, nc, n_dma_sems=48):
        self.nc = nc
        self.ops = {e: [] for e in ENGS}
        self.seq = {e: 0 for e in ENGS}
        self.waited = {e: {} for e in ENGS}
        self.sems = {}
        self.n_dma = n_dma_sems
        self.dma_uses = [0] * n_dma_sems
        self.dma_rr = 0
        self.same_engine_sync = True

    def alloc(self, stack):
        for e in ENGS:
            self.sems["c_" + e] = stack.enter_context(self.nc.semaphore("c_" + e))
        for i in range(self.n_dma):
            self.sems["d%d" % i] = stack.enter_context(self.nc.semaphore("d%d" % i))

    def op(self, eng, fn, reads=(), writes=(), dma=False):
        deps = {}

        def add(s, v):
            if deps.get(s, 0) < v:
                deps[s] = v

        for r in reads:
            if r.last_w is not None:
                add(*r.last_w)
        for w in writes:
            if w.last_w is not None:
                add(*w.last_w)
            for s, v in w.readers.items():
                add(s, v)
        if dma:
            i = self.dma_rr
            self.dma_rr = (self.dma_rr + 1) % self.n_dma
            s = "d%d" % i
            if self.dma_uses[i] > 0:
                add(s, 16 * self.dma_uses[i])
            self.dma_uses[i] += 1
            ev = (s, 16 * self.dma_uses[i])
        else:
            self.seq[eng] += 1
            ev = ("c_" + eng, self.seq[eng])
        waits = []
        wd = self.waited[eng]
        for s, v in deps.items():
            if s == "c_" + eng and (eng == "pe" or not self.same_engine_sync):
                continue
            if wd.get(s, 0) >= v:
                continue
            wd[s] = v
            waits.append((s, v))
        self.ops[eng].append((waits, fn, ev, dma))
        for r in reads:
            if r.readers.get(ev[0], 0) < ev[1]:
                r.readers[ev[0]] = ev[1]
        for w in writes:
            w.last_w = ev
            w.readers = {}
        return ev

    def barrier(self):
        allev = []
        for e in ENGS:
            if self.seq[e] > 0:
                allev.append(("c_" + e, self.seq[e]))
        for i in range(self.n_dma):
            if self.dma_uses[i] > 0:
                allev.append(("d%d" % i, 16 * self.dma_uses[i]))
        for e in ENGS:
            waits = []
            wd = self.waited[e]
            for s, v in allev:
                if s == "c_" + e:
                    continue
                if wd.get(s, 0) >= v:
                    continue
                wd[s] = v
                waits.append((s, v))
            if waits:
                self.ops[e].append((waits, None, None, False))

    def replay(self, eng, e):
        for waits, fn, ev, dma in self.ops[eng]:
            for s, v in waits:
                e.wait_ge(self.sems[s], v)
            if fn is None:
                continue
            inst = fn(e)
            inst.then_inc(self.sems[ev[0]], 16 if dma else 1)

    def run(self):
        nc = self.nc
        with nc.Block() as block:
            @block.tensor
            def _(e):
                self.replay("pe", e)

            @block.scalar
            def _(e):
                self.replay("act", e)

            @block.vector
            def _(e):
                self.replay("dve", e)

            @block.gpsimd
            def _(e):
                self.replay("pool", e)

            @block.sync
            def _(e):
                self.replay("sp", e)
        self.ops = {e: [] for e in ENGS}

    def flush(self):
        self.barrier()
        self.run()


def bc(ap, shape):
    return ap.broadcast_to(list(shape))


def build_nc(debug=False):
    nc = bass.Bass("TRN2", target_bir_lowering=False)

    def din(name, shape, dt=F32):
        return nc.dram_tensor(name, list(shape), dt, kind="ExternalInput").ap()

    def dscr(name, shape, dt=F32, out=False):
        return nc.dram_tensor(name, list(shape), dt, kind="ExternalOutput" if out else "Internal").ap()

    x = din("x", [S, D])
    c_in = din("c", [D])
    ada_w = din("ada_w", [D, 6 * D])
    ada_b = din("ada_b", [6 * D])
    norm1_g = din("norm1_g", [D])
    w_in = din("w_in", [D, IN_COLS])
    lb_table = din("lb_table", [2, 2, 512])
    hg_norm_g = din("hg_norm_g", [4, 128])
    w_o_hg = din("w_o_hg", [512, D])
    dw_w = din("dw_w", [31, 512])
    dw_b = din("dw_b", [512])
    cv_ln_g = din("cv_ln_g", [512])
    cv_ln_b = din("cv_ln_b", [512])
    w_o_cv = din("w_o_cv", [512, D])
    b_o_cv = din("b_o_cv", [D])
    w_out = din("w_out", [D, D])
    norm2_g = din("norm2_g", [D])
    router_w = din("router_w", [D, E])
    router_b = din("router_b", [E])
    w_gu = din("w_gate_up", [E * D, 2 * D])
    b_gu = din("b_gate_up", [E, 2 * D])
    w_dn = din("w_down", [E * D, D])
    b_dn = din("b_down", [E, D])
    fin_g = din("final_norm_g", [D])
    out = nc.dram_tensor("out", [S, D], F32, kind="ExternalOutput").ap()

    mod_d = dscr("mod_d", [6, D])
    projT_d = dscr("projT_d", [IN_COLS, S])
    vtok_d = dscr("vtok_d", [S, 512], BF16)
    hres_d = dscr("hres_d", [S, D], F32, out=debug)
    h2_d = dscr("h2_d", [S + 128, D], BF16)
    slot_d = dscr("slot_d", [NSLOT, 1], I32)
    ys_d = dscr("ys_d", [NSLOT, D], F32)
    dbg_d = dscr("dbg_d", [128, 32 * 40], F32, out=True) if debug else None

    with ExitStack() as top:
        P = Prog(nc)
        P.alloc(top)
        top.enter_context(nc.allow_non_contiguous_dma(reason="small parameter / index layouts"))

        def sb(st, name, shape, dt=F32):
            return st.enter_context(nc.sbuf_tensor(name, list(shape), dt))

        def ps(st, name, shape, dt=F32):
            return st.enter_context(nc.psum_tensor(name, list(shape), dt))

        ident_f = sb(top, "ident_f", [128, 128]); r_ident = Res()
        ident_b = sb(top, "ident_b", [128, 128], BF16)
        ones_f = sb(top, "ones_f", [128, 128]); r_ones = Res()
        lstrict = sb(top, "lstrict", [128, 128])
        epsb = sb(top, "epsb", [128, 1])
        iota_p = sb(top, "iota_p", [128, 1])
        g2b = sb(top, "g2b", [128, D]); r_g2b = Res()
        fgb = sb(top, "fgb", [128, D]); r_fgb = Res()
        dest4i = sb(top, "dest4i", [128, NT, 4], I32); r_dest4 = Res()
        w4 = sb(top, "w4", [128, NT, 4]); r_w4 = Res()
        slot_sb = sb(top, "slot_sb", [128, NBLK * 4], I32); r_slot_sb = Res()
        widx = sb(top, "widx", [128, NBLK, 8], I32); r_widx = Res()
        ohb = sb(top, "ohb", [32, NBLK]); r_ohb = Res()
        r_const = Res()

        P.op("pool", lambda e: e.memset(ident_f[:], 1.0), writes=[r_ident])
        P.op("pool", lambda e: e.affine_select(out=ident_f[:], in_=ident_f[:], pattern=[[-1, 128]],
                                               compare_op=ALU.is_equal, fill=0.0, base=0, channel_multiplier=1),
             reads=[r_ident], writes=[r_ident])
        P.op("dve", lambda e: e.tensor_copy(out=ident_b[:], in_=ident_f[:]), reads=[r_ident], writes=[r_const])
        P.op("pool", lambda e: e.memset(ones_f[:], 1.0), writes=[r_ones])
        P.op("pool", lambda e: e.memset(lstrict[:], 1.0), writes=[r_const])
        P.op("pool", lambda e: e.affine_select(out=lstrict[:], in_=lstrict[:], pattern=[[1, 128]],
                                               compare_op=ALU.is_ge, fill=0.0, base=-1, channel_multiplier=-1),
             reads=[r_const], writes=[r_const])
        P.op("pool", lambda e: e.memset(epsb[:], EPS), writes=[r_const])
        P.op("pool", lambda e: e.iota(iota_p[:], pattern=[[0, 1]], base=0, channel_multiplier=1,
                                      allow_small_or_imprecise_dtypes=True), writes=[r_const])

        r_mod = Res()
        r_projT = {}
        r_vtok = [Res() for _ in range(NT)]
        r_hres = [Res() for _ in range(NT)]
        r_h2d = [Res() for _ in range(NT + 1)]
        r_slotd = Res()
        r_ysd = Res()

        with ExitStack() as st:
            c_sb = sb(st, "c_sb", [128, 8]); r_c = Res()
            adw = [sb(st, "adw%d" % i, [128, 8, 512]) for i in range(2)]; r_adw = [Res(), Res()]
            modrow = sb(st, "modrow", [1, 6 * D]); r_modrow = Res()
            adb = sb(st, "adb", [1, 6 * D]); r_adb = Res()
            pmod = [ps(st, "pmod%d" % i, [128, 512]) for i in range(2)]; r_pmod = [Res(), Res()]
            P.op("sp", lambda e: e.dma_start(out=c_sb[:], in_=c_in.rearrange("(c p) -> p c", p=128)),
                 writes=[r_c], dma=True)
            P.op("sp", lambda e: e.dma_start(out=adb[:], in_=ada_b.rearrange("(o n) -> o n", o=1)),
                 writes=[r_adb], dma=True)
            P.op("act", lambda e: e.activation(out=c_sb[:], in_=c_sb[:], func=AF.Silu), reads=[r_c], writes=[r_c])
            adw_v = ada_w.rearrange("(c p) n -> p c n", p=128)
            for n in range(12):
                bi = n % 2
                P.op("sp", lambda e, n=n, bi=bi: e.dma_start(out=adw[bi][:], in_=adw_v[:, :, n * 512:(n + 1) * 512]),
                     writes=[r_adw[bi]], dma=True)
                for kc in range(8):
                    P.op("pe", lambda e, bi=bi, kc=kc: e.matmul(pmod[bi][0:1, :], lhsT=c_sb[:, kc:kc + 1],
                                                                rhs=adw[bi][:, kc, :], start=(kc == 0), stop=(kc == 7)),
                         reads=[r_c, r_adw[bi]], writes=[r_pmod[bi]])
                P.op("dve", lambda e, n=n, bi=bi: e.tensor_tensor(out=modrow[0:1, n * 512:(n + 1) * 512],
                                                                   in0=pmod[bi][0:1, :],
                                                                   in1=adb[0:1, n * 512:(n + 1) * 512], op=ALU.add),
                     reads=[r_pmod[bi], r_adb], writes=[r_modrow])
            P.op("sp", lambda e: e.dma_start(out=mod_d.rearrange("(o k) d -> o (k d)", o=1), in_=modrow[:]),
                 reads=[r_modrow], writes=[r_mod], dma=True)
            P.flush()

        stR = ExitStack()
        mask_all = sb(stR, "mask_all", [128, NT, E]); r_maskall = [Res() for _ in range(NT)]
        gw_all = sb(stR, "gw_all", [128, NT, E])
        rank_all = sb(stR, "rank_all", [128, NT, E]); r_rankall = [Res() for _ in range(NT)]
        cummask = sb(stR, "cummask", [128, E]); r_cum = Res()
        cnt_b = sb(stR, "cnt_b", [128, E]); r_cnt = Res()
        with ExitStack() as stA:

            with ExitStack() as st:
                hT = sb(st, "hT", [128, 8, S], BF16)
                r_hT = [Res() for _ in range(NT)]
                a1 = sb(st, "a1", [128, 8]); sh1 = sb(st, "sh1", [128, 8]); n1f = sb(st, "n1f", [128, 8])
                r_a1 = Res()
                A1 = sb(st, "A1", [128, 8, 128]); SH1 = sb(st, "SH1", [128, 8, 128]); r_A1 = Res()
                P.op("sp", lambda e: e.dma_start(out=sh1[:], in_=mod_d[0].rearrange("(c p) -> p c", p=128)),
                     reads=[r_mod], writes=[r_a1], dma=True)
                P.op("sp", lambda e: e.dma_start(out=a1[:], in_=mod_d[1].rearrange("(c p) -> p c", p=128)),
                     reads=[r_mod], writes=[r_a1], dma=True)
                P.op("sp", lambda e: e.dma_start(out=n1f[:], in_=norm1_g.rearrange("(c p) -> p c", p=128)),
                     writes=[r_a1], dma=True)
                P.op("dve", lambda e: e.scalar_tensor_tensor(out=a1[:], in0=a1[:], scalar=1.0, in1=n1f[:],
                                                             op0=ALU.add, op1=ALU.mult), reads=[r_a1], writes=[r_a1])
                P.op("dve", lambda e: e.tensor_copy(out=A1[:], in_=bc(a1[:, :].unsqueeze(2), [128, 8, 128])),
                     reads=[r_a1], writes=[r_A1])
                P.op("dve", lambda e: e.tensor_copy(out=SH1[:], in_=bc(sh1[:, :].unsqueeze(2), [128, 8, 128])),
                     reads=[r_a1], writes=[r_A1])
                NXB = 3
                xt = [sb(st, "xt%d" % i, [128, D]) for i in range(NXB)]; r_xt = [Res() for _ in range(NXB)]
                sq = sb(st, "sq", [128, D], BF16); r_sq = Res()
                ss = sb(st, "ss", [128, NT]); r_ss = [Res() for _ in range(NT)]
                rs = sb(st, "rs", [128, NT])
                xn = [sb(st, "xn%d" % i, [128, D], BF16) for i in range(2)]; r_xn = [Res(), Res()]
                ptr = [ps(st, "ptr%d" % i, [128, 8, 128], BF16) for i in range(2)]; r_ptr = [Res(), Res()]
                tmp = [sb(st, "tmp%d" % i, [128, 8, 128]) for i in range(2)]; r_tmp = [Res(), Res()]
                for i in range(NT):
                    xb = i % NXB
                    b2 = i % 2
                    P.op("sp", lambda e, i=i, xb=xb: e.dma_start(out=xt[xb][:], in_=x[i * 128:(i + 1) * 128, :]),
                         writes=[r_xt[xb]], dma=True)
                    P.op("act", lambda e, i=i, xb=xb: e.activation(out=sq[:], in_=xt[xb][:], func=AF.Square,
                                                                    accum_out=ss[:, i:i + 1]),
                         reads=[r_xt[xb]], writes=[r_sq, r_ss[i]])
                    P.op("act", lambda e, i=i: e.activation(out=rs[:, i:i + 1], in_=ss[:, i:i + 1], func=AF.Sqrt,
                                                            scale=1.0 / D, bias=epsb[:]),
                         reads=[r_ss[i], r_const], writes=[r_ss[i]])
                    P.op("dve", lambda e, i=i: e.reciprocal(out=rs[:, i:i + 1], in_=rs[:, i:i + 1]),
                         reads=[r_ss[i]], writes=[r_ss[i]])
                    P.op("dve", lambda e, i=i, xb=xb, b2=b2: e.tensor_scalar_mul(out=xn[b2][:], in0=xt[xb][:],
                                                                                  scalar1=rs[:, i:i + 1]),
                         reads=[r_xt[xb], r_ss[i]], writes=[r_xn[b2]])
                    for kc in range(8):
                        P.op("pe", lambda e, kc=kc, b2=b2: e.transpose(out=ptr[b2][:, kc, :],
                                                                        in_=xn[b2][:, kc * 128:(kc + 1) * 128],
                                                                        identity=ident_b[:]),
                             reads=[r_xn[b2], r_const], writes=[r_ptr[b2]])
                    P.op("dve", lambda e, b2=b2: e.tensor_tensor(out=tmp[b2][:], in0=ptr[b2][:], in1=A1[:], op=ALU.mult),
                         reads=[r_ptr[b2], r_A1], writes=[r_tmp[b2]])
                    P.op("pool", lambda e, i=i, b2=b2: e.tensor_tensor(out=hT[:, :, i * 128:(i + 1) * 128],
                                                                        in0=tmp[b2][:], in1=SH1[:], op=ALU.add),
                         reads=[r_tmp[b2], r_A1], writes=[r_hT[i]])

                wv = w_in.rearrange("(c p) n -> p c n", p=128)
                wt = [sb(st, "wt%d" % i, [128, 8, 512], BF16) for i in range(2)]; r_wt = [Res(), Res()]
                stg = [sb(st, "stg%d" % i, [128, S]) for i in range(2)]; r_stg = [Res(), Res()]
                vst = [sb(st, "vst%d" % i, [128, 512], BF16) for i in range(2)]; r_vst = [Res(), Res()]
                pp = [ps(st, "pp%d" % i, [128, 512]) for i in range(4)]; r_pp = [Res() for _ in range(4)]
                ppi = 0
                sgi = 0
                for g in range(11):
                    bi = g % 2
                    P.op("pool", lambda e, g=g, bi=bi: e.dma_start(out=wt[bi][:], in_=wv[:, :, g * 512:(g + 1) * 512]),
                         writes=[r_wt[bi]], dma=True)
                    if g == 3:
                        for i in range(NT):
                            pi = ppi % 4; ppi += 1
                            for kc in range(8):
                                P.op("pe", lambda e, i=i, kc=kc, pi=pi, bi=bi: e.matmul(
                                    pp[pi][:], lhsT=hT[:, kc, i * 128:(i + 1) * 128], rhs=wt[bi][:, kc, :],
                                    start=(kc == 0), stop=(kc == 7)),
                                    reads=[r_hT[i], r_wt[bi]], writes=[r_pp[pi]])
                            vb = i % 2
                            P.op("act", lambda e, pi=pi, vb=vb: e.copy(out=vst[vb][:], in_=pp[pi][:]),
                                 reads=[r_pp[pi]], writes=[r_vst[vb]])
                            P.op("sp", lambda e, i=i, vb=vb: e.dma_start(out=vtok_d[i * 128:(i + 1) * 128, :],
                                                                         in_=vst[vb][:]),
                                 reads=[r_vst[vb]], writes=[r_vtok[i]], dma=True)
                        continue
                    for mm in range(4):
                        sg = sgi % 2; sgi += 1
                        for tc in range(NCH):
                            pi = ppi % 4; ppi += 1
                            for kc in range(8):
                                P.op("pe", lambda e, mm=mm, tc=tc, kc=kc, pi=pi, bi=bi: e.matmul(
                                    pp[pi][:], lhsT=wt[bi][:, kc, mm * 128:(mm + 1) * 128],
                                    rhs=hT[:, kc, tc * 512:(tc + 1) * 512], start=(kc == 0), stop=(kc == 7)),
                                    reads=[r_wt[bi]] + r_hT[tc * 4:(tc + 1) * 4], writes=[r_pp[pi]])
                            if tc % 2 == 0:
                                P.op("act", lambda e, tc=tc, pi=pi, sg=sg: e.copy(
                                    out=stg[sg][:, tc * 512:(tc + 1) * 512], in_=pp[pi][:]),
                                    reads=[r_pp[pi]], writes=[r_stg[sg]])
                            else:
                                P.op("dve", lambda e, tc=tc, pi=pi, sg=sg: e.tensor_copy(
                                    out=stg[sg][:, tc * 512:(tc + 1) * 512], in_=pp[pi][:]),
                                    reads=[r_pp[pi]], writes=[r_stg[sg]])
                        row0 = g * 512 + mm * 128
                        r_projT[row0] = Res()
                        P.op("sp", lambda e, row0=row0, sg=sg: e.dma_start(out=projT_d[row0:row0 + 128, :],
                                                                           in_=stg[sg][:]),
                             reads=[r_stg[sg]], writes=[r_projT[row0]], dma=True)
                P.flush()

            hgT = sb(stA, "hgT", [128, 4, S], BF16)
            r_hgT = [Res() for _ in range(4)]
            with ExitStack() as st:
                lbt = sb(st, "lbt", [128, 2, 2, 4]); r_lb = Res()
                lbv = sb(st, "lbv", [128, 2, 4]); oml = sb(st, "oml", [128, 2, 4])
                ngf = sb(st, "ngf", [128, 4])
                P.op("sp", lambda e: e.dma_start(out=lbt[:, 0], in_=lb_table[0].rearrange("d (h p) -> p d h", p=128)),
                     writes=[r_lb], dma=True)
                P.op("sp", lambda e: e.dma_start(out=lbt[:, 1], in_=lb_table[1].rearrange("d (h p) -> p d h", p=128)),
                     writes=[r_lb], dma=True)
                P.op("sp", lambda e: e.dma_start(out=ngf[:], in_=hg_norm_g.rearrange("h p -> p h")),
                     writes=[r_lb], dma=True)
                P.op("dve", lambda e: e.tensor_tensor(out=lbv[:], in0=lbt[:, 0], in1=lbt[:, 1], op=ALU.subtract),
                     reads=[r_lb], writes=[r_lb])
                P.op("act", lambda e: e.activation(out=lbv[:], in_=lbv[:], func=AF.Sigmoid), reads=[r_lb], writes=[r_lb])
                P.op("dve", lambda e: e.tensor_scalar(out=oml[:], in0=lbv[:], scalar1=-1.0, scalar2=1.0,
                                                       op0=ALU.mult, op1=ALU.add), reads=[r_lb], writes=[r_lb])
                H = 2048
                ones_h = sb(st, "ones_h", [128, H]); r_onesh = Res()
                P.op("pool", lambda e: e.memset(ones_h[:], 1.0), writes=[r_onesh])
                mask_f = sb(st, "mask_f", [128, 128]); mask_b = sb(st, "mask_b", [128, 128]); r_mask = Res()
                for mk, sgn in ((mask_f, 1), (mask_b, -1)):
                    P.op("pool", lambda e, mk=mk: e.memset(mk[:], 1.0), writes=[r_mask])
                    P.op("pool", lambda e, mk=mk, sgn=sgn: e.affine_select(
                        out=mk[:], in_=mk[:], pattern=[[sgn, 128]], compare_op=ALU.is_ge, fill=0.0, base=0,
                        channel_multiplier=-sgn), reads=[r_mask], writes=[r_mask])
                    P.op("pool", lambda e, mk=mk: e.memset(mk[0:64, 64:128], 0.0), reads=[r_mask], writes=[r_mask])
                    P.op("pool", lambda e, mk=mk: e.memset(mk[64:128, 0:64], 0.0), reads=[r_mask], writes=[r_mask])

                T1 = sb(st, "T1", [128, H]); T2 = sb(st, "T2", [128, H]); T3 = sb(st, "T3", [128, H])
                TQ = sb(st, "TQ", [128, H])
                r_T1, r_T2, r_T3, r_TQ = Res(), Res(), Res(), Res()
                Bext = sb(st, "Bext", [128, S + 1]); r_B = Res()
                qt = [sb(st, "qt%d" % d, [128, S], BF16) for d in range(2)]
                kt = [sb(st, "kt%d" % d, [128, S], BF16) for d in range(2)]
                ktok = [sb(st, "ktok%d" % d, [128, NT, 128], BF16) for d in range(2)]
                dec = [sb(st, "dec%d" % d, [128, 64]) for d in range(2)]
                r_qk = [Res(), Res()]
                r_ktok = [Res(), Res()]
                vtok = sb(st, "vtok", [128, NT, 128], BF16); r_vt = Res()
                o_h = sb(st, "o_h", [128, S]); r_oh = [Res() for _ in range(NT)]
                Sf = [sb(st, "Sf%d" % d, [128, 128]) for d in range(2)]
                Sb = [sb(st, "Sb%d" % d, [128, 128], BF16) for d in range(2)]
                Stmp = [sb(st, "Stmp%d" % d, [128, 128]) for d in range(2)]
                r_S = [Res(), Res()]
                r_Sf = [Res(), Res()]
                r_Stmp = [Res(), Res()]
                sT = [sb(st, "sT%d" % d, [128, 128], BF16) for d in range(2)]; r_sT = [Res(), Res()]
                p_sc = [ps(st, "p_sc%d" % d, [128, 512])[:, 0:128] for d in range(2)]; r_psc = [Res(), Res()]
                p_o = [ps(st, "p_o%d" % d, [128, 512])[:, 0:128] for d in range(2)]; r_po = [Res(), Res()]
                p_P = [ps(st, "p_P%d" % d, [128, 512])[:, 0:128] for d in range(2)]; r_pP = [Res(), Res()]
                p_kt = ps(st, "p_kt", [128, 8, 128], BF16); r_pkt = Res()
                p_st = ps(st, "p_st", [128, 512]); r_pst = Res()

                for h in range(4):
                    P.op("sp", lambda e, h=h: e.dma_start(
                        out=vtok[:], in_=vtok_d[:, h * 128:(h + 1) * 128].rearrange("(i p) v -> p i v", p=128)),
                        reads=r_vtok, writes=[r_vt], dma=True)
                    P.op("pool", lambda e: e.memset(o_h[:], 0.0), writes=r_oh)
                    for d in range(2):
                        zrow = 512 + d * 512 + h * 128
                        B3 = Bext[:, 1:S + 1].rearrange("p (c j) -> p c j", j=64)
                        B0 = Bext[:, 0:S].rearrange("p (c j) -> p c j", j=64)
                        P.op("pool", lambda e: e.memset(Bext[:, 0:1], 0.0), writes=[r_B])
                        for hf in range(2):
                            c0 = hf * H
                            P.op("sp", lambda e, zrow=zrow, c0=c0: e.dma_start(
                                out=T1[:], in_=projT_d[zrow:zrow + 128, c0:c0 + H]),
                                reads=[r_projT[zrow]], writes=[r_T1], dma=True)
                            P.op("act", lambda e: e.activation(out=T1[:], in_=T1[:], func=AF.Sigmoid),
                                 reads=[r_T1], writes=[r_T1])
                            P.op("dve", lambda e, d=d, h=h: e.tensor_scalar(
                                out=T1[:], in0=T1[:], scalar1=oml[:, d, h:h + 1], scalar2=lbv[:, d, h:h + 1],
                                op0=ALU.mult, op1=ALU.add), reads=[r_T1, r_lb], writes=[r_T1])
                            P.op("act", lambda e: e.activation(out=T2[:], in_=T1[:], func=AF.Ln),
                                 reads=[r_T1], writes=[r_T2])
                            P.op("dve", lambda e, c0=c0: e.tensor_tensor_scan(
                                out=Bext[:, 1 + c0:1 + c0 + H], data0=ones_h[:], data1=T2[:],
                                initial=Bext[:, c0:c0 + 1], op0=ALU.mult, op1=ALU.add),
                                reads=[r_T2, r_onesh, r_B], writes=[r_B])
                            P.op("pool", lambda e: e.tensor_scalar(out=T1[:], in0=T1[:], scalar1=-1.0, scalar2=1.0,
                                                                   op0=ALU.mult, op1=ALU.add),
                                 reads=[r_T1], writes=[r_T1])
                            P.op("act", lambda e, d=d, c0=c0: e.copy(out=kt[d][:, c0:c0 + H], in_=T1[:]),
                                 reads=[r_T1], writes=[r_qk[d]])
                        for hf in range(2):
                            c0 = hf * H
                            cs = slice(hf * 32, (hf + 1) * 32)
                            T2v = T2[:].rearrange("p (c j) -> p c j", j=64)
                            if d == 0:
                                P.op("dve", lambda e, cs=cs: e.tensor_tensor(
                                    out=T2v, in0=B3[:, cs, :], in1=bc(B0[:, cs, 0:1], [128, 32, 64]),
                                    op=ALU.subtract), reads=[r_B], writes=[r_T2])
                            else:
                                P.op("dve", lambda e, cs=cs: e.tensor_tensor(
                                    out=T2v, in0=bc(B3[:, cs, 63:64], [128, 32, 64]), in1=B0[:, cs, :],
                                    op=ALU.subtract), reads=[r_B], writes=[r_T2])
                            P.op("act", lambda e: e.activation(out=T3[:], in_=T2[:], func=AF.Exp),
                                 reads=[r_T2], writes=[r_T3])
                            T3v = T3[:].rearrange("p (c j) -> p c j", j=64)
                            jj = 63 if d == 0 else 0
                            P.op("pool", lambda e, d=d, cs=cs, jj=jj: e.tensor_copy(
                                out=dec[d][:, cs].unsqueeze(2), in_=T3v[:, :, jj:jj + 1]),
                                reads=[r_T3], writes=[r_qk[d]])
                            qrow = h * 128
                            P.op("sp", lambda e, qrow=qrow, c0=c0: e.dma_start(
                                out=TQ[:], in_=projT_d[qrow:qrow + 128, c0:c0 + H]),
                                reads=[r_projT[qrow]], writes=[r_TQ], dma=True)
                            P.op("dve", lambda e, d=d, c0=c0: e.tensor_tensor(
                                out=qt[d][:, c0:c0 + H], in0=TQ[:], in1=T3[:], op=ALU.mult),
                                reads=[r_TQ, r_T3], writes=[r_qk[d]])
                            P.op("act", lambda e: e.activation(out=T3[:], in_=T2[:], func=AF.Exp, scale=-1.0),
                                 reads=[r_T2], writes=[r_T3])
                            P.op("pool", lambda e, d=d, c0=c0: e.tensor_tensor(
                                out=kt[d][:, c0:c0 + H], in0=kt[d][:, c0:c0 + H], in1=T3[:], op=ALU.mult),
                                reads=[r_T3, r_qk[d]], writes=[r_qk[d]])
                        for g8 in range(4):
                            for j in range(8):
                                i = g8 * 8 + j
                                P.op("pe", lambda e, d=d, i=i, j=j: e.transpose(
                                    out=p_kt[:, j, :], in_=kt[d][:, i * 128:(i + 1) * 128], identity=ident_b[:]),
                                    reads=[r_qk[d], r_const], writes=[r_pkt])
                            P.op("act", lambda e, d=d, g8=g8: e.copy(out=ktok[d][:, g8 * 8:(g8 + 1) * 8, :],
                                                                     in_=p_kt[:]),
                                 reads=[r_pkt], writes=[r_ktok[d]])
                        P.op("pool", lambda e, d=d: e.memset(Sf[d][:], 0.0), writes=[r_Sf[d]])
                        P.op("pool", lambda e, d=d: e.memset(Sb[d][:], 0.0), writes=[r_S[d]])

                    for step in range(NT):
                        for d in range(2):
                            i = step if d == 0 else NT - 1 - step
                            t0 = i * 128
                            mk = mask_f if d == 0 else mask_b
                            P.op("pe", lambda e, d=d, t0=t0: e.matmul(
                                p_sc[d][:], lhsT=kt[d][:, t0:t0 + 128], rhs=qt[d][:, t0:t0 + 128],
                                start=True, stop=True), reads=[r_qk[d]], writes=[r_psc[d]])
                            P.op("dve", lambda e, d=d, mk=mk: e.tensor_tensor(
                                out=sT[d][:], in0=p_sc[d][:], in1=mk[:], op=ALU.mult),
                                reads=[r_psc[d], r_mask], writes=[r_sT[d]])
                            P.op("pe", lambda e, d=d, i=i: e.matmul(
                                p_o[d][:], lhsT=vtok[:, i, :], rhs=sT[d][:], start=True, stop=False),
                                reads=[r_vt, r_sT[d]], writes=[r_po[d]])
                            order = (0, 1) if d == 0 else (1, 0)
                            for n_, half in enumerate(order):
                                c = i * 2 + half
                                hs = slice(half * 64, half * 64 + 64)
                                P.op("pe", lambda e, d=d, t0=t0, hs=hs, n_=n_: e.matmul(
                                    p_o[d][:, hs], lhsT=Sb[d][:], rhs=qt[d][:, t0 + hs.start:t0 + hs.stop],
                                    start=False, stop=(n_ == 1)),
                                    reads=[r_S[d], r_qk[d]], writes=[r_po[d]])
                                P.op("pe", lambda e, d=d, i=i, hs=hs: e.matmul(
                                    p_P[d][:], lhsT=ktok[d][hs, i, :], rhs=vtok[hs, i, :], start=True, stop=True),
                                    reads=[r_ktok[d], r_vt], writes=[r_pP[d]])
                                P.op("dve", lambda e, d=d: e.tensor_tensor(
                                    out=Stmp[d][:], in0=p_P[d][:], in1=Sf[d][:], op=ALU.add),
                                    reads=[r_pP[d], r_Sf[d]], writes=[r_Stmp[d]])
                                P.op("act", lambda e, d=d, c=c: e.activation(
                                    out=Sb[d][:], in_=Stmp[d][:], func=AF.Copy, scale=dec[d][:, c:c + 1]),
                                    reads=[r_Stmp[d], r_qk[d]], writes=[r_S[d]])
                                P.op("pool", lambda e, d=d, c=c: e.tensor_scalar_mul(
                                    out=Sf[d][:], in0=Stmp[d][:], scalar1=dec[d][:, c:c + 1]),
                                    reads=[r_Stmp[d], r_qk[d]], writes=[r_Sf[d]])
                            P.op("dve", lambda e, d=d, t0=t0: e.tensor_tensor(
                                out=o_h[:, t0:t0 + 128], in0=p_o[d][:], in1=o_h[:, t0:t0 + 128], op=ALU.add),
                                reads=[r_po[d], r_oh[i]], writes=[r_oh[i]])

                    for tc in range(NCH):
                        cs = slice(tc * 512, (tc + 1) * 512)
                        w0 = (tc % 4) * 512
                        P.op("act", lambda e, cs=cs, w0=w0: e.activation(out=T1[:, w0:w0 + 512], in_=o_h[:, cs],
                                                                         func=AF.Square),
                             reads=r_oh[tc * 4:(tc + 1) * 4], writes=[r_T1])
                        P.op("pe", lambda e, w0=w0: e.matmul(p_st[:], lhsT=ones_f[:], rhs=T1[:, w0:w0 + 512],
                                                            start=True, stop=True),
                             reads=[r_T1, r_ones], writes=[r_pst])
                        P.op("act", lambda e, w0=w0: e.activation(out=T2[:, w0:w0 + 512], in_=p_st[:], func=AF.Sqrt,
                                                                  scale=1.0 / 128, bias=epsb[:]),
                             reads=[r_pst, r_const], writes=[r_T2])
                        P.op("dve", lambda e, w0=w0: e.reciprocal(out=T2[:, w0:w0 + 512], in_=T2[:, w0:w0 + 512]),
                             reads=[r_T2], writes=[r_T2])
                        P.op("dve", lambda e, cs=cs, w0=w0: e.tensor_tensor(
                            out=T2[:, w0:w0 + 512], in0=T2[:, w0:w0 + 512], in1=o_h[:, cs], op=ALU.mult),
                            reads=[r_T2] + r_oh[tc * 4:(tc + 1) * 4], writes=[r_T2])
                        grow = 2048 + h * 128
                        P.op("sp", lambda e, grow=grow, cs=cs, w0=w0: e.dma_start(
                            out=T3[:, w0:w0 + 512], in_=projT_d[grow:grow + 128, cs]),
                            reads=[r_projT[grow]], writes=[r_T3], dma=True)
                        P.op("act", lambda e, w0=w0: e.activation(out=T3[:, w0:w0 + 512], in_=T3[:, w0:w0 + 512],
                                                                  func=AF.Silu), reads=[r_T3], writes=[r_T3])
                        P.op("dve", lambda e, h=h, cs=cs, w0=w0: e.scalar_tensor_tensor(
                            out=hgT[:, h, cs], in0=T2[:, w0:w0 + 512], scalar=ngf[:, h:h + 1],
                            in1=T3[:, w0:w0 + 512], op0=ALU.mult, op1=ALU.mult),
                            reads=[r_T2, r_T3, r_lb], writes=[r_hgT[h]])
                P.flush()

            cvT = sb(stA, "cvT", [128, 4, S], BF16)
            r_cvT = [Res() for _ in range(4)]
            with ExitStack() as st:
                dww = sb(st, "dww", [128, 4, 31]); dwb = sb(st, "dwb", [128, 4])
                lng = sb(st, "lng", [128, 4]); lnb = sb(st, "lnb", [128, 4]); r_cp = Res()
                for cc in range(4):
                    P.op("sp", lambda e, cc=cc: e.dma_start(out=dww[:, cc, :],
                                                            in_=dw_w[:, cc * 128:(cc + 1) * 128].rearrange("j p -> p j")),
                         writes=[r_cp], dma=True)
                for t_, src in ((dwb, dw_b), (lng, cv_ln_g), (lnb, cv_ln_b)):
                    P.op("sp", lambda e, t_=t_, src=src: e.dma_start(out=t_[:], in_=src.rearrange("(c p) -> p c", p=128)),
                         writes=[r_cp], dma=True)
                uc = sb(st, "uc", [128, 4, S], BF16); r_uc = [Res() for _ in range(4)]
                upad = [sb(st, "upad%d" % i, [128, S + 30]) for i in range(2)]
                acc = [sb(st, "acc%d" % i, [128, S]) for i in range(2)]
                r_val = [Res(), Res()]; r_up = [Res(), Res()]; r_acc = [Res(), Res()]
                for cc in range(4):
                    b2 = cc % 2
                    eng = "dve"
                    vrow = 2560 + cc * 128
                    grow = 3072 + cc * 128
                    P.op("sp", lambda e, vrow=vrow, b2=b2: e.dma_start(out=upad[b2][:, 15:S + 15], in_=projT_d[vrow:vrow + 128, :]),
                         reads=[r_projT[vrow]], writes=[r_up[b2]], dma=True)
                    P.op("sp", lambda e, grow=grow, b2=b2: e.dma_start(out=acc[b2][:], in_=projT_d[grow:grow + 128, :]),
                         reads=[r_projT[grow]], writes=[r_acc[b2]], dma=True)
                    P.op("act", lambda e, b2=b2: e.activation(out=acc[b2][:], in_=acc[b2][:], func=AF.Sigmoid),
                         reads=[r_acc[b2]], writes=[r_acc[b2]])
                    P.op(eng, lambda e, b2=b2: e.memset(upad[b2][:, 0:15], 0.0), writes=[r_up[b2]])
                    P.op(eng, lambda e, b2=b2: e.memset(upad[b2][:, S + 15:S + 30], 0.0), writes=[r_up[b2]])
                    P.op(eng, lambda e, b2=b2: e.tensor_tensor(out=upad[b2][:, 15:S + 15], in0=upad[b2][:, 15:S + 15],
                                                               in1=acc[b2][:], op=ALU.mult),
                         reads=[r_up[b2], r_acc[b2]], writes=[r_up[b2]])
                    P.op(eng, lambda e, b2=b2, cc=cc: e.tensor_scalar(
                        out=acc[b2][:], in0=upad[b2][:, 0:S], scalar1=dww[:, cc, 0:1], scalar2=dwb[:, cc:cc + 1],
                        op0=ALU.mult, op1=ALU.add), reads=[r_up[b2], r_cp], writes=[r_acc[b2]])
                    for j in range(1, 31):
                        dst = acc[b2] if j < 30 else None
                        if j < 30:
                            P.op(eng, lambda e, b2=b2, cc=cc, j=j: e.scalar_tensor_tensor(
                                out=acc[b2][:], in0=upad[b2][:, j:j + S], scalar=dww[:, cc, j:j + 1], in1=acc[b2][:],
                                op0=ALU.mult, op1=ALU.add), reads=[r_up[b2], r_acc[b2], r_cp], writes=[r_acc[b2]])
                        else:
                            P.op(eng, lambda e, b2=b2, cc=cc, j=j: e.scalar_tensor_tensor(
                                out=uc[:, cc, :], in0=upad[b2][:, j:j + S], scalar=dww[:, cc, j:j + 1], in1=acc[b2][:],
                                op0=ALU.mult, op1=ALU.add), reads=[r_up[b2], r_acc[b2], r_cp], writes=[r_uc[cc]])
                ones_b = sb(st, "ones_b", [128, 128], BF16); r_ob = Res()
                P.op("dve", lambda e: e.tensor_copy(out=ones_b[:], in_=ones_f[:]), reads=[r_ones], writes=[r_ob])
                usq = sb(st, "usq", [128, 4, 512], BF16); r_usq = Res()
                p_s1 = ps(st, "p_s1", [128, 512]); p_s2 = ps(st, "p_s2", [128, 512]); r_ps1 = Res(); r_ps2 = Res()
                mean = sb(st, "mean", [128, 512]); msq = sb(st, "msq", [128, 512]); rstd = sb(st, "rstd", [128, 512])
                r_mean = Res(); r_rstd = Res()
                tt = [sb(st, "tt%d" % i, [128, 512]) for i in range(2)]; r_tt = [Res(), Res()]
                for tc in range(NCH):
                    cs = slice(tc * 512, (tc + 1) * 512)
                    for cc in range(4):
                        P.op("act", lambda e, cc=cc, cs=cs: e.activation(out=usq[:, cc, :], in_=uc[:, cc, cs],
                                                                         func=AF.Square),
                             reads=[r_uc[cc]], writes=[r_usq])
                    for cc in range(4):
                        P.op("pe", lambda e, cc=cc, cs=cs: e.matmul(p_s1[:], lhsT=ones_b[:], rhs=uc[:, cc, cs],
                                                                    start=(cc == 0), stop=(cc == 3)),
                             reads=[r_uc[cc], r_ob], writes=[r_ps1])
                    for cc in range(4):
                        P.op("pe", lambda e, cc=cc: e.matmul(p_s2[:], lhsT=ones_b[:], rhs=usq[:, cc, :],
                                                             start=(cc == 0), stop=(cc == 3)),
                             reads=[r_usq, r_ob], writes=[r_ps2])
                    P.op("act", lambda e: e.activation(out=mean[:], in_=p_s1[:], func=AF.Copy, scale=1.0 / 512),
                         reads=[r_ps1], writes=[r_mean])
                    P.op("dve", lambda e: e.tensor_tensor(out=msq[:], in0=mean[:], in1=mean[:], op=ALU.mult),
                         reads=[r_mean], writes=[r_rstd])
                    P.op("dve", lambda e: e.scalar_tensor_tensor(out=rstd[:], in0=p_s2[:], scalar=1.0 / 512, in1=msq[:],
                                                                 op0=ALU.mult, op1=ALU.subtract),
                         reads=[r_ps2, r_rstd], writes=[r_rstd])
                    P.op("dve", lambda e: e.tensor_scalar_max(out=rstd[:], in0=rstd[:], scalar1=0.0),
                         reads=[r_rstd], writes=[r_rstd])
                    P.op("act", lambda e: e.activation(out=rstd[:], in_=rstd[:], func=AF.Sqrt, bias=epsb[:]),
                         reads=[r_rstd, r_const], writes=[r_rstd])
                    P.op("dve", lambda e: e.reciprocal(out=rstd[:], in_=rstd[:]), reads=[r_rstd], writes=[r_rstd])
                    for cc in range(4):
                        b2 = cc % 2
                        P.op("dve", lambda e, cc=cc, cs=cs, b2=b2: e.tensor_tensor(
                            out=tt[b2][:], in0=uc[:, cc, cs], in1=mean[:], op=ALU.subtract),
                            reads=[r_uc[cc], r_mean], writes=[r_tt[b2]])
                        P.op("pool", lambda e, b2=b2: e.tensor_tensor(out=tt[b2][:], in0=tt[b2][:], in1=rstd[:],
                                                                     op=ALU.mult),
                             reads=[r_tt[b2], r_rstd], writes=[r_tt[b2]])
                        P.op("act", lambda e, cc=cc, cs=cs, b2=b2: e.activation(
                            out=cvT[:, cc, cs], in_=tt[b2][:], func=AF.Silu, scale=lng[:, cc:cc + 1],
                            bias=lnb[:, cc:cc + 1]), reads=[r_tt[b2], r_cp], writes=[r_cvT[cc]])
                P.flush()

            with ExitStack() as st:
                wohg = sb(st, "wohg", [128, 4, D], BF16); wocv = sb(st, "wocv", [128, 4, D], BF16)
                wo = sb(st, "wo", [128, 8, D], BF16); r_w5 = Res()
                rw = sb(st, "rw", [128, 8, E]); rbb = sb(st, "rbb", [128, E]); bocv = sb(st, "bocv", [128, 8])
                g1b = sb(st, "g1b", [128, D]); a2b = sb(st, "a2b", [128, D]); sh2b = sb(st, "sh2b", [128, D])
                r_bt = Res()
                P.op("pool", lambda e: e.dma_start(out=wohg[:], in_=w_o_hg.rearrange("(c p) n -> p c n", p=128)),
                     writes=[r_w5], dma=True)
                P.op("pool", lambda e: e.dma_start(out=wocv[:], in_=w_o_cv.rearrange("(c p) n -> p c n", p=128)),
                     writes=[r_w5], dma=True)
                P.op("pool", lambda e: e.dma_start(out=wo[:], in_=w_out.rearrange("(c p) n -> p c n", p=128)),
                     writes=[r_w5], dma=True)
                P.op("sp", lambda e: e.dma_start(out=rw[:], in_=router_w.rearrange("(c p) n -> p c n", p=128)),
                     writes=[r_w5], dma=True)
                P.op("sp", lambda e: e.dma_start(out=rbb[:], in_=router_b.partition_broadcast(128)),
                     writes=[r_w5], dma=True)
                P.op("sp", lambda e: e.dma_start(out=bocv[:], in_=b_o_cv.rearrange("(c p) -> p c", p=128)),
                     writes=[r_w5], dma=True)
                P.op("sp", lambda e: e.dma_start(out=g1b[:], in_=mod_d[2, :].partition_broadcast(128)),
                     reads=[r_mod], writes=[r_bt], dma=True)
                P.op("sp", lambda e: e.dma_start(out=sh2b[:], in_=mod_d[3, :].partition_broadcast(128)),
                     reads=[r_mod], writes=[r_bt], dma=True)
                P.op("sp", lambda e: e.dma_start(out=a2b[:], in_=mod_d[4, :].partition_broadcast(128)),
                     reads=[r_mod], writes=[r_bt], dma=True)
                P.op("sp", lambda e: e.dma_start(out=g2b[:], in_=mod_d[5, :].partition_broadcast(128)),
                     reads=[r_mod], writes=[r_g2b], dma=True)
                P.op("sp", lambda e: e.dma_start(out=fgb[:], in_=fin_g.partition_broadcast(128)),
                     writes=[r_fgb], dma=True)

                gh = [sb(st, "gh%d" % i, [128, 512]) for i in range(2)]; r_gh = [Res(), Res()]
                gc = [sb(st, "gc%d" % i, [128, 512]) for i in range(2)]; r_gc = [Res(), Res()]
                mT = sb(st, "mT", [128, 8, 512], BF16); r_mT = Res()
                m1 = [sb(st, "m1_%d" % i, [128, 512]) for i in range(2)]
                m2 = [sb(st, "m2_%d" % i, [128, 512]) for i in range(2)]
                r_m1 = [Res(), Res()]; r_m2 = [Res(), Res()]
                p_yh = [ps(st, "p_yh%d" % i, [128, 512]) for i in range(2)]; r_pyh = [Res(), Res()]
                p_yc = [ps(st, "p_yc%d" % i, [128, 512]) for i in range(2)]; r_pyc = [Res(), Res()]
                p_o5 = [ps(st, "p_o5%d" % i, [128, 512]) for i in range(2)]; r_po5 = [Res(), Res()]
                p_tr = ps(st, "p_tr", [128, 4, 128]); r_ptr5 = Res()
                p_lg = ps(st, "p_lg", [128, 16, E]); r_plg = Res()
                xr = [sb(st, "xr%d" % i, [128, D]) for i in range(2)]; r_xr = [Res(), Res()]
                hr = [sb(st, "hr%d" % i, [128, D]) for i in range(2)]; r_hr = [Res(), Res()]
                h2f = [sb(st, "h2f%d" % i, [128, D]) for i in range(2)]; r_h2f = [Res(), Res()]
                P.op("sp", lambda e: e.dma_start(out=h2f[0][:], in_=norm2_g.partition_broadcast(128)),
                     writes=[r_h2f[0]], dma=True)
                P.op("dve", lambda e: e.scalar_tensor_tensor(out=a2b[:], in0=a2b[:], scalar=1.0, in1=h2f[0][:],
                                                             op0=ALU.add, op1=ALU.mult), reads=[r_bt, r_h2f[0]], writes=[r_bt])
                h2b = [sb(st, "h2b%d" % i, [128, D], BF16) for i in range(2)]; r_h2b = [Res(), Res()]
                junk = sb(st, "junk", [128, D], BF16); r_junk = Res()
                ss2 = sb(st, "ss2", [128, NT]); r_ss2 = [Res() for _ in range(NT)]
                h2T = [sb(st, "h2T%d" % i, [128, 8, 128]) for i in range(2)]; r_h2T = [Res(), Res()]
                lg_all = sb(st, "lg_all", [128, NT, E]); r_lgall = [Res() for _ in range(NT)]
                m8a = sb(st, "m8a", [128, NT, 8]); r_m8a = Res()
                den_all = sb(st, "den_all", [128, NT]); r_den = Res(); r_gwall = Res()
                P.op("pool", lambda e: e.memset(junk[:], 0.0), writes=[r_junk])
                P.op("sp", lambda e: e.dma_start(out=h2_d[S:S + 128, :], in_=junk[:]), reads=[r_junk], writes=[r_h2d[NT]],
                     dma=True)
                for tc in range(NCH):
                    cs = slice(tc * 512, (tc + 1) * 512)
                    for dch in range(8):
                        b2 = dch % 2
                        rh = 3584 + dch * 128
                        rc = 4608 + dch * 128
                        P.op("sp", lambda e, cs=cs, rh=rh, b2=b2: e.dma_start(out=gh[b2][:], in_=projT_d[rh:rh + 128, cs]),
                             reads=[r_projT[rh]], writes=[r_gh[b2]], dma=True)
                        P.op("sp", lambda e, cs=cs, rc=rc, b2=b2: e.dma_start(out=gc[b2][:], in_=projT_d[rc:rc + 128, cs]),
                             reads=[r_projT[rc]], writes=[r_gc[b2]], dma=True)
                        P.op("act", lambda e, b2=b2: e.activation(out=gh[b2][:], in_=gh[b2][:], func=AF.Sigmoid),
                             reads=[r_gh[b2]], writes=[r_gh[b2]])
                        P.op("act", lambda e, b2=b2: e.activation(out=gc[b2][:], in_=gc[b2][:], func=AF.Sigmoid),
                             reads=[r_gc[b2]], writes=[r_gc[b2]])
                        for kc in range(4):
                            P.op("pe", lambda e, dch=dch, kc=kc, b2=b2, cs=cs: e.matmul(
                                p_yh[b2][:], lhsT=wohg[:, kc, dch * 128:(dch + 1) * 128], rhs=hgT[:, kc, cs],
                                start=(kc == 0), stop=(kc == 3)), reads=[r_w5, r_hgT[kc]], writes=[r_pyh[b2]])
                        for kc in range(4):
                            P.op("pe", lambda e, dch=dch, kc=kc, b2=b2, cs=cs: e.matmul(
                                p_yc[b2][:], lhsT=wocv[:, kc, dch * 128:(dch + 1) * 128], rhs=cvT[:, kc, cs],
                                start=(kc == 0), stop=(kc == 3)), reads=[r_w5, r_cvT[kc]], writes=[r_pyc[b2]])
                        P.op("dve", lambda e, dch=dch, b2=b2: e.tensor_tensor(
                            out=m1[b2][:], in0=p_yh[b2][:], in1=gh[b2][:], op=ALU.mult),
                            reads=[r_pyh[b2], r_gh[b2]], writes=[r_m1[b2]])
                        P.op("dve", lambda e, dch=dch, b2=b2: e.scalar_tensor_tensor(
                            out=m2[b2][:], in0=p_yc[b2][:], scalar=bocv[:, dch:dch + 1], in1=gc[b2][:],
                            op0=ALU.add, op1=ALU.mult), reads=[r_pyc[b2], r_gc[b2], r_w5], writes=[r_m2[b2]])
                        P.op("pool", lambda e, dch=dch, b2=b2: e.tensor_tensor(
                            out=mT[:, dch, :], in0=m1[b2][:], in1=m2[b2][:], op=ALU.add),
                            reads=[r_m1[b2], r_m2[b2]], writes=[r_mT])
                    for q in range(4):
                        i = tc * 4 + q
                        b2 = i % 2
                        P.op("sp", lambda e, i=i, b2=b2: e.dma_start(out=xr[b2][:], in_=x[i * 128:(i + 1) * 128, :]),
                             writes=[r_xr[b2]], dma=True)
                        for dh in range(2):
                            for kc in range(8):
                                P.op("pe", lambda e, q=q, dh=dh, kc=kc: e.matmul(
                                    p_o5[dh][:], lhsT=mT[:, kc, q * 128:(q + 1) * 128],
                                    rhs=wo[:, kc, dh * 512:(dh + 1) * 512], start=(kc == 0), stop=(kc == 7)),
                                    reads=[r_mT, r_w5], writes=[r_po5[dh]])
                            ds_ = slice(dh * 512, (dh + 1) * 512)
                            P.op("dve", lambda e, dh=dh, ds_=ds_, b2=b2: e.tensor_tensor(
                                out=hr[b2][:, ds_], in0=p_o5[dh][:], in1=g1b[:, ds_], op=ALU.mult),
                                reads=[r_po5[dh], r_bt], writes=[r_hr[b2]])
                        P.op("pool", lambda e, b2=b2: e.tensor_tensor(out=hr[b2][:], in0=hr[b2][:], in1=xr[b2][:],
                                                                     op=ALU.add),
                             reads=[r_hr[b2], r_xr[b2]], writes=[r_hr[b2]])
                        P.op("sp", lambda e, i=i, b2=b2: e.dma_start(out=hres_d[i * 128:(i + 1) * 128, :], in_=hr[b2][:]),
                             reads=[r_hr[b2]], writes=[r_hres[i]], dma=True)
                        P.op("act", lambda e, i=i, b2=b2: e.activation(out=junk[:], in_=hr[b2][:], func=AF.Square,
                                                                        accum_out=ss2[:, i:i + 1]),
                             reads=[r_hr[b2]], writes=[r_junk, r_ss2[i]])
                        P.op("act", lambda e, i=i: e.activation(out=ss2[:, i:i + 1], in_=ss2[:, i:i + 1], func=AF.Sqrt,
                                                                scale=1.0 / D, bias=epsb[:]),
                             reads=[r_ss2[i], r_const], writes=[r_ss2[i]])
                        P.op("dve", lambda e, i=i: e.reciprocal(out=ss2[:, i:i + 1], in_=ss2[:, i:i + 1]),
                             reads=[r_ss2[i]], writes=[r_ss2[i]])
                        P.op("dve", lambda e, i=i, b2=b2: e.scalar_tensor_tensor(
                            out=h2f[b2][:], in0=hr[b2][:], scalar=ss2[:, i:i + 1], in1=a2b[:],
                            op0=ALU.mult, op1=ALU.mult), reads=[r_hr[b2], r_ss2[i], r_bt], writes=[r_h2f[b2]])
                        P.op("pool", lambda e, b2=b2: e.tensor_tensor(out=h2f[b2][:], in0=h2f[b2][:], in1=sh2b[:],
                                                                     op=ALU.add),
                             reads=[r_h2f[b2], r_bt], writes=[r_h2f[b2]])
                        P.op("act", lambda e, b2=b2: e.copy(out=h2b[b2][:], in_=h2f[b2][:]),
                             reads=[r_h2f[b2]], writes=[r_h2b[b2]])
                        P.op("sp", lambda e, i=i, b2=b2: e.dma_start(out=h2_d[i * 128:(i + 1) * 128, :], in_=h2b[b2][:]),
                             reads=[r_h2b[b2]], writes=[r_h2d[i]], dma=True)
                        for hf in range(2):
                            for k4 in range(4):
                                kc = hf * 4 + k4
                                P.op("pe", lambda e, kc=kc, k4=k4, b2=b2: e.transpose(
                                    out=p_tr[:, k4, :], in_=h2f[b2][:, kc * 128:(kc + 1) * 128], identity=ident_f[:]),
                                    reads=[r_h2f[b2], r_ident], writes=[r_ptr5])
                            P.op("act", lambda e, hf=hf, b2=b2: e.copy(out=h2T[b2][:, hf * 4:(hf + 1) * 4, :], in_=p_tr[:]),
                                 reads=[r_ptr5], writes=[r_h2T[b2]])
                        for kc in range(8):
                            P.op("pe", lambda e, kc=kc, b2=b2: e.matmul(p_lg[:, 0, :], lhsT=h2T[b2][:, kc, :], rhs=rw[:, kc, :],
                                                                 start=(kc == 0), stop=(kc == 7)),
                                 reads=[r_h2T[b2], r_w5], writes=[r_plg])
                        P.op("dve", lambda e, i=i: e.tensor_tensor(out=lg_all[:, i, :], in0=p_lg[:, 0, :], in1=rbb[:],
                                                                    op=ALU.add),
                             reads=[r_plg, r_w5], writes=[r_lgall[i]])
                tot_all = xr[0][:].rearrange("p (a b) -> p a b", a=NT); r_tot = r_xr[0]
                cum_all = xr[1][:].rearrange("p (a b) -> p a b", a=NT); r_cumall = r_xr[1]
                for i in range(NT):
                    P.op("dve", lambda e, i=i: e.max(out=m8a[:, i, :], in_=lg_all[:, i, :]),
                         reads=[r_lgall[i]], writes=[r_m8a])
                P.op("dve", lambda e: e.tensor_tensor(out=mask_all[:], in0=lg_all[:],
                                                      in1=bc(m8a[:, :, 3:4], [128, NT, E]), op=ALU.is_ge),
                     reads=r_lgall + [r_m8a], writes=r_maskall)
                P.op("dve", lambda e: e.tensor_tensor(out=lg_all[:], in0=lg_all[:],
                                                      in1=bc(m8a[:, :, 0:1], [128, NT, E]), op=ALU.subtract),
                     reads=r_lgall + [r_m8a], writes=r_lgall)
                P.op("act", lambda e: e.activation(out=lg_all[:], in_=lg_all[:], func=AF.Exp),
                     reads=r_lgall, writes=r_lgall)
                P.op("dve", lambda e: e.tensor_tensor(out=lg_all[:], in0=lg_all[:], in1=mask_all[:], op=ALU.mult),
                     reads=r_lgall + r_maskall, writes=r_lgall)
                P.op("dve", lambda e: e.reduce_sum(out=den_all[:], in_=lg_all[:], axis=AX.X), reads=r_lgall, writes=[r_den])
                P.op("dve", lambda e: e.reciprocal(out=den_all[:], in_=den_all[:]), reads=[r_den], writes=[r_den])
                P.op("dve", lambda e: e.tensor_tensor(out=gw_all[:], in0=lg_all[:],
                                                      in1=bc(den_all[:, :].unsqueeze(2), [128, NT, E]), op=ALU.mult),
                     reads=r_lgall + [r_den], writes=[r_gwall])
                for i in range(NT):
                    P.op("pe", lambda e, i=i: e.matmul(p_yh[i // 16][:, (i % 16) * E:(i % 16 + 1) * E], lhsT=ones_f[:],
                                                       rhs=mask_all[:, i, :], start=True, stop=True),
                         reads=[r_maskall[i], r_ones], writes=[r_pyh[i // 16]])
                    P.op("pe", lambda e, i=i: e.matmul(p_yc[i // 16][:, (i % 16) * E:(i % 16 + 1) * E], lhsT=lstrict[:],
                                                       rhs=mask_all[:, i, :], start=True, stop=True),
                         reads=[r_maskall[i], r_const], writes=[r_pyc[i // 16]])
                for hf in range(2):
                    P.op("act", lambda e, hf=hf: e.copy(out=tot_all[:, hf * 16:(hf + 1) * 16, :].rearrange("p a b -> p (a b)"),
                                                        in_=p_yh[hf][:]), reads=[r_pyh[hf]], writes=[r_tot])
                P.op("pool", lambda e: e.memset(cum_all[:, 0, :], 0.0), writes=[r_cumall])
                for i in range(1, NT):
                    P.op("dve", lambda e, i=i: e.tensor_tensor(out=cum_all[:, i, :], in0=cum_all[:, i - 1, :],
                                                                in1=tot_all[:, i - 1, :], op=ALU.add),
                         reads=[r_cumall, r_tot], writes=[r_cumall])
                P.op("dve", lambda e: e.tensor_tensor(out=cnt_b[:], in0=cum_all[:, NT - 1, :], in1=tot_all[:, NT - 1, :],
                                                      op=ALU.add), reads=[r_cumall, r_tot], writes=[r_cnt])
                for hf in range(2):
                    P.op("dve", lambda e, hf=hf: e.tensor_tensor(
                        out=rank_all[:, hf * 16:(hf + 1) * 16, :].rearrange("p a b -> p (a b)"), in0=p_yc[hf][:],
                        in1=cum_all[:, hf * 16:(hf + 1) * 16, :].rearrange("p a b -> p (a b)"), op=ALU.add),
                        reads=[r_pyc[hf], r_cumall], writes=r_rankall)
                P.flush()

        with ExitStack() as st:
            padb = sb(st, "padb", [128, E]); endb = sb(st, "endb", [128, E]); startb = sb(st, "startb", [128, E])
            r_rt = Res()
            P.op("pool", lambda e: e.memset(padb[:], 0.0), writes=[r_rt])
            for k in range(8):
                P.op("dve", lambda e, k=k: e.scalar_tensor_tensor(out=padb[:], in0=cnt_b[:], scalar=float(512 * k + 1),
                                                                  in1=padb[:], op0=ALU.is_ge, op1=ALU.add),
                     reads=[r_cnt, r_rt], writes=[r_rt])
            P.op("dve", lambda e: e.tensor_scalar_mul(out=padb[:], in0=padb[:], scalar1=512.0), reads=[r_rt], writes=[r_rt])
            P.op("dve", lambda e: e.tensor_tensor_scan(out=endb[:], data0=ones_f[:, 0:E], data1=padb[:], initial=0.0,
                                                       op0=ALU.mult, op1=ALU.add), reads=[r_rt, r_ones], writes=[r_rt])
            P.op("dve", lambda e: e.tensor_tensor(out=startb[:], in0=endb[:], in1=padb[:], op=ALU.subtract),
                 reads=[r_rt], writes=[r_rt])
            dsel = sb(st, "dsel", [128, NT, E]); r_dsel = Res()
            P.op("dve", lambda e: e.tensor_tensor(out=dsel[:], in0=rank_all[:], in1=bc(startb[:, :].unsqueeze(1), [128, NT, E]),
                                                  op=ALU.add), reads=r_rankall + [r_rt], writes=[r_dsel])
            P.op("dve", lambda e: e.scalar_tensor_tensor(out=dsel[:], in0=dsel[:], scalar=1.0, in1=mask_all[:],
                                                         op0=ALU.add, op1=ALU.mult),
                 reads=[r_dsel] + r_maskall, writes=[r_dsel])
            t8 = sb(st, "t8", [128, NT, 8]); r_t8 = Res()
            oh = sb(st, "oh", [128, E]); r_oh_ = Res()
            d4f = sb(st, "d4f", [128, NT, 4]); r_d4f = Res()
            junk2 = sb(st, "junk2", [128, E])
            for i in range(NT):
                P.op("dve", lambda e, i=i: e.max(out=t8[:, i, :], in_=dsel[:, i, :]), reads=[r_dsel], writes=[r_t8])
                for k in range(4):
                    P.op("dve", lambda e, i=i, k=k: e.tensor_scalar(out=oh[:], in0=dsel[:, i, :], scalar1=t8[:, i, k:k + 1],
                                                                     scalar2=None, op0=ALU.is_equal),
                         reads=[r_dsel, r_t8], writes=[r_oh_])
                    P.op("dve", lambda e, i=i: e.tensor_tensor(out=junk2[:], in0=oh[:], in1=gw_all[:, i, :], op=ALU.mult),
                         reads=[r_oh_, r_gwall], writes=[r_oh_])
                    P.op("dve", lambda e, i=i, k=k: e.reduce_sum(out=w4[:, i, k:k + 1], in_=junk2[:], axis=AX.X),
                         reads=[r_oh_], writes=[r_w4])
            P.op("dve", lambda e: e.tensor_scalar_add(out=d4f[:], in0=t8[:, :, 0:4], scalar1=-1.0),
                 reads=[r_t8], writes=[r_d4f])
            P.op("dve", lambda e: e.tensor_copy(out=dest4i[:], in_=d4f[:]), reads=[r_d4f], writes=[r_dest4])
            fill = sb(st, "fill", [128, NBLK * 4], I32); r_fill = Res()
            tokid = sb(st, "tokid", [128, NT], I32); r_tok = Res()
            P.op("pool", lambda e: e.iota(fill[:], pattern=[[0, NBLK * 4]], base=S, channel_multiplier=0), writes=[r_fill])
            P.op("pool", lambda e: e.iota(tokid[:], pattern=[[128, NT]], base=0, channel_multiplier=1), writes=[r_tok])
            P.op("sp", lambda e: e.dma_start(out=slot_d.rearrange("(p c) o -> p (c o)", p=128), in_=fill[:]),
                 reads=[r_fill], writes=[r_slotd], dma=True)
            r_sc = [Res() for _ in range(NT * 4)]
            for i in range(NT):
                for k in range(4):
                    P.op("pool", lambda e, i=i, k=k: e.indirect_dma_start(
                        out=slot_d[:, :], out_offset=bass.IndirectOffsetOnAxis(ap=dest4i[:, i, k:k + 1], axis=0),
                        in_=tokid[:, i:i + 1], in_offset=None),
                        reads=[r_dest4, r_tok, r_slotd], writes=[r_sc[i * 4 + k]], dma=True)
            for g in range(8):
                P.op("sp", lambda e, g=g: e.dma_start(
                    out=slot_sb[:, g * 32:(g + 1) * 32],
                    in_=slot_d[g * 4096:(g + 1) * 4096, :].rearrange("(c p) o -> p (c o)", p=128)),
                    reads=[r_slotd] + r_sc, writes=[r_slot_sb], dma=True)
            thr = sb(st, "thr", [128, NBLK]); cmp = sb(st, "cmp", [128, NBLK, E]); beb = sb(st, "beb", [128, NBLK])
            iokc = sb(st, "iokc", [128, 8]); wif = sb(st, "wif", [128, NBLK, 8]); r_be = Res()
            P.op("pool", lambda e: e.iota(thr[:], pattern=[[512, NBLK]], base=0, channel_multiplier=0,
                                          allow_small_or_imprecise_dtypes=True), writes=[r_be])
            P.op("pool", lambda e: e.iota(iokc[:], pattern=[[128, 8]], base=0, channel_multiplier=1,
                                          allow_small_or_imprecise_dtypes=True), writes=[r_be])
            P.op("dve", lambda e: e.tensor_tensor(out=cmp[:], in0=bc(thr[:, :].unsqueeze(2), [128, NBLK, E]),
                                                  in1=bc(endb[:, :].unsqueeze(1), [128, NBLK, E]), op=ALU.is_ge),
                 reads=[r_rt, r_be], writes=[r_be])
            P.op("dve", lambda e: e.reduce_sum(out=beb[:], in_=cmp[:], axis=AX.X), reads=[r_be], writes=[r_be])
            P.op("dve", lambda e: e.tensor_scalar_min(out=beb[:], in0=beb[:], scalar1=float(E - 1)), reads=[r_be], writes=[r_be])
            P.op("dve", lambda e: e.tensor_scalar(out=ohb[:], in0=beb[0:32, :], scalar1=iota_p[0:32, 0:1], scalar2=None,
                                                   op0=ALU.is_equal), reads=[r_be, r_const], writes=[r_ohb])
            P.op("dve", lambda e: e.scalar_tensor_tensor(out=wif[:], in0=bc(beb[:, :].unsqueeze(2), [128, NBLK, 8]),
                                                         scalar=1024.0, in1=bc(iokc[:, :].unsqueeze(1), [128, NBLK, 8]),
                                                         op0=ALU.mult, op1=ALU.add), reads=[r_be], writes=[r_be])
            same = sb(st, "same", [128, NBLK]); pm1 = sb(st, "pm1", [128, 1])
            P.op("dve", lambda e: e.memset(same[:, 0:1], 0.0), reads=[r_be], writes=[r_be])
            P.op("dve", lambda e: e.tensor_tensor(out=same[:, 1:NBLK], in0=beb[:, 1:NBLK], in1=beb[:, 0:NBLK - 1],
                                                  op=ALU.is_equal), reads=[r_be], writes=[r_be])
            P.op("dve", lambda e: e.tensor_scalar(out=pm1[:], in0=iota_p[:], scalar1=1.0, scalar2=1048576.0,
                                                   op0=ALU.min, op1=ALU.mult), reads=[r_be, r_const], writes=[r_be])
            P.op("dve", lambda e: e.tensor_scalar_mul(out=same[:], in0=same[:], scalar1=pm1[:, 0:1]),
                 reads=[r_be], writes=[r_be])
            P.op("dve", lambda e: e.tensor_tensor(out=wif[:], in0=wif[:], in1=bc(same[:, :].unsqueeze(2), [128, NBLK, 8]),
                                                  op=ALU.add), reads=[r_be], writes=[r_be])
            P.op("dve", lambda e: e.tensor_copy(out=widx[:], in_=wif[:]), reads=[r_be], writes=[r_widx])
            if debug:
                P.op("sp", lambda e: e.dma_start(out=dbg_d[:, 0:NT * E], in_=gw_all[:].rearrange("p a b -> p (a b)")),
                     reads=r_maskall, writes=[Res()], dma=True)
                P.op("sp", lambda e: e.dma_start(out=dbg_d[:, NT * E:NT * E + NBLK], in_=beb[:]),
                     reads=[r_be], writes=[Res()], dma=True)
                P.op("sp", lambda e: e.dma_start(out=dbg_d[:, NT * E + NBLK:NT * E + NBLK + NT * 4],
                                                 in_=d4f[:].rearrange("p a b -> p (a b)")),
                     reads=[r_d4f], writes=[Res()], dma=True)
            P.flush()
        stR.close()

        with ExitStack() as st:
            bgu = sb(st, "bgu", [E, 2 * D], BF16); bdn = sb(st, "bdn", [E, D], BF16); r_bias = Res()
            P.op("pool", lambda e: e.dma_start(out=bgu[:], in_=b_gu[:, :]), writes=[r_bias], dma=True)
            P.op("pool", lambda e: e.dma_start(out=bdn[:], in_=b_dn[:, :]), writes=[r_bias], dma=True)
            wgu = [sb(st, "wgu%d" % i, [128, 8, 2 * D], BF16) for i in range(2)]; r_wgu = [[Res() for _ in range(8)] for _ in range(2)]
            wdn = [sb(st, "wdn%d" % i, [128, 8, D], BF16) for i in range(2)]; r_wdn = [[Res() for _ in range(8)] for _ in range(2)]
            xg = [sb(st, "xg%d" % i, [128, 4, D], BF16) for i in range(2)]; r_xg = [[Res() for _ in range(4)] for _ in range(2)]
            xT = sb(st, "xT", [128, 8, 512], BF16); r_xT = Res()
            aT = sb(st, "aT", [128, 8, 512], BF16); r_aT = Res()
            ohj = [sb(st, "ohj%d" % i, [E, 512], BF16) for i in range(2)]; r_ohj = [Res(), Res()]
            yst = sb(st, "yst", [128, 4, D]); r_yst = Res()
            gp = [sb(st, "gp%d" % i, [128, 512]) for i in range(2)]; r_gp = [Res(), Res()]
            sg_ = [sb(st, "sg%d" % i, [128, 512]) for i in range(2)]; r_sg = [Res(), Res()]
            up = [sb(st, "up%d" % i, [128, 512]) for i in range(2)]; r_up7 = [Res(), Res()]
            p_x = [ps(st, "p_x%d" % i, [128, 8, 128], BF16) for i in range(2)]; r_px = [Res(), Res()]
            p_g = [ps(st, "p_g%d" % i, [128, 512]) for i in range(2)]; r_pg = [Res(), Res()]
            p_u = [ps(st, "p_u%d" % i, [128, 512]) for i in range(2)]; r_pu = [Res(), Res()]
            p_y = [ps(st, "p_y%d" % i, [128, 512]) for i in range(2)]; r_py = [Res(), Res()]

            bnd_reg = []

            def get_bnd(e):
                if not bnd_reg:
                    r = e.alloc_register("bnd_reg")
                    e.reg_mov(r, E * D - 1)
                    bnd_reg.append(r)
                return bnd_reg[0]

            def load_block(j):
                bi = j % 2
                if j >= 1:
                    P.op("dve", lambda e, bi=bi: e.tensor_copy(out=wgu[bi][:], in_=wgu[1 - bi][:]),
                         reads=r_wgu[1 - bi], writes=r_wgu[bi])
                    P.op("dve", lambda e, bi=bi: e.tensor_copy(out=wdn[bi][:], in_=wdn[1 - bi][:]),
                         reads=r_wdn[1 - bi], writes=r_wdn[bi])
                for kc in range(8):
                    P.op("pool", lambda e, j=j, kc=kc, bi=bi: e.indirect_dma_start(
                        out=wgu[bi][:, kc, :], out_offset=None, in_=w_gu[:, :],
                        in_offset=bass.IndirectOffsetOnAxis(ap=widx[:, j, kc:kc + 1], axis=0),
                        bounds_check=get_bnd(e), oob_is_err=False),
                        reads=[r_widx], writes=[r_wgu[bi][kc]], dma=True)
                for kc in range(8):
                    P.op("pool", lambda e, j=j, kc=kc, bi=bi: e.indirect_dma_start(
                        out=wdn[bi][:, kc, :], out_offset=None, in_=w_dn[:, :],
                        in_offset=bass.IndirectOffsetOnAxis(ap=widx[:, j, kc:kc + 1], axis=0),
                        bounds_check=get_bnd(e), oob_is_err=False),
                        reads=[r_widx], writes=[r_wdn[bi][kc]], dma=True)
                for q in range(4):
                    P.op("pool", lambda e, j=j, q=q, bi=bi: e.indirect_dma_start(
                        out=xg[bi][:, q, :], out_offset=None, in_=h2_d[:, :],
                        in_offset=bass.IndirectOffsetOnAxis(ap=slot_sb[:, j * 4 + q:j * 4 + q + 1], axis=0)),
                        reads=[r_slot_sb] + r_h2d, writes=[r_xg[bi][q]], dma=True)
                P.op("dve", lambda e, j=j, bi=bi: e.tensor_copy(out=ohj[bi][:], in_=bc(ohb[:, j:j + 1], [E, 512])),
                     reads=[r_ohb], writes=[r_ohj[bi]])

            load_block(0)
            pxi = 0
            for j in range(NBLK):
                bi = j % 2
                if j + 1 < NBLK:
                    load_block(j + 1)
                for kc in range(8):
                    pb = pxi % 2; pxi += 1
                    for q in range(4):
                        P.op("pe", lambda e, kc=kc, q=q, pb=pb, bi=bi: e.transpose(
                            out=p_x[pb][:, q, :], in_=xg[bi][:, q, kc * 128:(kc + 1) * 128], identity=ident_b[:]),
                            reads=[r_xg[bi][q], r_const], writes=[r_px[pb]])
                    P.op("act", lambda e, kc=kc, pb=pb: e.copy(out=xT[:, kc, :].rearrange("p (a b) -> p a b", a=4), in_=p_x[pb][:, 0:4, :]),
                         reads=[r_px[pb]], writes=[r_xT])
                for m in range(8):
                    b2 = m % 2
                    for (pt_, rp, col0) in ((p_g[b2], r_pg[b2], m * 128), (p_u[b2], r_pu[b2], D + m * 128)):
                        for kc in range(8):
                            P.op("pe", lambda e, pt_=pt_, kc=kc, col0=col0, bi=bi: e.matmul(
                                pt_[:], lhsT=wgu[bi][:, kc, col0:col0 + 128], rhs=xT[:, kc, :],
                                start=(kc == 0), stop=False), reads=[r_wgu[bi][kc], r_xT], writes=[rp])
                        P.op("pe", lambda e, pt_=pt_, col0=col0, bi=bi: e.matmul(
                            pt_[:], lhsT=bgu[:, col0:col0 + 128], rhs=ohj[bi][:], start=False, stop=True),
                            reads=[r_bias, r_ohj[bi]], writes=[rp])
                    P.op("dve", lambda e, b2=b2: e.tensor_scalar_min(out=gp[b2][:], in0=p_g[b2][:], scalar1=7.0),
                         reads=[r_pg[b2]], writes=[r_gp[b2]])
                    P.op("act", lambda e, b2=b2: e.activation(out=sg_[b2][:], in_=gp[b2][:], func=AF.Sigmoid, scale=1.702),
                         reads=[r_gp[b2]], writes=[r_sg[b2]])
                    P.op("dve", lambda e, b2=b2: e.tensor_scalar(out=up[b2][:], in0=p_u[b2][:], scalar1=-7.0, scalar2=7.0,
                                                                  op0=ALU.max, op1=ALU.min),
                         reads=[r_pu[b2]], writes=[r_up7[b2]])
                    P.op("dve", lambda e, b2=b2: e.tensor_tensor(out=gp[b2][:], in0=gp[b2][:], in1=sg_[b2][:], op=ALU.mult),
                         reads=[r_gp[b2], r_sg[b2]], writes=[r_gp[b2]])
                    P.op("dve", lambda e, b2=b2, m=m: e.scalar_tensor_tensor(
                        out=aT[:, m, :], in0=up[b2][:], scalar=1.0, in1=gp[b2][:], op0=ALU.add, op1=ALU.mult),
                        reads=[r_up7[b2], r_gp[b2]], writes=[r_aT])
                for q in range(4):
                    for dh in range(2):
                        pb = (q * 2 + dh) % 2
                        for m in range(8):
                            P.op("pe", lambda e, q=q, dh=dh, m=m, pb=pb, bi=bi: e.matmul(
                                p_y[pb][:], lhsT=aT[:, m, q * 128:(q + 1) * 128], rhs=wdn[bi][:, m, dh * 512:(dh + 1) * 512],
                                start=(m == 0), stop=False), reads=[r_aT, r_wdn[bi][m]], writes=[r_py[pb]])
                        P.op("pe", lambda e, dh=dh, pb=pb, bi=bi: e.matmul(
                            p_y[pb][:], lhsT=ohj[bi][:, 0:128], rhs=bdn[:, dh * 512:(dh + 1) * 512], start=False, stop=True),
                            reads=[r_ohj[bi], r_bias], writes=[r_py[pb]])
                        if dh == 0:
                            P.op("act", lambda e, q=q, pb=pb: e.copy(out=yst[:, q, 0:512], in_=p_y[pb][:]),
                                 reads=[r_py[pb]], writes=[r_yst])
                        else:
                            P.op("dve", lambda e, q=q, pb=pb: e.tensor_copy(out=yst[:, q, 512:1024], in_=p_y[pb][:]),
                                 reads=[r_py[pb]], writes=[r_yst])
                P.op("sp", lambda e, j=j: e.dma_start(
                    out=ys_d[j * 512:(j + 1) * 512, :].rearrange("(q p) d -> p q d", p=128), in_=yst[:]),
                    reads=[r_yst], writes=[r_ysd], dma=True)
            P.flush()

        with ExitStack() as st:
            G = [[sb(st, "G%d_%d" % (b_, k), [128, D]) for k in range(4)] for b_ in range(2)]
            r_G = [[Res() for _ in range(4)] for _ in range(2)]
            hx = [sb(st, "hx%d" % i, [128, D]) for i in range(2)]; r_hx = [Res(), Res()]
            ac = [sb(st, "ac%d" % i, [128, D]) for i in range(2)]; r_ac = [Res(), Res()]
            ot = [sb(st, "ot%d" % i, [128, D]) for i in range(2)]; r_ot = [Res(), Res()]
            junk3 = sb(st, "junk3", [128, D], BF16); r_j3 = Res()
            ss3 = sb(st, "ss3", [128, NT]); r_ss3 = [Res() for _ in range(NT)]
            r_out = Res()
            for i in range(NT):
                b2 = i % 2
                for k in range(4):
                    P.op("pool", lambda e, i=i, k=k, b2=b2: e.indirect_dma_start(
                        out=G[b2][k][:], out_offset=None, in_=ys_d[:, :],
                        in_offset=bass.IndirectOffsetOnAxis(ap=dest4i[:, i, k:k + 1], axis=0)),
                        reads=[r_dest4, r_ysd], writes=[r_G[b2][k]], dma=True)
                P.op("sp", lambda e, i=i, b2=b2: e.dma_start(out=hx[b2][:], in_=hres_d[i * 128:(i + 1) * 128, :]),
                     reads=[r_hres[i]], writes=[r_hx[b2]], dma=True)
                P.op("dve", lambda e, i=i, b2=b2: e.tensor_scalar_mul(out=ac[b2][:], in0=G[b2][0][:], scalar1=w4[:, i, 0:1]),
                     reads=[r_G[b2][0], r_w4], writes=[r_ac[b2]])
                for k in range(1, 4):
                    P.op("dve", lambda e, i=i, k=k, b2=b2: e.scalar_tensor_tensor(
                        out=ac[b2][:], in0=G[b2][k][:], scalar=w4[:, i, k:k + 1], in1=ac[b2][:], op0=ALU.mult, op1=ALU.add),
                        reads=[r_G[b2][k], r_w4, r_ac[b2]], writes=[r_ac[b2]])
                P.op("dve", lambda e, b2=b2: e.tensor_tensor(out=ac[b2][:], in0=ac[b2][:], in1=g2b[:], op=ALU.mult),
                     reads=[r_ac[b2], r_g2b], writes=[r_ac[b2]])
                P.op("dve", lambda e, b2=b2: e.tensor_tensor(out=ac[b2][:], in0=ac[b2][:], in1=hx[b2][:], op=ALU.add),
                     reads=[r_ac[b2], r_hx[b2]], writes=[r_ac[b2]])
                P.op("act", lambda e, i=i, b2=b2: e.activation(out=junk3[:], in_=ac[b2][:], func=AF.Square,
                                                                accum_out=ss3[:, i:i + 1]),
                     reads=[r_ac[b2]], writes=[r_j3, r_ss3[i]])
                P.op("act", lambda e, i=i: e.activation(out=ss3[:, i:i + 1], in_=ss3[:, i:i + 1], func=AF.Sqrt,
                                                        scale=1.0 / D, bias=epsb[:]),
                     reads=[r_ss3[i], r_const], writes=[r_ss3[i]])
                P.op("dve", lambda e, i=i: e.reciprocal(out=ss3[:, i:i + 1], in_=ss3[:, i:i + 1]),
                     reads=[r_ss3[i]], writes=[r_ss3[i]])
                P.op("dve", lambda e, i=i, b2=b2: e.scalar_tensor_tensor(
                    out=ot[b2][:], in0=ac[b2][:], scalar=ss3[:, i:i + 1], in1=fgb[:], op0=ALU.mult, op1=ALU.mult),
                    reads=[r_ac[b2], r_ss3[i], r_fgb], writes=[r_ot[b2]])
                P.op("sp", lambda e, i=i, b2=b2: e.dma_start(out=out[i * 128:(i + 1) * 128, :], in_=ot[b2][:]),
                     reads=[r_ot[b2]], writes=[r_out], dma=True)
            P.flush()
    return nc


_NC_CACHE = {}


def _get_nc(debug=False):
    if debug not in _NC_CACHE:
        _NC_CACHE[debug] = build_nc(debug)
    return _NC_CACHE[debug]


def make_in_maps(inputs, cores):
    g = lambda k: np.ascontiguousarray(np.asarray(inputs[k], dtype=np.float32))
    shared = {
        "ada_w": g("ada_w")[0], "ada_b": g("ada_b")[0], "norm1_g": g("norm1_g")[0], "w_in": g("w_in")[0],
        "lb_table": g("lb_table"), "hg_norm_g": g("hg_norm_g")[0], "w_o_hg": g("w_o_hg")[0],
        "dw_w": g("dw_w")[0], "dw_b": g("dw_b")[0], "cv_ln_g": g("cv_ln_g")[0], "cv_ln_b": g("cv_ln_b")[0],
        "w_o_cv": g("w_o_cv")[0], "b_o_cv": g("b_o_cv")[0], "w_out": g("w_out")[0], "norm2_g": g("norm2_g")[0],
        "router_w": g("router_w")[0], "router_b": g("router_b")[0],
        "w_gate_up": g("w_gate_up")[0].reshape(E * D, 2 * D), "b_gate_up": g("b_gate_up")[0],
        "w_down": g("w_down")[0].reshape(E * D, D), "b_down": g("b_down")[0],
        "final_norm_g": g("final_norm_g"),
    }
    xs = g("x")
    cs = g("c")
    maps = []
    for b in cores:
        m = dict(shared)
        m["x"] = xs[b]
        m["c"] = cs[b]
        maps.append(m)
    return maps


def kernel(**inputs):
    nc = _get_nc(False)
    maps = make_in_maps(inputs, list(range(8)))
    res = run_bass_kernel_spmd(nc, maps, core_ids=list(range(8)))
    return np.stack([np.asarray(r["out"], dtype=np.float32) for r in res.results], axis=0)
```

```python
import os
from contextlib import ExitStack

import numpy as np
import concourse.bass as bass
import concourse.mybir as mybir
from concourse.bass_utils import run_bass_kernel_spmd

F32 = mybir.dt.float32
BF16 = mybir.dt.bfloat16
I32 = mybir.dt.int32
AF = mybir.ActivationFunctionType
ALU = mybir.AluOpType
AX = mybir.AxisListType

S = 4096
D = 1024
NT = 32
NCH = 8
E = 32
NBLK = 64
NSLOT = NBLK * 512
EPS = 1e-6
IN_COLS = 5632

ENGS = ("pe", "act", "dve", "pool", "sp")


class Res:
    __slots__ = ("last_w", "readers")

    def __init__(self):
        self.last_w = None
        self.readers = {}


class Prog:
    def __init__(self, nc, n_dma_sems=48):
        self.nc = nc
        self.ops = {e: [] for e in ENGS}
        self.seq = {e: 0 for e in ENGS}
        self.waited = {e: {} for e in ENGS}
        self.sems = {}
        self.n_dma = n_dma_sems
        self.dma_uses = [0] * n_dma_sems
        self.dma_rr = 0
        self.same_engine_sync = True

    def alloc(self, stack):
        for e in ENGS:
            self.sems["c_" + e] = stack.enter_context(self.nc.semaphore("c_" + e))
        for i in range(self.n_dma):
            self.sems["d%d" % i] = stack.enter_context(self.nc.semaphore("d%d" % i))

    def op(self, eng, fn, reads=(), writes=(), dma=False):
        deps = {}

        def add(s, v):
            if deps.get(s, 0) < v:
                deps[s] = v

        for r in reads:
            if r.last_w is not None:
                add(*r.last_w)
        for w in writes:
            if w.last_w is not None:
                add(*w.last_w)
            for s, v in w.readers.items():
                add(s, v)
        if dma:
            i = self.dma_rr
            self.dma_rr = (self.dma_rr + 1) % self.n_dma
            s = "d%d" % i
            if self.dma_uses[i] > 0:
                add(s, 16 * self.dma_uses[i])
            self.dma_uses[i] += 1
            ev = (s, 16 * self.dma_uses[i])
        else:
            self.seq[eng] += 1
            ev = ("c_" + eng, self.seq[eng])
        waits = []
        wd = self.waited[eng]
        for s, v in deps.items():
            if s == "c_" + eng and (eng == "pe" or not self.same_engine_sync):
                continue
            if wd.get(s, 0) >= v:
                continue
            wd[s] = v
            waits.append((s, v))
        self.ops[eng].append((waits, fn, ev, dma))
        for r in reads:
            if r.readers.get(ev[0], 0) < ev[1]:
                r.readers[ev[0]] = ev[1]
        for w in writes:
            w.last_w = ev
            w.readers = {}
        return ev

    def barrier(self):
        allev = []
        for e in ENGS:
            if self.seq[e] > 0:
                allev.append(("c_" + e, self.seq[e]))
        for i in range(self.n_dma):
            if self.dma_uses[i] > 0:
                allev.append(("d%d" % i, 16 * self.dma_uses[i]))
        for e in ENGS:
            waits = []
            wd = self.waited[e]
            for s, v in allev:
                if s == "c_" + e:
                    continue
                if wd.get(s, 0) >= v:
                    continue
                wd[s] = v
                waits.append((s, v))
            if waits:
                self.ops[e].append((waits, None, None, False))

    def replay(self, eng, e):
        for waits, fn, ev, dma in self.ops[eng]:
            for s, v in waits:
                e.wait_ge(self.sems[s], v)
            if fn is None:
                continue
            inst = fn(e)
            inst.then_inc(self.sems[ev[0]], 16 if dma else 1)

    def run(self):
        nc = self.nc
        with nc.Block() as block:
            @block.tensor
            def _(e):
                self.replay("pe", e)

            @block.scalar
            def _(e):
                self.replay("act", e)

            @block.vector
            def _(e):
                self.replay("dve", e)

            @block.gpsimd
            def _(e):
                self.replay("pool", e)

            @block.sync
            def _(e):
                self.replay("sp", e)
        self.ops = {e: [] for e in ENGS}

    def flush(self):
        self.barrier()
        self.run()


def bc(ap, shape):
    return ap.broadcast_to(list(shape))


def build_nc(debug=False):
    nc = bass.Bass("TRN2", target_bir_lowering=False)

    def din(name, shape, dt=F32):
        return nc.dram_tensor(name, list(shape), dt, kind="ExternalInput").ap()

    def dscr(name, shape, dt=F32, out=False):
        return nc.dram_tensor(name, list(shape), dt, kind="ExternalOutput" if out else "Internal").ap()

    x = din("x", [S, D])
    c_in = din("c", [D])
    ada_w = din("ada_w", [D, 6 * D])
    ada_b = din("ada_b", [6 * D])
    norm1_g = din("norm1_g", [D])
    w_in = din("w_in", [D, IN_COLS])
    lb_table = din("lb_table", [2, 2, 512])
    hg_norm_g = din("hg_norm_g", [4, 128])
    w_o_hg = din("w_o_hg", [512, D])
    dw_w = din("dw_w", [31, 512])
    dw_b = din("dw_b", [512])
    cv_ln_g = din("cv_ln_g", [512])
    cv_ln_b = din("cv_ln_b", [512])
    w_o_cv = din("w_o_cv", [512, D])
    b_o_cv = din("b_o_cv", [D])
    w_out = din("w_out", [D, D])
    norm2_g = din("norm2_g", [D])
    router_w = din("router_w", [D, E])
    router_b = din("router_b", [E])
    w_gu = din("w_gate_up", [E * D, 2 * D])
    b_gu = din("b_gate_up", [E, 2 * D])
    w_dn = din("w_down", [E * D, D])
    b_dn = din("b_down", [E, D])
    fin_g = din("final_norm_g", [D])
    out = nc.dram_tensor("out", [S, D], F32, kind="ExternalOutput").ap()

    mod_d = dscr("mod_d", [6, D])
    projT_d = dscr("projT_d", [IN_COLS, S])
    vtok_d = dscr("vtok_d", [S, 512], BF16)
    hres_d = dscr("hres_d", [S, D], F32, out=debug)
    h2_d = dscr("h2_d", [S + 128, D], BF16)
    slot_d = dscr("slot_d", [NSLOT, 1], I32)
    ys_d = dscr("ys_d", [NSLOT, D], F32)
    dbg_d = dscr("dbg_d", [128, 32 * 40], F32, out=True) if debug else None

    with ExitStack() as top:
        P = Prog(nc)
        P.alloc(top)
        top.enter_context(nc.allow_non_contiguous_dma(reason="small parameter / index layouts"))

        def sb(st, name, shape, dt=F32):
            return st.enter_context(nc.sbuf_tensor(name, list(shape), dt))

        def ps(st, name, shape, dt=F32):
            return st.enter_context(nc.psum_tensor(name, list(shape), dt))

        ident_f = sb(top, "ident_f", [128, 128]); r_ident = Res()
        ident_b = sb(top, "ident_b", [128, 128], BF16)
        ones_f = sb(top, "ones_f", [128, 128]); r_ones = Res()
        lstrict = sb(top, "lstrict", [128, 128])
        epsb = sb(top, "epsb", [128, 1])
        iota_p = sb(top, "iota_p", [128, 1])
        g2b = sb(top, "g2b", [128, D]); r_g2b = Res()
        fgb = sb(top, "fgb", [128, D]); r_fgb = Res()
        dest4i = sb(top, "dest4i", [128, NT, 4], I32); r_dest4 = Res()
        w4 = sb(top, "w4", [128, NT, 4]); r_w4 = Res()
        slot_sb = sb(top, "slot_sb", [128, NBLK * 4], I32); r_slot_sb = Res()
        widx = sb(top, "widx", [128, NBLK, 8], I32); r_widx = Res()
        ohb = sb(top, "ohb", [32, NBLK]); r_ohb = Res()
        r_const = Res()

        P.op("pool", lambda e: e.memset(ident_f[:], 1.0), writes=[r_ident])
        P.op("pool", lambda e: e.affine_select(out=ident_f[:], in_=ident_f[:], pattern=[[-1, 128]],
                                               compare_op=ALU.is_equal, fill=0.0, base=0, channel_multiplier=1),
             reads=[r_ident], writes=[r_ident])
        P.op("dve", lambda e: e.tensor_copy(out=ident_b[:], in_=ident_f[:]), reads=[r_ident], writes=[r_const])
        P.op("pool", lambda e: e.memset(ones_f[:], 1.0), writes=[r_ones])
        P.op("pool", lambda e: e.memset(lstrict[:], 1.0), writes=[r_const])
        P.op("pool", lambda e: e.affine_select(out=lstrict[:], in_=lstrict[:], pattern=[[1, 128]],
                                               compare_op=ALU.is_ge, fill=0.0, base=-1, channel_multiplier=-1),
             reads=[r_const], writes=[r_const])
        P.op("pool", lambda e: e.memset(epsb[:], EPS), writes=[r_const])
        P.op("pool", lambda e: e.iota(iota_p[:], pattern=[[0, 1]], base=0, channel_multiplier=1,
                                      allow_small_or_imprecise_dtypes=True), writes=[r_const])

        r_mod = Res()
        r_projT = {}
        r_vtok = [Res() for _ in range(NT)]
        r_hres = [Res() for _ in range(NT)]
        r_h2d = [Res() for _ in range(NT + 1)]
        r_slotd = Res()
        r_ysd = Res()

        with ExitStack() as st:
            c_sb = sb(st, "c_sb", [128, 8]); r_c = Res()
            adw = [sb(st, "adw%d" % i, [128, 8, 512]) for i in range(2)]; r_adw = [Res(), Res()]
            modrow = sb(st, "modrow", [1, 6 * D]); r_modrow = Res()
            adb = sb(st, "adb", [1, 6 * D]); r_adb = Res()
            pmod = [ps(st, "pmod%d" % i, [128, 512]) for i in range(2)]; r_pmod = [Res(), Res()]
            P.op("sp", lambda e: e.dma_start(out=c_sb[:], in_=c_in.rearrange("(c p) -> p c", p=128)),
                 writes=[r_c], dma=True)
            P.op("sp", lambda e: e.dma_start(out=adb[:], in_=ada_b.rearrange("(o n) -> o n", o=1)),
                 writes=[r_adb], dma=True)
            P.op("act", lambda e: e.activation(out=c_sb[:], in_=c_sb[:], func=AF.Silu), reads=[r_c], writes=[r_c])
            adw_v = ada_w.rearrange("(c p) n -> p c n", p=128)
            for n in range(12):
                bi = n % 2
                P.op("sp", lambda e, n=n, bi=bi: e.dma_start(out=adw[bi][:], in_=adw_v[:, :, n * 512:(n + 1) * 512]),
                     writes=[r_adw[bi]], dma=True)
                for kc in range(8):
                    P.op("pe", lambda e, bi=bi, kc=kc: e.matmul(pmod[bi][0:1, :], lhsT=c_sb[:, kc:kc + 1],
                                                                rhs=adw[bi][:, kc, :], start=(kc == 0), stop=(kc == 7)),
                         reads=[r_c, r_adw[bi]], writes=[r_pmod[bi]])
                P.op("dve", lambda e, n=n, bi=bi: e.tensor_tensor(out=modrow[0:1, n * 512:(n + 1) * 512],
                                                                   in0=pmod[bi][0:1, :],
                                                                   in1=adb[0:1, n * 512:(n + 1) * 512], op=ALU.add),
                     reads=[r_pmod[bi], r_adb], writes=[r_modrow])
            P.op("sp", lambda e: e.dma_start(out=mod_d.rearrange("(o k) d -> o (k d)", o=1), in_=modrow[:]),
                 reads=[r_modrow], writes=[r_mod], dma=True)
            P.flush()

        stR = ExitStack()
        mask_all = sb(stR, "mask_all", [128, NT, E]); r_maskall = [Res() for _ in range(NT)]
        gw_all = sb(stR, "gw_all", [128, NT, E])
        rank_all = sb(stR, "rank_all", [128, NT, E]); r_rankall = [Res() for _ in range(NT)]
        cummask = sb(stR, "cummask", [128, E]); r_cum = Res()
        cnt_b = sb(stR, "cnt_b", [128, E]); r_cnt = Res()
        with ExitStack() as stA:

            with ExitStack() as st:
                hT = sb(st, "hT", [128, 8, S], BF16)
                r_hT = [Res() for _ in range(NT)]
                a1 = sb(st, "a1", [128, 8]); sh1 = sb(st, "sh1", [128, 8]); n1f = sb(st, "n1f", [128, 8])
                r_a1 = Res()
                A1 = sb(st, "A1", [128, 8, 128]); SH1 = sb(st, "SH1", [128, 8, 128]); r_A1 = Res()
                P.op("sp", lambda e: e.dma_start(out=sh1[:], in_=mod_d[0].rearrange("(c p) -> p c", p=128)),
                     reads=[r_mod], writes=[r_a1], dma=True)
                P.op("sp", lambda e: e.dma_start(out=a1[:], in_=mod_d[1].rearrange("(c p) -> p c", p=128)),
                     reads=[r_mod], writes=[r_a1], dma=True)
                P.op("sp", lambda e: e.dma_start(out=n1f[:], in_=norm1_g.rearrange("(c p) -> p c", p=128)),
                     writes=[r_a1], dma=True)
                P.op("dve", lambda e: e.scalar_tensor_tensor(out=a1[:], in0=a1[:], scalar=1.0, in1=n1f[:],
                                                             op0=ALU.add, op1=ALU.mult), reads=[r_a1], writes=[r_a1])
                P.op("dve", lambda e: e.tensor_copy(out=A1[:], in_=bc(a1[:, :].unsqueeze(2), [128, 8, 128])),
                     reads=[r_a1], writes=[r_A1])
                P.op("dve", lambda e: e.tensor_copy(out=SH1[:], in_=bc(sh1[:, :].unsqueeze(2), [128, 8, 128])),
                     reads=[r_a1], writes=[r_A1])
                NXB = 3
                xt = [sb(st, "xt%d" % i, [128, D]) for i in range(NXB)]; r_xt = [Res() for _ in range(NXB)]
                sq = sb(st, "sq", [128, D], BF16); r_sq = Res()
                ss = sb(st, "ss", [128, NT]); r_ss = [Res() for _ in range(NT)]
                rs = sb(st, "rs", [128, NT])
                xn = [sb(st, "xn%d" % i, [128, D], BF16) for i in range(2)]; r_xn = [Res(), Res()]
                ptr = [ps(st, "ptr%d" % i, [128, 8, 128], BF16) for i in range(2)]; r_ptr = [Res(), Res()]
                tmp = [sb(st, "tmp%d" % i, [128, 8, 128]) for i in range(2)]; r_tmp = [Res(), Res()]
                for i in range(NT):
                    xb = i % NXB
                    b2 = i % 2
                    P.op("sp", lambda e, i=i, xb=xb: e.dma_start(out=xt[xb][:], in_=x[i * 128:(i + 1) * 128, :]),
                         writes=[r_xt[xb]], dma=True)
                    P.op("act", lambda e, i=i, xb=xb: e.activation(out=sq[:], in_=xt[xb][:], func=AF.Square,
                                                                    accum_out=ss[:, i:i + 1]),
                         reads=[r_xt[xb]], writes=[r_sq, r_ss[i]])
                    P.op("act", lambda e, i=i: e.activation(out=rs[:, i:i + 1], in_=ss[:, i:i + 1], func=AF.Sqrt,
                                                            scale=1.0 / D, bias=epsb[:]),
                         reads=[r_ss[i], r_const], writes=[r_ss[i]])
                    P.op("dve", lambda e, i=i: e.reciprocal(out=rs[:, i:i + 1], in_=rs[:, i:i + 1]),
                         reads=[r_ss[i]], writes=[r_ss[i]])
                    P.op("dve", lambda e, i=i, xb=xb, b2=b2: e.tensor_scalar_mul(out=xn[b2][:], in0=xt[xb][:],
                                                                                  scalar1=rs[:, i:i + 1]),
                         reads=[r_xt[xb], r_ss[i]], writes=[r_xn[b2]])
                    for kc in range(8):
                        P.op("pe", lambda e, kc=kc, b2=b2: e.transpose(out=ptr[b2][:, kc, :],
                                                                        in_=xn[b2][:, kc * 128:(kc + 1) * 128],
                                                                        identity=ident_b[:]),
                             reads=[r_xn[b2], r_const], writes=[r_ptr[b2]])
                    P.op("dve", lambda e, b2=b2: e.tensor_tensor(out=tmp[b2][:], in0=ptr[b2][:], in1=A1[:], op=ALU.mult),
                         reads=[r_ptr[b2], r_A1], writes=[r_tmp[b2]])
                    P.op("pool", lambda e, i=i, b2=b2: e.tensor_tensor(out=hT[:, :, i * 128:(i + 1) * 128],
                                                                        in0=tmp[b2][:], in1=SH1[:], op=ALU.add),
                         reads=[r_tmp[b2], r_A1], writes=[r_hT[i]])

                wv = w_in.rearrange("(c p) n -> p c n", p=128)
                wt = [sb(st, "wt%d" % i, [128, 8, 512], BF16) for i in range(2)]; r_wt = [Res(), Res()]
                stg = [sb(st, "stg%d" % i, [128, S]) for i in range(2)]; r_stg = [Res(), Res()]
                vst = [sb(st, "vst%d" % i, [128, 512], BF16) for i in range(2)]; r_vst = [Res(), Res()]
                pp = [ps(st, "pp%d" % i, [128, 512]) for i in range(4)]; r_pp = [Res() for _ in range(4)]
                ppi = 0
                sgi = 0
                for g in range(11):
                    bi = g % 2
                    P.op("pool", lambda e, g=g, bi=bi: e.dma_start(out=wt[bi][:], in_=wv[:, :, g * 512:(g + 1) * 512]),
                         writes=[r_wt[bi]], dma=True)
                    if g == 3:
                        for i in range(NT):
                            pi = ppi % 4; ppi += 1
                            for kc in range(8):
                                P.op("pe", lambda e, i=i, kc=kc, pi=pi, bi=bi: e.matmul(
                                    pp[pi][:], lhsT=hT[:, kc, i * 128:(i + 1) * 128], rhs=wt[bi][:, kc, :],
                                    start=(kc == 0), stop=(kc == 7)),
                                    reads=[r_hT[i], r_wt[bi]], writes=[r_pp[pi]])
                            vb = i % 2
                            P.op("act", lambda e, pi=pi, vb=vb: e.copy(out=vst[vb][:], in_=pp[pi][:]),
                                 reads=[r_pp[pi]], writes=[r_vst[vb]])
                            P.op("sp", lambda e, i=i, vb=vb: e.dma_start(out=vtok_d[i * 128:(i + 1) * 128, :],
                                                                         in_=vst[vb][:]),
                                 reads=[r_vst[vb]], writes=[r_vtok[i]], dma=True)
                        continue
                    for mm in range(4):
                        sg = sgi % 2; sgi += 1
                        for tc in range(NCH):
                            pi = ppi % 4; ppi += 1
                            for kc in range(8):
                                P.op("pe", lambda e, mm=mm, tc=tc, kc=kc, pi=pi, bi=bi: e.matmul(
                                    pp[pi][:], lhsT=wt[bi][:, kc, mm * 128:(mm + 1) * 128],
                                    rhs=hT[:, kc, tc * 512:(tc + 1) * 512], start=(kc == 0), stop=(kc == 7)),
                                    reads=[r_wt[bi]] + r_hT[tc * 4:(tc + 1) * 4], writes=[r_pp[pi]])
                            if tc % 2 == 0:
                                P.op("act", lambda e, tc=tc, pi=pi, sg=sg: e.copy(
                                    out=stg[sg][:, tc * 512:(tc + 1) * 512], in_=pp[pi][:]),
                                    reads=[r_pp[pi]], writes=[r_stg[sg]])
                            else:
                                P.op("dve", lambda e, tc=tc, pi=pi, sg=sg: e.tensor_copy(
                                    out=stg[sg][:, tc * 512:(tc + 1) * 512], in_=pp[pi][:]),
                                    reads=[r_pp[pi]], writes=[r_stg[sg]])
                        row0 = g * 512 + mm * 128
                        r_projT[row0] = Res()
                        P.op("sp", lambda e, row0=row0, sg=sg: e.dma_start(out=projT_d[row0:row0 + 128, :],
                                                                           in_=stg[sg][:]),
                             reads=[r_stg[sg]], writes=[r_projT[row0]], dma=True)
                P.flush()

            hgT = sb(stA, "hgT", [128, 4, S], BF16)
            r_hgT = [Res() for _ in range(4)]
            with ExitStack() as st:
                lbt = sb(st, "lbt", [128, 2, 2, 4]); r_lb = Res()
                lbv = sb(st, "lbv", [128, 2, 4]); oml = sb(st, "oml", [128, 2, 4])
                ngf = sb(st, "ngf", [128, 4])
                P.op("sp", lambda e: e.dma_start(out=lbt[:, 0], in_=lb_table[0].rearrange("d (h p) -> p d h", p=128)),
                     writes=[r_lb], dma=True)
                P.op("sp", lambda e: e.dma_start(out=lbt[:, 1], in_=lb_table[1].rearrange("d (h p) -> p d h", p=128)),
                     writes=[r_lb], dma=True)
                P.op("sp", lambda e: e.dma_start(out=ngf[:], in_=hg_norm_g.rearrange("h p -> p h")),
                     writes=[r_lb], dma=True)
                P.op("dve", lambda e: e.tensor_tensor(out=lbv[:], in0=lbt[:, 0], in1=lbt[:, 1], op=ALU.subtract),
                     reads=[r_lb], writes=[r_lb])
                P.op("act", lambda e: e.activation(out=lbv[:], in_=lbv[:], func=AF.Sigmoid), reads=[r_lb], writes=[r_lb])
                P.op("dve", lambda e: e.tensor_scalar(out=oml[:], in0=lbv[:], scalar1=-1.0, scalar2=1.0,
                                                       op0=ALU.mult, op1=ALU.add), reads=[r_lb], writes=[r_lb])
                H = 2048
                ones_h = sb(st, "ones_h", [128, H]); r_onesh = Res()
                P.op("pool", lambda e: e.memset(ones_h[:], 1.0), writes=[r_onesh])
                mask_f = sb(st, "mask_f", [128, 128]); mask_b = sb(st, "mask_b", [128, 128]); r_mask = Res()
                for mk, sgn in ((mask_f, 1), (mask_b, -1)):
                    P.op("pool", lambda e, mk=mk: e.memset(mk[:], 1.0), writes=[r_mask])
                    P.op("pool", lambda e, mk=mk, sgn=sgn: e.affine_select(
                        out=mk[:], in_=mk[:], pattern=[[sgn, 128]], compare_op=ALU.is_ge, fill=0.0, base=0,
                        channel_multiplier=-sgn), reads=[r_mask], writes=[r_mask])
                    P.op("pool", lambda e, mk=mk: e.memset(mk[0:64, 64:128], 0.0), reads=[r_mask], writes=[r_mask])
                    P.op("pool", lambda e, mk=mk: e.memset(mk[64:128, 0:64], 0.0), reads=[r_mask], writes=[r_mask])

                T1 = sb(st, "T1", [128, H]); T2 = sb(st, "T2", [128, H]); T3 = sb(st, "T3", [128, H])
                TQ = sb(st, "TQ", [128, H])
                r_T1, r_T2, r_T3, r_TQ = Res(), Res(), Res(), Res()
                Bext = sb(st, "Bext", [128, S + 1]); r_B = Res()
                qt = [sb(st, "qt%d" % d, [128, S], BF16) for d in range(2)]
                kt = [sb(st, "kt%d" % d, [128, S], BF16) for d in range(2)]
                ktok = [sb(st, "ktok%d" % d, [128, NT, 128], BF16) for d in range(2)]
                dec = [sb(st, "dec%d" % d, [128, 64]) for d in range(2)]
                r_qk = [Res(), Res()]
                r_ktok = [Res(), Res()]
                vtok = sb(st, "vtok", [128, NT, 128], BF16); r_vt = Res()
                o_h = sb(st, "o_h", [128, S]); r_oh = [Res() for _ in range(NT)]
                Sf = [sb(st, "Sf%d" % d, [128, 128]) for d in range(2)]
                Sb = [sb(st, "Sb%d" % d, [128, 128], BF16) for d in range(2)]
                Stmp = [sb(st, "Stmp%d" % d, [128, 128]) for d in range(2)]
                r_S = [Res(), Res()]
                r_Sf = [Res(), Res()]
                r_Stmp = [Res(), Res()]
                sT = [sb(st, "sT%d" % d, [128, 128], BF16) for d in range(2)]; r_sT = [Res(), Res()]
                p_sc = [ps(st, "p_sc%d" % d, [128, 512])[:, 0:128] for d in range(2)]; r_psc = [Res(), Res()]
                p_o = [ps(st, "p_o%d" % d, [128, 512])[:, 0:128] for d in range(2)]; r_po = [Res(), Res()]
                p_P = [ps(st, "p_P%d" % d, [128, 512])[:, 0:128] for d in range(2)]; r_pP = [Res(), Res()]
                p_kt = ps(st, "p_kt", [128, 8, 128], BF16); r_pkt = Res()
                p_st = ps(st, "p_st", [128, 512]); r_pst = Res()

                for h in range(4):
                    P.op("sp", lambda e, h=h: e.dma_start(
                        out=vtok[:], in_=vtok_d[:, h * 128:(h + 1) * 128].rearrange("(i p) v -> p i v", p=128)),
                        reads=r_vtok, writes=[r_vt], dma=True)
                    P.op("pool", lambda e: e.memset(o_h[:], 0.0), writes=r_oh)
                    for d in range(2):
                        zrow = 512 + d * 512 + h * 128
                        B3 = Bext[:, 1:S + 1].rearrange("p (c j) -> p c j", j=64)
                        B0 = Bext[:, 0:S].rearrange("p (c j) -> p c j", j=64)
                        P.op("pool", lambda e: e.memset(Bext[:, 0:1], 0.0), writes=[r_B])
                        for hf in range(2):
                            c0 = hf * H
                            P.op("sp", lambda e, zrow=zrow, c0=c0: e.dma_start(
                                out=T1[:], in_=projT_d[zrow:zrow + 128, c0:c0 + H]),
                                reads=[r_projT[zrow]], writes=[r_T1], dma=True)
                            P.op("act", lambda e: e.activation(out=T1[:], in_=T1[:], func=AF.Sigmoid),
                                 reads=[r_T1], writes=[r_T1])
                            P.op("dve", lambda e, d=d, h=h: e.tensor_scalar(
                                out=T1[:], in0=T1[:], scalar1=oml[:, d, h:h + 1], scalar2=lbv[:, d, h:h + 1],
                                op0=ALU.mult, op1=ALU.add), reads=[r_T1, r_lb], writes=[r_T1])
                            P.op("act", lambda e: e.activation(out=T2[:], in_=T1[:], func=AF.Ln),
                                 reads=[r_T1], writes=[r_T2])
                            P.op("dve", lambda e, c0=c0: e.tensor_tensor_scan(
                                out=Bext[:, 1 + c0:1 + c0 + H], data0=ones_h[:], data1=T2[:],
                                initial=Bext[:, c0:c0 + 1], op0=ALU.mult, op1=ALU.add),
                                reads=[r_T2, r_onesh, r_B], writes=[r_B])
                            P.op("pool", lambda e: e.tensor_scalar(out=T1[:], in0=T1[:], scalar1=-1.0, scalar2=1.0,
                                                                   op0=ALU.mult, op1=ALU.add),
                                 reads=[r_T1], writes=[r_T1])
                            P.op("act", lambda e, d=d, c0=c0: e.copy(out=kt[d][:, c0:c0 + H], in_=T1[:]),
                                 reads=[r_T1], writes=[r_qk[d]])
                        for hf in range(2):
                            c0 = hf * H
                            cs = slice(hf * 32, (hf + 1) * 32)
                            T2v = T2[:].rearrange("p (c j) -> p c j", j=64)
                            if d == 0:
                                P.op("dve", lambda e, cs=cs: e.tensor_tensor(
                                    out=T2v, in0=B3[:, cs, :], in1=bc(B0[:, cs, 0:1], [128, 32, 64]),
                                    op=ALU.subtract), reads=[r_B], writes=[r_T2])
                            else:
                                P.op("dve", lambda e, cs=cs: e.tensor_tensor(
                                    out=T2v, in0=bc(B3[:, cs, 63:64], [128, 32, 64]), in1=B0[:, cs, :],
                                    op=ALU.subtract), reads=[r_B], writes=[r_T2])
                            P.op("act", lambda e: e.activation(out=T3[:], in_=T2[:], func=AF.Exp),
                                 reads=[r_T2], writes=[r_T3])
                            T3v = T3[:].rearrange("p (c j) -> p c j", j=64)
                            jj = 63 if d == 0 else 0
                            P.op("pool", lambda e, d=d, cs=cs, jj=jj: e.tensor_copy(
                                out=dec[d][:, cs].unsqueeze(2), in_=T3v[:, :, jj:jj + 1]),
                                reads=[r_T3], writes=[r_qk[d]])
                            qrow = h * 128
                            P.op("sp", lambda e, qrow=qrow, c0=c0: e.dma_start(
                                out=TQ[:], in_=projT_d[qrow:qrow + 128, c0:c0 + H]),
                                reads=[r_projT[qrow]], writes=[r_TQ], dma=True)
                            P.op("dve", lambda e, d=d, c0=c0: e.tensor_tensor(
                                out=qt[d][:, c0:c0 + H], in0=TQ[:], in1=T3[:], op=ALU.mult),
                                reads=[r_TQ, r_T3], writes=[r_qk[d]])
                            P.op("act", lambda e: e.activation(out=T3[:], in_=T2[:], func=AF.Exp, scale=-1.0),
                                 reads=[r_T2], writes=[r_T3])
                            P.op("pool", lambda e, d=d, c0=c0: e.tensor_tensor(
                                out=kt[d][:, c0:c0 + H], in0=kt[d][:, c0:c0 + H], in1=T3[:], op=ALU.mult),
                                reads=[r_T3, r_qk[d]], writes=[r_qk[d]])
                        for g8 in range(4):
                            for j in range(8):
                                i = g8 * 8 + j
                                P.op("pe", lambda e, d=d, i=i, j=j: e.transpose(
                                    out=p_kt[:, j, :], in_=kt[d][:, i * 128:(i + 1) * 128], identity=ident_b[:]),
                                    reads=[r_qk[d], r_const], writes=[r_pkt])
                            P.op("act", lambda e, d=d, g8=g8: e.copy(out=ktok[d][:, g8 * 8:(g8 + 1) * 8, :],
                                                                     in_=p_kt[:]),
                                 reads=[r_pkt], writes=[r_ktok[d]])
                        P.op("pool", lambda e, d=d: e.memset(Sf[d][:], 0.0), writes=[r_Sf[d]])
                        P.op("pool", lambda e, d=d: e.memset(Sb[d][:], 0.0), writes=[r_S[d]])

                    for step in range(NT):
                        for d in range(2):
                            i = step if d == 0 else NT - 1 - step
                            t0 = i * 128
                            mk = mask_f if d == 0 else mask_b
                            P.op("pe", lambda e, d=d, t0=t0: e.matmul(
                                p_sc[d][:], lhsT=kt[d][:, t0:t0 + 128], rhs=qt[d][:, t0:t0 + 128],
                                start=True, stop=True), reads=[r_qk[d]], writes=[r_psc[d]])
                            P.op("dve", lambda e, d=d, mk=mk: e.tensor_tensor(
                                out=sT[d][:], in0=p_sc[d][:], in1=mk[:], op=ALU.mult),
                                reads=[r_psc[d], r_mask], writes=[r_sT[d]])
                            P.op("pe", lambda e, d=d, i=i: e.matmul(
                                p_o[d][:], lhsT=vtok[:, i, :], rhs=sT[d][:], start=True, stop=False),
                                reads=[r_vt, r_sT[d]], writes=[r_po[d]])
                            order = (0, 1) if d == 0 else (1, 0)
                            for n_, half in enumerate(order):
                                c = i * 2 + half
                                hs = slice(half * 64, half * 64 + 64)
                                P.op("pe", lambda e, d=d, t0=t0, hs=hs, n_=n_: e.matmul(
                                    p_o[d][:, hs], lhsT=Sb[d][:], rhs=qt[d][:, t0 + hs.start:t0 + hs.stop],
                                    start=False, stop=(n_ == 1)),
                                    reads=[r_S[d], r_qk[d]], writes=[r_po[d]])
                                P.op("pe", lambda e, d=d, i=i, hs=hs: e.matmul(
                                    p_P[d][:], lhsT=ktok[d][hs, i, :], rhs=vtok[hs, i, :], start=True, stop=True),
                                    reads=[r_ktok[d], r_vt], writes=[r_pP[d]])
                                P.op("dve", lambda e, d=d: e.tensor_tensor(
                                    out=Stmp[d][:], in0=p_P[d][:], in1=Sf[d][:], op=ALU.add),
                                    reads=[r_pP[d], r_Sf[d]], writes=[r_Stmp[d]])
                                P.op("act", lambda e, d=d, c=c: e.activation(
                                    out=Sb[d][:], in_=Stmp[d][:], func=AF.Copy, scale=dec[d][:, c:c + 1]),
                                    reads=[r_Stmp[d], r_qk[d]], writes=[r_S[d]])
                                P.op("pool", lambda e, d=d, c=c: e.tensor_scalar_mul(
                                    out=Sf[d][:], in0=Stmp[d][:], scalar1=dec[d][:, c:c + 1]),
                                    reads=[r_Stmp[d], r_qk[d]], writes=[r_Sf[d]])
                            P.op("dve", lambda e, d=d, t0=t0: e.tensor_tensor(
                                out=o_h[:, t0:t0 + 128], in0=p_o[d][:], in1=o_h[:, t0:t0 + 128], op=ALU.add),
                                reads=[r_po[d], r_oh[i]], writes=[r_oh[i]])

                    for tc in range(NCH):
                        cs = slice(tc * 512, (tc + 1) * 512)
                        w0 = (tc % 4) * 512
                        P.op("act", lambda e, cs=cs, w0=w0: e.activation(out=T1[:, w0:w0 + 512], in_=o_h[:, cs],
                                                                         func=AF.Square),
                             reads=r_oh[tc * 4:(tc + 1) * 4], writes=[r_T1])
                        P.op("pe", lambda e, w0=w0: e.matmul(p_st[:], lhsT=ones_f[:], rhs=T1[:, w0:w0 + 512],
                                                            start=True, stop=True),
                             reads=[r_T1, r_ones], writes=[r_pst])
                        P.op("act", lambda e, w0=w0: e.activation(out=T2[:, w0:w0 + 512], in_=p_st[:], func=AF.Sqrt,
                                                                  scale=1.0 / 128, bias=epsb[:]),
                             reads=[r_pst, r_const], writes=[r_T2])
                        P.op("dve", lambda e, w0=w0: e.reciprocal(out=T2[:, w0:w0 + 512], in_=T2[:, w0:w0 + 512]),
                             reads=[r_T2], writes=[r_T2])
                        P.op("dve", lambda e, cs=cs, w0=w0: e.tensor_tensor(
                            out=T2[:, w0:w0 + 512], in0=T2[:, w0:w0 + 512], in1=o_h[:, cs], op=ALU.mult),
                            reads=[r_T2] + r_oh[tc * 4:(tc + 1) * 4], writes=[r_T2])
                        grow = 2048 + h * 128
                        P.op("sp", lambda e, grow=grow, cs=cs, w0=w0: e.dma_start(
                            out=T3[:, w0:w0 + 512], in_=projT_d[grow:grow + 128, cs]),
                            reads=[r_projT[grow]], writes=[r_T3], dma=True)
                        P.op("act", lambda e, w0=w0: e.activation(out=T3[:, w0:w0 + 512], in_=T3[:, w0:w0 + 512],
                                                                  func=AF.Silu), reads=[r_T3], writes=[r_T3])
                        P.op("dve", lambda e, h=h, cs=cs, w0=w0: e.scalar_tensor_tensor(
                            out=hgT[:, h, cs], in0=T2[:, w0:w0 + 512], scalar=ngf[:, h:h + 1],
                            in1=T3[:, w0:w0 + 512], op0=ALU.mult, op1=ALU.mult),
                            reads=[r_T2, r_T3, r_lb], writes=[r_hgT[h]])
                P.flush()

            cvT = sb(stA, "cvT", [128, 4, S], BF16)
            r_cvT = [Res() for _ in range(4)]
            with ExitStack() as st:
                dww = sb(st, "dww", [128, 4, 31]); dwb = sb(st, "dwb", [128, 4])
                lng = sb(st, "lng", [128, 4]); lnb = sb(st, "lnb", [128, 4]); r_cp = Res()
                for cc in range(4):
                    P.op("sp", lambda e, cc=cc: e.dma_start(out=dww[:, cc, :],
                                                            in_=dw_w[:, cc * 128:(cc + 1) * 128].rearrange("j p -> p j")),
                         writes=[r_cp], dma=True)
                for t_, src_ in ((dwb, dw_b), (lng, cv_ln_g), (lnb, cv_ln_b)):
                    P.op("sp", lambda e, t_=t_, src_=src_: e.dma_start(out=t_[:], in_=src_.rearrange("(c p) -> p c", p=128)),
                         writes=[r_cp], dma=True)
                ub = sb(st, "ub", [128, 4, S + 30], BF16); r_ub = [Res() for _ in range(4)]
                for cc in range(4):
                    P.op("pool", lambda e, cc=cc: e.memset(ub[:, cc, 0:15], 0.0), writes=[r_ub[cc]])
                    P.op("pool", lambda e, cc=cc: e.memset(ub[:, cc, S + 15:S + 30], 0.0), writes=[r_ub[cc]])
                HH = 2048
                with ExitStack() as st2:
                    vt = [sb(st2, "vt%d" % i, [128, HH]) for i in range(2)]; r_vt4 = [Res(), Res()]
                    gt = [sb(st2, "gt%d" % i, [128, HH]) for i in range(2)]; r_gt4 = [Res(), Res()]
                    n_it = 0
                    for cc in range(4):
                        vrow = 2560 + cc * 128
                        grow = 3072 + cc * 128
                        for hf in range(2):
                            b2 = n_it % 2; n_it += 1
                            c0 = hf * HH
                            P.op("sp", lambda e, vrow=vrow, c0=c0, b2=b2: e.dma_start(
                                out=vt[b2][:], in_=projT_d[vrow:vrow + 128, c0:c0 + HH]),
                                reads=[r_projT[vrow]], writes=[r_vt4[b2]], dma=True)
                            P.op("sp", lambda e, grow=grow, c0=c0, b2=b2: e.dma_start(
                                out=gt[b2][:], in_=projT_d[grow:grow + 128, c0:c0 + HH]),
                                reads=[r_projT[grow]], writes=[r_gt4[b2]], dma=True)
                            P.op("act", lambda e, b2=b2: e.activation(out=gt[b2][:], in_=gt[b2][:], func=AF.Sigmoid),
                                 reads=[r_gt4[b2]], writes=[r_gt4[b2]])
                            P.op("dve", lambda e, cc=cc, c0=c0, b2=b2: e.tensor_tensor(
                                out=ub[:, cc, 15 + c0:15 + c0 + HH], in0=vt[b2][:], in1=gt[b2][:], op=ALU.mult),
                                reads=[r_vt4[b2], r_gt4[b2]], writes=[r_ub[cc]])
                    P.flush()
                dg = sb(st, "dg", [128, 4, 31, 128], BF16); r_dg = [Res() for _ in range(4)]
                n_it = 0
                for cc in range(4):
                    for j in range(31):
                        if n_it % 2 == 0:
                            P.op("pool", lambda e, cc=cc, j=j: e.tensor_scalar_mul(
                                out=dg[:, cc, j, :], in0=ident_f[:], scalar1=dww[:, cc, j:j + 1]),
                                reads=[r_ident, r_cp], writes=[r_dg[cc]])
                        else:
                            P.op("act", lambda e, cc=cc, j=j: e.activation(
                                out=dg[:, cc, j, :], in_=ident_f[:], func=AF.Copy, scale=dww[:, cc, j:j + 1]),
                                reads=[r_ident, r_cp], writes=[r_dg[cc]])
                        n_it += 1
                ones_b = sb(st, "ones_b", [128, 128], BF16); r_ob = Res()
                P.op("dve", lambda e: e.tensor_copy(out=ones_b[:], in_=ones_f[:]), reads=[r_ones], writes=[r_ob])
                uc = [sb(st, "uc%d" % i, [128, 4, 512], BF16) for i in range(2)]; r_uc = [[Res() for _ in range(4)] for _ in range(2)]
                usq = sb(st, "usq", [128, 4, 512], BF16); r_usq = Res()
                p_cv = [ps(st, "p_cv%d" % i, [128, 512]) for i in range(4)]; r_pcv = [Res() for _ in range(4)]
                p_s1 = ps(st, "p_s1", [128, 512]); p_s2 = ps(st, "p_s2", [128, 512]); r_ps1 = Res(); r_ps2 = Res()
                mean = sb(st, "mean", [128, 512]); msq = sb(st, "msq", [128, 512]); rstd = sb(st, "rstd", [128, 512])
                r_mean = Res(); r_rstd = Res()
                tt = [sb(st, "tt%d" % i, [128, 512]) for i in range(2)]; r_tt = [Res(), Res()]
                for tc in range(NCH):
                    cs = slice(tc * 512, (tc + 1) * 512)
                    ub_ = tc % 2
                    for cc in range(4):
                        for j in range(31):
                            P.op("pe", lambda e, cc=cc, j=j, tc=tc: e.matmul(
                                p_cv[cc][:], lhsT=dg[:, cc, j, :], rhs=ub[:, cc, tc * 512 + j:tc * 512 + j + 512],
                                start=(j == 0), stop=(j == 30)), reads=[r_dg[cc], r_ub[cc]], writes=[r_pcv[cc]])
                        P.op("act", lambda e, cc=cc, ub_=ub_: e.activation(
                            out=uc[ub_][:, cc, :], in_=p_cv[cc][:], func=AF.Identity, bias=dwb[:, cc:cc + 1]),
                            reads=[r_pcv[cc], r_cp], writes=[r_uc[ub_][cc]])
                    for cc in range(4):
                        P.op("act", lambda e, cc=cc, ub_=ub_: e.activation(out=usq[:, cc, :], in_=uc[ub_][:, cc, :],
                                                                          func=AF.Square),
                             reads=[r_uc[ub_][cc]], writes=[r_usq])
                    for cc in range(4):
                        P.op("pe", lambda e, cc=cc, ub_=ub_: e.matmul(p_s1[:], lhsT=ones_b[:], rhs=uc[ub_][:, cc, :],
                                                                      start=(cc == 0), stop=(cc == 3)),
                             reads=[r_uc[ub_][cc], r_ob], writes=[r_ps1])
                    for cc in range(4):
                        P.op("pe", lambda e, cc=cc: e.matmul(p_s2[:], lhsT=ones_b[:], rhs=usq[:, cc, :],
                                                             start=(cc == 0), stop=(cc == 3)),
                             reads=[r_usq, r_ob], writes=[r_ps2])
                    P.op("act", lambda e: e.activation(out=mean[:], in_=p_s1[:], func=AF.Copy, scale=1.0 / 512),
                         reads=[r_ps1], writes=[r_mean])
                    P.op("dve", lambda e: e.tensor_tensor(out=msq[:], in0=mean[:], in1=mean[:], op=ALU.mult),
                         reads=[r_mean], writes=[r_rstd])
                    P.op("dve", lambda e: e.scalar_tensor_tensor(out=rstd[:], in0=p_s2[:], scalar=1.0 / 512, in1=msq[:],
                                                                 op0=ALU.mult, op1=ALU.subtract),
                         reads=[r_ps2, r_rstd], writes=[r_rstd])
                    P.op("dve", lambda e: e.tensor_scalar_max(out=rstd[:], in0=rstd[:], scalar1=0.0),
                         reads=[r_rstd], writes=[r_rstd])
                    P.op("act", lambda e: e.activation(out=rstd[:], in_=rstd[:], func=AF.Sqrt, bias=epsb[:]),
                         reads=[r_rstd, r_const], writes=[r_rstd])
                    P.op("dve", lambda e: e.reciprocal(out=rstd[:], in_=rstd[:]), reads=[r_rstd], writes=[r_rstd])
                    for cc in range(4):
                        b2 = cc % 2
                        P.op("dve", lambda e, cc=cc, ub_=ub_, b2=b2: e.tensor_tensor(
                            out=tt[b2][:], in0=uc[ub_][:, cc, :], in1=mean[:], op=ALU.subtract),
                            reads=[r_uc[ub_][cc], r_mean], writes=[r_tt[b2]])
                        P.op("pool", lambda e, b2=b2: e.tensor_tensor(out=tt[b2][:], in0=tt[b2][:], in1=rstd[:],
                                                                     op=ALU.mult),
                             reads=[r_tt[b2], r_rstd], writes=[r_tt[b2]])
                        P.op("act", lambda e, cc=cc, cs=cs, b2=b2: e.activation(
                            out=cvT[:, cc, cs], in_=tt[b2][:], func=AF.Silu, scale=lng[:, cc:cc + 1],
                            bias=lnb[:, cc:cc + 1]), reads=[r_tt[b2], r_cp], writes=[r_cvT[cc]])
                P.flush()

            with ExitStack() as st:
                wohg = sb(st, "wohg", [128, 4, D], BF16); wocv = sb(st, "wocv", [128, 4, D], BF16)
                wo = sb(st, "wo", [128, 8, D], BF16); r_w5 = Res()
                rw = sb(st, "rw", [128, 8, E]); rbb = sb(st, "rbb", [128, E]); bocv = sb(st, "bocv", [128, 8])
                g1b = sb(st, "g1b", [128, D]); a2b = sb(st, "a2b", [128, D]); sh2b = sb(st, "sh2b", [128, D])
                r_bt = Res()
                P.op("pool", lambda e: e.dma_start(out=wohg[:], in_=w_o_hg.rearrange("(c p) n -> p c n", p=128)),
                     writes=[r_w5], dma=True)
                P.op("pool", lambda e: e.dma_start(out=wocv[:], in_=w_o_cv.rearrange("(c p) n -> p c n", p=128)),
                     writes=[r_w5], dma=True)
                P.op("pool", lambda e: e.dma_start(out=wo[:], in_=w_out.rearrange("(c p) n -> p c n", p=128)),
                     writes=[r_w5], dma=True)
                P.op("sp", lambda e: e.dma_start(out=rw[:], in_=router_w.rearrange("(c p) n -> p c n", p=128)),
                     writes=[r_w5], dma=True)
                P.op("sp", lambda e: e.dma_start(out=rbb[:], in_=router_b.partition_broadcast(128)),
                     writes=[r_w5], dma=True)
                P.op("sp", lambda e: e.dma_start(out=bocv[:], in_=b_o_cv.rearrange("(c p) -> p c", p=128)),
                     writes=[r_w5], dma=True)
                P.op("sp", lambda e: e.dma_start(out=g1b[:], in_=mod_d[2, :].partition_broadcast(128)),
                     reads=[r_mod], writes=[r_bt], dma=True)
                P.op("sp", lambda e: e.dma_start(out=sh2b[:], in_=mod_d[3, :].partition_broadcast(128)),
                     reads=[r_mod], writes=[r_bt], dma=True)
                P.op("sp", lambda e: e.dma_start(out=a2b[:], in_=mod_d[4, :].partition_broadcast(128)),
                     reads=[r_mod], writes=[r_bt], dma=True)
                P.op("sp", lambda e: e.dma_start(out=g2b[:], in_=mod_d[5, :].partition_broadcast(128)),
                     reads=[r_mod], writes=[r_g2b], dma=True)
                P.op("sp", lambda e: e.dma_start(out=fgb[:], in_=fin_g.partition_broadcast(128)),
                     writes=[r_fgb], dma=True)

                gh = [sb(st, "gh%d" % i, [128, 512]) for i in range(2)]; r_gh = [Res(), Res()]
                gc = [sb(st, "gc%d" % i, [128, 512]) for i in range(2)]; r_gc = [Res(), Res()]
                mT = sb(st, "mT", [128, 8, 512], BF16); r_mT = Res()
                m1 = [sb(st, "m1_%d" % i, [128, 512]) for i in range(2)]
                m2 = [sb(st, "m2_%d" % i, [128, 512]) for i in range(2)]
                r_m1 = [Res(), Res()]; r_m2 = [Res(), Res()]
                p_yh = [ps(st, "p_yh%d" % i, [128, 512]) for i in range(2)]; r_pyh = [Res(), Res()]
                p_yc = [ps(st, "p_yc%d" % i, [128, 512]) for i in range(2)]; r_pyc = [Res(), Res()]
                p_o5 = [ps(st, "p_o5%d" % i, [128, 512]) for i in range(2)]; r_po5 = [Res(), Res()]
                p_tr = ps(st, "p_tr", [128, 8, 128], BF16); r_ptr5 = Res()
                p_lg = ps(st, "p_lg", [128, 16, E]); r_plg = Res()
                xr = [sb(st, "xr%d" % i, [128, D]) for i in range(2)]; r_xr = [Res(), Res()]
                hr = [sb(st, "hr%d" % i, [128, D]) for i in range(2)]; r_hr = [Res(), Res()]
                h2f = [sb(st, "h2f%d" % i, [128, D]) for i in range(2)]; r_h2f = [Res(), Res()]
                P.op("sp", lambda e: e.dma_start(out=h2f[0][:], in_=norm2_g.partition_broadcast(128)),
                     writes=[r_h2f[0]], dma=True)
                P.op("dve", lambda e: e.scalar_tensor_tensor(out=a2b[:], in0=a2b[:], scalar=1.0, in1=h2f[0][:],
                                                             op0=ALU.add, op1=ALU.mult), reads=[r_bt, r_h2f[0]], writes=[r_bt])
                h2b = [sb(st, "h2b%d" % i, [128, D], BF16) for i in range(2)]; r_h2b = [Res(), Res()]
                junk = sb(st, "junk", [128, D], BF16); r_junk = Res()
                ss2 = sb(st, "ss2", [128, NT]); r_ss2 = [Res() for _ in range(NT)]
                h2Th = sb(st, "h2Th", [128, 8, 128], BF16); r_h2Th = Res()
                h2Tl = sb(st, "h2Tl", [128, 8, 128], BF16); r_h2Tl = Res()
                h2l = [sb(st, "h2l%d" % i, [128, D], BF16) for i in range(2)]; r_h2l = [Res(), Res()]
                rwh = sb(st, "rwh", [128, 8, E], BF16); rwl = sb(st, "rwl", [128, 8, E], BF16)
                P.op("dve", lambda e: e.tensor_copy(out=rwh[:], in_=rw[:]), reads=[r_w5], writes=[r_w5])
                P.op("dve", lambda e: e.tensor_tensor(out=rwl[:], in0=rw[:], in1=rwh[:], op=ALU.subtract),
                     reads=[r_w5], writes=[r_w5])
                lg_all = sb(st, "lg_all", [128, NT, E]); r_lgall = [Res() for _ in range(NT)]
                m8a = sb(st, "m8a", [128, NT, 8]); r_m8a = Res()
                den_all = sb(st, "den_all", [128, NT]); r_den = Res(); r_gwall = Res()
                P.op("pool", lambda e: e.memset(junk[:], 0.0), writes=[r_junk])
                P.op("sp", lambda e: e.dma_start(out=h2_d[S:S + 128, :], in_=junk[:]), reads=[r_junk], writes=[r_h2d[NT]],
                     dma=True)
                for tc in range(NCH):
                    cs = slice(tc * 512, (tc + 1) * 512)
                    for dch in range(8):
                        b2 = dch % 2
                        rh = 3584 + dch * 128
                        rc = 4608 + dch * 128
                        P.op("sp", lambda e, cs=cs, rh=rh, b2=b2: e.dma_start(out=gh[b2][:], in_=projT_d[rh:rh + 128, cs]),
                             reads=[r_projT[rh]], writes=[r_gh[b2]], dma=True)
                        P.op("sp", lambda e, cs=cs, rc=rc, b2=b2: e.dma_start(out=gc[b2][:], in_=projT_d[rc:rc + 128, cs]),
                             reads=[r_projT[rc]], writes=[r_gc[b2]], dma=True)
                        P.op("act", lambda e, b2=b2: e.activation(out=gh[b2][:], in_=gh[b2][:], func=AF.Sigmoid),
                             reads=[r_gh[b2]], writes=[r_gh[b2]])
                        P.op("act", lambda e, b2=b2: e.activation(out=gc[b2][:], in_=gc[b2][:], func=AF.Sigmoid),
                             reads=[r_gc[b2]], writes=[r_gc[b2]])
                        for kc in range(4):
                            P.op("pe", lambda e, dch=dch, kc=kc, b2=b2, cs=cs: e.matmul(
                                p_yh[b2][:], lhsT=wohg[:, kc, dch * 128:(dch + 1) * 128], rhs=hgT[:, kc, cs],
                                start=(kc == 0), stop=(kc == 3)), reads=[r_w5, r_hgT[kc]], writes=[r_pyh[b2]])
                        for kc in range(4):
                            P.op("pe", lambda e, dch=dch, kc=kc, b2=b2, cs=cs: e.matmul(
                                p_yc[b2][:], lhsT=wocv[:, kc, dch * 128:(dch + 1) * 128], rhs=cvT[:, kc, cs],
                                start=(kc == 0), stop=(kc == 3)), reads=[r_w5, r_cvT[kc]], writes=[r_pyc[b2]])
                        P.op("dve", lambda e, dch=dch, b2=b2: e.tensor_tensor(
                            out=m1[b2][:], in0=p_yh[b2][:], in1=gh[b2][:], op=ALU.mult),
                            reads=[r_pyh[b2], r_gh[b2]], writes=[r_m1[b2]])
                        P.op("dve", lambda e, dch=dch, b2=b2: e.scalar_tensor_tensor(
                            out=m2[b2][:], in0=p_yc[b2][:], scalar=bocv[:, dch:dch + 1], in1=gc[b2][:],
                            op0=ALU.add, op1=ALU.mult), reads=[r_pyc[b2], r_gc[b2], r_w5], writes=[r_m2[b2]])
                        P.op("pool", lambda e, dch=dch, b2=b2: e.tensor_tensor(
                            out=mT[:, dch, :], in0=m1[b2][:], in1=m2[b2][:], op=ALU.add),
                            reads=[r_m1[b2], r_m2[b2]], writes=[r_mT])
                    for q in range(4):
                        i = tc * 4 + q
                        b2 = i % 2
                        P.op("sp", lambda e, i=i, b2=b2: e.dma_start(out=xr[b2][:], in_=x[i * 128:(i + 1) * 128, :]),
                             writes=[r_xr[b2]], dma=True)
                        for dh in range(2):
                            for kc in range(8):
                                P.op("pe", lambda e, q=q, dh=dh, kc=kc: e.matmul(
                                    p_o5[dh][:], lhsT=mT[:, kc, q * 128:(q + 1) * 128],
                                    rhs=wo[:, kc, dh * 512:(dh + 1) * 512], start=(kc == 0), stop=(kc == 7)),
                                    reads=[r_mT, r_w5], writes=[r_po5[dh]])
                            ds_ = slice(dh * 512, (dh + 1) * 512)
                            P.op("dve", lambda e, dh=dh, ds_=ds_, b2=b2: e.tensor_tensor(
                                out=hr[b2][:, ds_], in0=p_o5[dh][:], in1=g1b[:, ds_], op=ALU.mult),
                                reads=[r_po5[dh], r_bt], writes=[r_hr[b2]])
                        P.op("pool", lambda e, b2=b2: e.tensor_tensor(out=hr[b2][:], in0=hr[b2][:], in1=xr[b2][:],
                                                                     op=ALU.add),
                             reads=[r_hr[b2], r_xr[b2]], writes=[r_hr[b2]])
                        P.op("sp", lambda e, i=i, b2=b2: e.dma_start(out=hres_d[i * 128:(i + 1) * 128, :], in_=hr[b2][:]),
                             reads=[r_hr[b2]], writes=[r_hres[i]], dma=True)
                        P.op("act", lambda e, i=i, b2=b2: e.activation(out=junk[:], in_=hr[b2][:], func=AF.Square,
                                                                        accum_out=ss2[:, i:i + 1]),
                             reads=[r_hr[b2]], writes=[r_junk, r_ss2[i]])
                        P.op("act", lambda e, i=i: e.activation(out=ss2[:, i:i + 1], in_=ss2[:, i:i + 1], func=AF.Sqrt,
                                                                scale=1.0 / D, bias=epsb[:]),
                             reads=[r_ss2[i], r_const], writes=[r_ss2[i]])
                        P.op("dve", lambda e, i=i: e.reciprocal(out=ss2[:, i:i + 1], in_=ss2[:, i:i + 1]),
                             reads=[r_ss2[i]], writes=[r_ss2[i]])
                        P.op("dve", lambda e, i=i, b2=b2: e.scalar_tensor_tensor(
                            out=h2f[b2][:], in0=hr[b2][:], scalar=ss2[:, i:i + 1], in1=a2b[:],
                            op0=ALU.mult, op1=ALU.mult), reads=[r_hr[b2], r_ss2[i], r_bt], writes=[r_h2f[b2]])
                        P.op("pool", lambda e, b2=b2: e.tensor_tensor(out=h2f[b2][:], in0=h2f[b2][:], in1=sh2b[:],
                                                                     op=ALU.add),
                             reads=[r_h2f[b2], r_bt], writes=[r_h2f[b2]])
                        P.op("act", lambda e, b2=b2: e.copy(out=h2b[b2][:], in_=h2f[b2][:]),
                             reads=[r_h2f[b2]], writes=[r_h2b[b2]])
                        P.op("sp", lambda e, i=i, b2=b2: e.dma_start(out=h2_d[i * 128:(i + 1) * 128, :], in_=h2b[b2][:]),
                             reads=[r_h2b[b2]], writes=[r_h2d[i]], dma=True)
                        P.op("dve", lambda e, b2=b2: e.tensor_tensor(out=h2l[b2][:], in0=h2f[b2][:], in1=h2b[b2][:],
                                                                      op=ALU.subtract),
                             reads=[r_h2f[b2], r_h2b[b2]], writes=[r_h2l[b2]])
                        for part, (srcT, r_src, dstT, r_dst) in enumerate(((h2b[b2], r_h2b[b2], h2Th, r_h2Th),
                                                                          (h2l[b2], r_h2l[b2], h2Tl, r_h2Tl))):
                            for kc in range(8):
                                P.op("pe", lambda e, kc=kc, srcT=srcT: e.transpose(
                                    out=p_tr[:, kc, :], in_=srcT[:, kc * 128:(kc + 1) * 128], identity=ident_b[:]),
                                    reads=[r_src, r_const], writes=[r_ptr5])
                            if part == 0:
                                P.op("act", lambda e, dstT=dstT: e.copy(out=dstT[:], in_=p_tr[:]),
                                     reads=[r_ptr5], writes=[r_dst])
                            else:
                                P.op("dve", lambda e, dstT=dstT: e.tensor_copy(out=dstT[:], in_=p_tr[:]),
                                     reads=[r_ptr5], writes=[r_dst])
                        n_acc = 0
                        for (aT, r_a, wpart) in ((h2Th, r_h2Th, rwh), (h2Tl, r_h2Tl, rwh), (h2Th, r_h2Th, rwl)):
                            for kc in range(8):
                                P.op("pe", lambda e, kc=kc, aT=aT, wpart=wpart, n_acc=n_acc: e.matmul(
                                    p_lg[:, 0, :], lhsT=aT[:, kc, :], rhs=wpart[:, kc, :],
                                    start=(n_acc == 0), stop=(n_acc == 23)),
                                    reads=[r_a, r_w5], writes=[r_plg])
                                n_acc += 1
                        P.op("dve", lambda e, i=i: e.tensor_tensor(out=lg_all[:, i, :], in0=p_lg[:, 0, :], in1=rbb[:],
                                                                    op=ALU.add),
                             reads=[r_plg, r_w5], writes=[r_lgall[i]])
                tot_all = xr[0][:].rearrange("p (a b) -> p a b", a=NT); r_tot = r_xr[0]
                cum_all = xr[1][:].rearrange("p (a b) -> p a b", a=NT); r_cumall = r_xr[1]
                for i in range(NT):
                    P.op("dve", lambda e, i=i: e.max(out=m8a[:, i, :], in_=lg_all[:, i, :]),
                         reads=[r_lgall[i]], writes=[r_m8a])
                P.op("dve", lambda e: e.tensor_tensor(out=mask_all[:], in0=lg_all[:],
                                                      in1=bc(m8a[:, :, 3:4], [128, NT, E]), op=ALU.is_ge),
                     reads=r_lgall + [r_m8a], writes=r_maskall)
                P.op("dve", lambda e: e.tensor_tensor(out=lg_all[:], in0=lg_all[:],
                                                      in1=bc(m8a[:, :, 0:1], [128, NT, E]), op=ALU.subtract),
                     reads=r_lgall + [r_m8a], writes=r_lgall)
                P.op("act", lambda e: e.activation(out=lg_all[:], in_=lg_all[:], func=AF.Exp),
                     reads=r_lgall, writes=r_lgall)
                P.op("dve", lambda e: e.tensor_tensor(out=lg_all[:], in0=lg_all[:], in1=mask_all[:], op=ALU.mult),
                     reads=r_lgall + r_maskall, writes=r_lgall)
                P.op("dve", lambda e: e.reduce_sum(out=den_all[:], in_=lg_all[:], axis=AX.X), reads=r_lgall, writes=[r_den])
                P.op("dve", lambda e: e.reciprocal(out=den_all[:], in_=den_all[:]), reads=[r_den], writes=[r_den])
                P.op("dve", lambda e: e.tensor_tensor(out=gw_all[:], in0=lg_all[:],
                                                      in1=bc(den_all[:, :].unsqueeze(2), [128, NT, E]), op=ALU.mult),
                     reads=r_lgall + [r_den], writes=[r_gwall])
                for i in range(NT):
                    P.op("pe", lambda e, i=i: e.matmul(p_yh[i // 16][:, (i % 16) * E:(i % 16 + 1) * E], lhsT=ones_f[:],
                                                       rhs=mask_all[:, i, :], start=True, stop=True),
                         reads=[r_maskall[i], r_ones], writes=[r_pyh[i // 16]])
                    P.op("pe", lambda e, i=i: e.matmul(p_yc[i // 16][:, (i % 16) * E:(i % 16 + 1) * E], lhsT=lstrict[:],
                                                       rhs=mask_all[:, i, :], start=True, stop=True),
                         reads=[r_maskall[i], r_const], writes=[r_pyc[i // 16]])
                for hf in range(2):
                    P.op("act", lambda e, hf=hf: e.copy(out=tot_all[:, hf * 16:(hf + 1) * 16, :].rearrange("p a b -> p (a b)"),
                                                        in_=p_yh[hf][:]), reads=[r_pyh[hf]], writes=[r_tot])
                P.op("pool", lambda e: e.memset(cum_all[:, 0, :], 0.0), writes=[r_cumall])
                for i in range(1, NT):
                    P.op("dve", lambda e, i=i: e.tensor_tensor(out=cum_all[:, i, :], in0=cum_all[:, i - 1, :],
                                                                in1=tot_all[:, i - 1, :], op=ALU.add),
                         reads=[r_cumall, r_tot], writes=[r_cumall])
                P.op("dve", lambda e: e.tensor_tensor(out=cnt_b[:], in0=cum_all[:, NT - 1, :], in1=tot_all[:, NT - 1, :],
                                                      op=ALU.add), reads=[r_cumall, r_tot], writes=[r_cnt])
                for hf in range(2):
                    P.op("dve", lambda e, hf=hf: e.tensor_tensor(
                        out=rank_all[:, hf * 16:(hf + 1) * 16, :].rearrange("p a b -> p (a b)"), in0=p_yc[hf][:],
                        in1=cum_all[:, hf * 16:(hf + 1) * 16, :].rearrange("p a b -> p (a b)"), op=ALU.add),
                        reads=[r_pyc[hf], r_cumall], writes=r_rankall)
                P.flush()

        with ExitStack() as st:
            padb = sb(st, "padb", [128, E]); endb = sb(st, "endb", [128, E]); startb = sb(st, "startb", [128, E])
            r_rt = Res()
            P.op("pool", lambda e: e.memset(padb[:], 0.0), writes=[r_rt])
            for k in range(8):
                P.op("dve", lambda e, k=k: e.scalar_tensor_tensor(out=padb[:], in0=cnt_b[:], scalar=float(512 * k + 1),
                                                                  in1=padb[:], op0=ALU.is_ge, op1=ALU.add),
                     reads=[r_cnt, r_rt], writes=[r_rt])
            P.op("dve", lambda e: e.tensor_scalar_mul(out=padb[:], in0=padb[:], scalar1=512.0), reads=[r_rt], writes=[r_rt])
            P.op("dve", lambda e: e.tensor_tensor_scan(out=endb[:], data0=ones_f[:, 0:E], data1=padb[:], initial=0.0,
                                                       op0=ALU.mult, op1=ALU.add), reads=[r_rt, r_ones], writes=[r_rt])
            P.op("dve", lambda e: e.tensor_tensor(out=startb[:], in0=endb[:], in1=padb[:], op=ALU.subtract),
                 reads=[r_rt], writes=[r_rt])
            dsel = sb(st, "dsel", [128, NT, E]); r_dsel = Res()
            P.op("dve", lambda e: e.tensor_tensor(out=dsel[:], in0=rank_all[:], in1=bc(startb[:, :].unsqueeze(1), [128, NT, E]),
                                                  op=ALU.add), reads=r_rankall + [r_rt], writes=[r_dsel])
            P.op("dve", lambda e: e.scalar_tensor_tensor(out=dsel[:], in0=dsel[:], scalar=1.0, in1=mask_all[:],
                                                         op0=ALU.add, op1=ALU.mult),
                 reads=[r_dsel] + r_maskall, writes=[r_dsel])
            t8 = sb(st, "t8", [128, NT, 8]); r_t8 = Res()
            oh = sb(st, "oh", [128, E]); r_oh_ = Res()
            d4f = sb(st, "d4f", [128, NT, 4]); r_d4f = Res()
            junk2 = sb(st, "junk2", [128, E])
            for i in range(NT):
                P.op("dve", lambda e, i=i: e.max(out=t8[:, i, :], in_=dsel[:, i, :]), reads=[r_dsel], writes=[r_t8])
                for k in range(4):
                    P.op("dve", lambda e, i=i, k=k: e.tensor_scalar(out=oh[:], in0=dsel[:, i, :], scalar1=t8[:, i, k:k + 1],
                                                                     scalar2=None, op0=ALU.is_equal),
                         reads=[r_dsel, r_t8], writes=[r_oh_])
                    P.op("dve", lambda e, i=i: e.tensor_tensor(out=junk2[:], in0=oh[:], in1=gw_all[:, i, :], op=ALU.mult),
                         reads=[r_oh_, r_gwall], writes=[r_oh_])
                    P.op("dve", lambda e, i=i, k=k: e.reduce_sum(out=w4[:, i, k:k + 1], in_=junk2[:], axis=AX.X),
                         reads=[r_oh_], writes=[r_w4])
            P.op("dve", lambda e: e.tensor_scalar_add(out=d4f[:], in0=t8[:, :, 0:4], scalar1=-1.0),
                 reads=[r_t8], writes=[r_d4f])
            P.op("dve", lambda e: e.tensor_copy(out=dest4i[:], in_=d4f[:]), reads=[r_d4f], writes=[r_dest4])
            fill = sb(st, "fill", [128, NBLK * 4], I32); r_fill = Res()
            tokid = sb(st, "tokid", [128, NT], I32); r_tok = Res()
            P.op("pool", lambda e: e.iota(fill[:], pattern=[[0, NBLK * 4]], base=S, channel_multiplier=0), writes=[r_fill])
            P.op("pool", lambda e: e.iota(tokid[:], pattern=[[128, NT]], base=0, channel_multiplier=1), writes=[r_tok])
            P.op("sp", lambda e: e.dma_start(out=slot_d.rearrange("(p c) o -> p (c o)", p=128), in_=fill[:]),
                 reads=[r_fill], writes=[r_slotd], dma=True)
            r_sc = [Res() for _ in range(NT * 4)]
            for i in range(NT):
                for k in range(4):
                    P.op("pool", lambda e, i=i, k=k: e.indirect_dma_start(
                        out=slot_d[:, :], out_offset=bass.IndirectOffsetOnAxis(ap=dest4i[:, i, k:k + 1], axis=0),
                        in_=tokid[:, i:i + 1], in_offset=None),
                        reads=[r_dest4, r_tok, r_slotd], writes=[r_sc[i * 4 + k]], dma=True)
            for g in range(8):
                P.op("sp", lambda e, g=g: e.dma_start(
                    out=slot_sb[:, g * 32:(g + 1) * 32],
                    in_=slot_d[g * 4096:(g + 1) * 4096, :].rearrange("(c p) o -> p (c o)", p=128)),
                    reads=[r_slotd] + r_sc, writes=[r_slot_sb], dma=True)
            thr = sb(st, "thr", [128, NBLK]); cmp = sb(st, "cmp", [128, NBLK, E]); beb = sb(st, "beb", [128, NBLK])
            iokc = sb(st, "iokc", [128, 8]); wif = sb(st, "wif", [128, NBLK, 8]); r_be = Res()
            P.op("pool", lambda e: e.iota(thr[:], pattern=[[512, NBLK]], base=0, channel_multiplier=0,
                                          allow_small_or_imprecise_dtypes=True), writes=[r_be])
            P.op("pool", lambda e: e.iota(iokc[:], pattern=[[128, 8]], base=0, channel_multiplier=1,
                                          allow_small_or_imprecise_dtypes=True), writes=[r_be])
            P.op("dve", lambda e: e.tensor_tensor(out=cmp[:], in0=bc(thr[:, :].unsqueeze(2), [128, NBLK, E]),
                                                  in1=bc(endb[:, :].unsqueeze(1), [128, NBLK, E]), op=ALU.is_ge),
                 reads=[r_rt, r_be], writes=[r_be])
            P.op("dve", lambda e: e.reduce_sum(out=beb[:], in_=cmp[:], axis=AX.X), reads=[r_be], writes=[r_be])
            P.op("dve", lambda e: e.tensor_scalar_min(out=beb[:], in0=beb[:], scalar1=float(E - 1)), reads=[r_be], writes=[r_be])
            P.op("dve", lambda e: e.tensor_scalar(out=ohb[:], in0=beb[0:32, :], scalar1=iota_p[0:32, 0:1], scalar2=None,
                                                   op0=ALU.is_equal), reads=[r_be, r_const], writes=[r_ohb])
            P.op("dve", lambda e: e.scalar_tensor_tensor(out=wif[:], in0=bc(beb[:, :].unsqueeze(2), [128, NBLK, 8]),
                                                         scalar=1024.0, in1=bc(iokc[:, :].unsqueeze(1), [128, NBLK, 8]),
                                                         op0=ALU.mult, op1=ALU.add), reads=[r_be], writes=[r_be])
            same = sb(st, "same", [128, NBLK]); pm1 = sb(st, "pm1", [128, 1])
            P.op("dve", lambda e: e.memset(same[:, 0:1], 0.0), reads=[r_be], writes=[r_be])
            P.op("dve", lambda e: e.tensor_tensor(out=same[:, 1:NBLK], in0=beb[:, 1:NBLK], in1=beb[:, 0:NBLK - 1],
                                                  op=ALU.is_equal), reads=[r_be], writes=[r_be])
            P.op("dve", lambda e: e.tensor_scalar(out=pm1[:], in0=iota_p[:], scalar1=1.0, scalar2=1048576.0,
                                                   op0=ALU.min, op1=ALU.mult), reads=[r_be, r_const], writes=[r_be])
            P.op("dve", lambda e: e.tensor_scalar_mul(out=same[:], in0=same[:], scalar1=pm1[:, 0:1]),
                 reads=[r_be], writes=[r_be])
            P.op("dve", lambda e: e.tensor_tensor(out=wif[:], in0=wif[:], in1=bc(same[:, :].unsqueeze(2), [128, NBLK, 8]),
                                                  op=ALU.add), reads=[r_be], writes=[r_be])
            P.op("dve", lambda e: e.tensor_copy(out=widx[:], in_=wif[:]), reads=[r_be], writes=[r_widx])
            if debug:
                P.op("sp", lambda e: e.dma_start(out=dbg_d[:, 0:NT * E], in_=gw_all[:].rearrange("p a b -> p (a b)")),
                     reads=r_maskall, writes=[Res()], dma=True)
                P.op("sp", lambda e: e.dma_start(out=dbg_d[:, NT * E:NT * E + NBLK], in_=beb[:]),
                     reads=[r_be], writes=[Res()], dma=True)
                P.op("sp", lambda e: e.dma_start(out=dbg_d[:, NT * E + NBLK:NT * E + NBLK + NT * 4],
                                                 in_=d4f[:].rearrange("p a b -> p (a b)")),
                     reads=[r_d4f], writes=[Res()], dma=True)
            P.flush()
        stR.close()

        with ExitStack() as st:
            bgu = sb(st, "bgu", [E, 2 * D], BF16); bdn = sb(st, "bdn", [E, D], BF16); r_bias = Res()
            P.op("pool", lambda e: e.dma_start(out=bgu[:], in_=b_gu[:, :]), writes=[r_bias], dma=True)
            P.op("pool", lambda e: e.dma_start(out=bdn[:], in_=b_dn[:, :]), writes=[r_bias], dma=True)
            wgu = [sb(st, "wgu%d" % i, [128, 8, 2 * D], BF16) for i in range(2)]; r_wgu = [[Res() for _ in range(8)] for _ in range(2)]
            wdn = [sb(st, "wdn%d" % i, [128, 8, D], BF16) for i in range(2)]; r_wdn = [[Res() for _ in range(8)] for _ in range(2)]
            xg = [sb(st, "xg%d" % i, [128, 4, D], BF16) for i in range(2)]; r_xg = [[Res() for _ in range(4)] for _ in range(2)]
            xT = sb(st, "xT", [128, 8, 512], BF16); r_xT = Res()
            aT = sb(st, "aT", [128, 8, 512], BF16); r_aT = Res()
            ohj = [sb(st, "ohj%d" % i, [E, 512], BF16) for i in range(2)]; r_ohj = [Res(), Res()]
            yst = sb(st, "yst", [128, 4, D]); r_yst = Res()
            gp = [sb(st, "gp%d" % i, [128, 512]) for i in range(2)]; r_gp = [Res(), Res()]
            sg_ = [sb(st, "sg%d" % i, [128, 512]) for i in range(2)]; r_sg = [Res(), Res()]
            up = [sb(st, "up%d" % i, [128, 512]) for i in range(2)]; r_up7 = [Res(), Res()]
            p_x = [ps(st, "p_x%d" % i, [128, 8, 128], BF16) for i in range(2)]; r_px = [Res(), Res()]
            p_g = [ps(st, "p_g%d" % i, [128, 512]) for i in range(2)]; r_pg = [Res(), Res()]
            p_u = [ps(st, "p_u%d" % i, [128, 512]) for i in range(2)]; r_pu = [Res(), Res()]
            p_y = [ps(st, "p_y%d" % i, [128, 512]) for i in range(2)]; r_py = [Res(), Res()]

            bnd_reg = []

            def get_bnd(e):
                if not bnd_reg:
                    r = e.alloc_register("bnd_reg")
                    e.reg_mov(r, E * D - 1)
                    bnd_reg.append(r)
                return bnd_reg[0]

            def load_block(j):
                bi = j % 2
                if j >= 1:
                    P.op("dve", lambda e, bi=bi: e.tensor_copy(out=wgu[bi][:], in_=wgu[1 - bi][:]),
                         reads=r_wgu[1 - bi], writes=r_wgu[bi])
                    P.op("dve", lambda e, bi=bi: e.tensor_copy(out=wdn[bi][:], in_=wdn[1 - bi][:]),
                         reads=r_wdn[1 - bi], writes=r_wdn[bi])
                for kc in range(8):
                    P.op("pool", lambda e, j=j, kc=kc, bi=bi: e.indirect_dma_start(
                        out=wgu[bi][:, kc, :], out_offset=None, in_=w_gu[:, :],
                        in_offset=bass.IndirectOffsetOnAxis(ap=widx[:, j, kc:kc + 1], axis=0),
                        bounds_check=get_bnd(e), oob_is_err=False),
                        reads=[r_widx], writes=[r_wgu[bi][kc]], dma=True)
                for kc in range(8):
                    P.op("pool", lambda e, j=j, kc=kc, bi=bi: e.indirect_dma_start(
                        out=wdn[bi][:, kc, :], out_offset=None, in_=w_dn[:, :],
                        in_offset=bass.IndirectOffsetOnAxis(ap=widx[:, j, kc:kc + 1], axis=0),
                        bounds_check=get_bnd(e), oob_is_err=False),
                        reads=[r_widx], writes=[r_wdn[bi][kc]], dma=True)
                for q in range(4):
                    P.op("pool", lambda e, j=j, q=q, bi=bi: e.indirect_dma_start(
                        out=xg[bi][:, q, :], out_offset=None, in_=h2_d[:, :],
                        in_offset=bass.IndirectOffsetOnAxis(ap=slot_sb[:, j * 4 + q:j * 4 + q + 1], axis=0)),
                        reads=[r_slot_sb] + r_h2d, writes=[r_xg[bi][q]], dma=True)
                P.op("dve", lambda e, j=j, bi=bi: e.tensor_copy(out=ohj[bi][:], in_=bc(ohb[:, j:j + 1], [E, 512])),
                     reads=[r_ohb], writes=[r_ohj[bi]])

            load_block(0)
            pxi = 0
            for j in range(NBLK):
                bi = j % 2
                if j + 1 < NBLK:
                    load_block(j + 1)
                for kc in range(8):
                    pb = pxi % 2; pxi += 1
                    for q in range(4):
                        P.op("pe", lambda e, kc=kc, q=q, pb=pb, bi=bi: e.transpose(
                            out=p_x[pb][:, q, :], in_=xg[bi][:, q, kc * 128:(kc + 1) * 128], identity=ident_b[:]),
                            reads=[r_xg[bi][q], r_const], writes=[r_px[pb]])
                    P.op("act", lambda e, kc=kc, pb=pb: e.copy(out=xT[:, kc, :].rearrange("p (a b) -> p a b", a=4), in_=p_x[pb][:, 0:4, :]),
                         reads=[r_px[pb]], writes=[r_xT])
                for m in range(8):
                    b2 = m % 2
                    for (pt_, rp, col0) in ((p_g[b2], r_pg[b2], m * 128), (p_u[b2], r_pu[b2], D + m * 128)):
                        for kc in range(8):
                            P.op("pe", lambda e, pt_=pt_, kc=kc, col0=col0, bi=bi: e.matmul(
                                pt_[:], lhsT=wgu[bi][:, kc, col0:col0 + 128], rhs=xT[:, kc, :],
                                start=(kc == 0), stop=False), reads=[r_wgu[bi][kc], r_xT], writes=[rp])
                        P.op("pe", lambda e, pt_=pt_, col0=col0, bi=bi: e.matmul(
                            pt_[:], lhsT=bgu[:, col0:col0 + 128], rhs=ohj[bi][:], start=False, stop=True),
                            reads=[r_bias, r_ohj[bi]], writes=[rp])
                    P.op("dve", lambda e, b2=b2: e.tensor_scalar_min(out=gp[b2][:], in0=p_g[b2][:], scalar1=7.0),
                         reads=[r_pg[b2]], writes=[r_gp[b2]])
                    P.op("act", lambda e, b2=b2: e.activation(out=sg_[b2][:], in_=gp[b2][:], func=AF.Sigmoid, scale=1.702),
                         reads=[r_gp[b2]], writes=[r_sg[b2]])
                    P.op("dve", lambda e, b2=b2: e.tensor_scalar(out=up[b2][:], in0=p_u[b2][:], scalar1=-7.0, scalar2=7.0,
                                                                  op0=ALU.max, op1=ALU.min),
                         reads=[r_pu[b2]], writes=[r_up7[b2]])
                    P.op("dve", lambda e, b2=b2: e.tensor_tensor(out=gp[b2][:], in0=gp[b2][:], in1=sg_[b2][:], op=ALU.mult),
                         reads=[r_gp[b2], r_sg[b2]], writes=[r_gp[b2]])
                    P.op("dve", lambda e, b2=b2, m=m: e.scalar_tensor_tensor(
                        out=aT[:, m, :], in0=up[b2][:], scalar=1.0, in1=gp[b2][:], op0=ALU.add, op1=ALU.mult),
                        reads=[r_up7[b2], r_gp[b2]], writes=[r_aT])
                for q in range(4):
                    for dh in range(2):
                        pb = (q * 2 + dh) % 2
                        for m in range(8):
                            P.op("pe", lambda e, q=q, dh=dh, m=m, pb=pb, bi=bi: e.matmul(
                                p_y[pb][:], lhsT=aT[:, m, q * 128:(q + 1) * 128], rhs=wdn[bi][:, m, dh * 512:(dh + 1) * 512],
                                start=(m == 0), stop=False), reads=[r_aT, r_wdn[bi][m]], writes=[r_py[pb]])
                        P.op("pe", lambda e, dh=dh, pb=pb, bi=bi: e.matmul(
                            p_y[pb][:], lhsT=ohj[bi][:, 0:128], rhs=bdn[:, dh * 512:(dh + 1) * 512], start=False, stop=True),
                            reads=[r_ohj[bi], r_bias], writes=[r_py[pb]])
                        if dh == 0:
                            P.op("act", lambda e, q=q, pb=pb: e.copy(out=yst[:, q, 0:512], in_=p_y[pb][:]),
                                 reads=[r_py[pb]], writes=[r_yst])
                        else:
                            P.op("dve", lambda e, q=q, pb=pb: e.tensor_copy(out=yst[:, q, 512:1024], in_=p_y[pb][:]),
                                 reads=[r_py[pb]], writes=[r_yst])
                P.op("sp", lambda e, j=j: e.dma_start(
                    out=ys_d[j * 512:(j + 1) * 512, :].rearrange("(q p) d -> p q d", p=128), in_=yst[:]),
                    reads=[r_yst], writes=[r_ysd], dma=True)
            P.flush()

        with ExitStack() as st:
            G = [[sb(st, "G%d_%d" % (b_, k), [128, D]) for k in range(4)] for b_ in range(2)]
            r_G = [[Res() for _ in range(4)] for _ in range(2)]
            hx = [sb(st, "hx%d" % i, [128, D]) for i in range(2)]; r_hx = [Res(), Res()]
            ac = [sb(st, "ac%d" % i, [128, D]) for i in range(2)]; r_ac = [Res(), Res()]
            ot = [sb(st, "ot%d" % i, [128, D]) for i in range(2)]; r_ot = [Res(), Res()]
            junk3 = sb(st, "junk3", [128, D], BF16); r_j3 = Res()
            ss3 = sb(st, "ss3", [128, NT]); r_ss3 = [Res() for _ in range(NT)]
            r_out = Res()
            for i in range(NT):
                b2 = i % 2
                for k in range(4):
                    P.op("pool", lambda e, i=i, k=k, b2=b2: e.indirect_dma_start(
                        out=G[b2][k][:], out_offset=None, in_=ys_d[:, :],
                        in_offset=bass.IndirectOffsetOnAxis(ap=dest4i[:, i, k:k + 1], axis=0)),
                        reads=[r_dest4, r_ysd], writes=[r_G[b2][k]], dma=True)
                P.op("sp", lambda e, i=i, b2=b2: e.dma_start(out=hx[b2][:], in_=hres_d[i * 128:(i + 1) * 128, :]),
                     reads=[r_hres[i]], writes=[r_hx[b2]], dma=True)
                P.op("dve", lambda e, i=i, b2=b2: e.tensor_scalar_mul(out=ac[b2][:], in0=G[b2][0][:], scalar1=w4[:, i, 0:1]),
                     reads=[r_G[b2][0], r_w4], writes=[r_ac[b2]])
                for k in range(1, 4):
                    P.op("dve", lambda e, i=i, k=k, b2=b2: e.scalar_tensor_tensor(
                        out=ac[b2][:], in0=G[b2][k][:], scalar=w4[:, i, k:k + 1], in1=ac[b2][:], op0=ALU.mult, op1=ALU.add),
                        reads=[r_G[b2][k], r_w4, r_ac[b2]], writes=[r_ac[b2]])
                P.op("dve", lambda e, b2=b2: e.tensor_tensor(out=ac[b2][:], in0=ac[b2][:], in1=g2b[:], op=ALU.mult),
                     reads=[r_ac[b2], r_g2b], writes=[r_ac[b2]])
                P.op("dve", lambda e, b2=b2: e.tensor_tensor(out=ac[b2][:], in0=ac[b2][:], in1=hx[b2][:], op=ALU.add),
                     reads=[r_ac[b2], r_hx[b2]], writes=[r_ac[b2]])
                P.op("act", lambda e, i=i, b2=b2: e.activation(out=junk3[:], in_=ac[b2][:], func=AF.Square,
                                                                accum_out=ss3[:, i:i + 1]),
                     reads=[r_ac[b2]], writes=[r_j3, r_ss3[i]])
                P.op("act", lambda e, i=i: e.activation(out=ss3[:, i:i + 1], in_=ss3[:, i:i + 1], func=AF.Sqrt,
                                                        scale=1.0 / D, bias=epsb[:]),
                     reads=[r_ss3[i], r_const], writes=[r_ss3[i]])
                P.op("dve", lambda e, i=i: e.reciprocal(out=ss3[:, i:i + 1], in_=ss3[:, i:i + 1]),
                     reads=[r_ss3[i]], writes=[r_ss3[i]])
                P.op("dve", lambda e, i=i, b2=b2: e.scalar_tensor_tensor(
                    out=ot[b2][:], in0=ac[b2][:], scalar=ss3[:, i:i + 1], in1=fgb[:], op0=ALU.mult, op1=ALU.mult),
                    reads=[r_ac[b2], r_ss3[i], r_fgb], writes=[r_ot[b2]])
                P.op("sp", lambda e, i=i, b2=b2: e.dma_start(out=out[i * 128:(i + 1) * 128, :], in_=ot[b2][:]),
                     reads=[r_ot[b2]], writes=[r_out], dma=True)
            P.flush()
    return nc


_NC_CACHE = {}


def _get_nc(debug=False):
    if debug not in _NC_CACHE:
        _NC_CACHE[debug] = build_nc(debug)
    return _NC_CACHE[debug]


def make_in_maps(inputs, cores):
    g = lambda k: np.ascontiguousarray(np.asarray(inputs[k], dtype=np.float32))
    shared = {
        "ada_w": g("ada_w")[0], "ada_b": g("ada_b")[0], "norm1_g": g("norm1_g")[0], "w_in": g("w_in")[0],
        "lb_table": g("lb_table"), "hg_norm_g": g("hg_norm_g")[0], "w_o_hg": g("w_o_hg")[0],
        "dw_w": g("dw_w")[0], "dw_b": g("dw_b")[0], "cv_ln_g": g("cv_ln_g")[0], "cv_ln_b": g("cv_ln_b")[0],
        "w_o_cv": g("w_o_cv")[0], "b_o_cv": g("b_o_cv")[0], "w_out": g("w_out")[0], "norm2_g": g("norm2_g")[0],
        "router_w": g("router_w")[0], "router_b": g("router_b")[0],
        "w_gate_up": g("w_gate_up")[0].reshape(E * D, 2 * D), "b_gate_up": g("b_gate_up")[0],
        "w_down": g("w_down")[0].reshape(E * D, D), "b_down": g("b_down")[0],
        "final_norm_g": g("final_norm_g"),
    }
    xs = g("x")
    cs = g("c")
    maps = []
    for b in cores:
        m = dict(shared)
        m["x"] = xs[b]
        m["c"] = cs[b]
        maps.append(m)
    return maps


def kernel(**inputs):
    nc = _get_nc(False)
    maps = make_in_maps(inputs, list(range(8)))
    res = run_bass_kernel_spmd(nc, maps, core_ids=list(range(8)))
    return np.stack([np.asarray(r["out"], dtype=np.float32) for r in res.results], axis=0)
```

```python
import os
from contextlib import ExitStack

import numpy as np
import concourse.bass as bass
import concourse.mybir as mybir
from concourse.bass_utils import run_bass_kernel_spmd

F32 = mybir.dt.float32
BF16 = mybir.dt.bfloat16
I32 = mybir.dt.int32
AF = mybir.ActivationFunctionType
ALU = mybir.AluOpType
AX = mybir.AxisListType

S = 4096
D = 1024
NT = 32
NCH = 8
E = 32
NBLK = 64
NSLOT = NBLK * 512
EPS = 1e-6
IN_COLS = 5632

ENGS = ("pe", "act", "dve", "pool", "sp")


class Res:
    __slots__ = ("last_w", "readers")

    def __init__(self):
        self.last_w = None
        self.readers = {}


class Prog:
    def __init__(self, nc, n_dma_sems=48):
        self.nc = nc
        self.ops = {e: [] for e in ENGS}
        self.seq = {e: 0 for e in ENGS}
        self.waited = {e: {} for e in ENGS}
        self.sems = {}
        self.n_dma = n_dma_sems
        self.dma_uses = [0] * n_dma_sems
        self.dma_rr = 0
        self.same_engine_sync = True

    def alloc(self, stack):
        for e in ENGS:
            self.sems["c_" + e] = stack.enter_context(self.nc.semaphore("c_" + e))
        for i in range(self.n_dma):
            self.sems["d%d" % i] = stack.enter_context(self.nc.semaphore("d%d" % i))

    def op(self, eng, fn, reads=(), writes=(), dma=False):
        deps = {}

        def add(s, v):
            if deps.get(s, 0) < v:
                deps[s] = v

        for r in reads:
            if r.last_w is not None:
                add(*r.last_w)
        for w in writes:
            if w.last_w is not None:
                add(*w.last_w)
            for s, v in w.readers.items():
                add(s, v)
        if dma:
            i = self.dma_rr
            self.dma_rr = (self.dma_rr + 1) % self.n_dma
            s = "d%d" % i
            if self.dma_uses[i] > 0:
                add(s, 16 * self.dma_uses[i])
            self.dma_uses[i] += 1
            ev = (s, 16 * self.dma_uses[i])
        else:
            self.seq[eng] += 1
            ev = ("c_" + eng, self.seq[eng])
        waits = []
        wd = self.waited[eng]
        for s, v in deps.items():
            if s == "c_" + eng and (eng == "pe" or not self.same_engine_sync):
                continue
            if wd.get(s, 0) >= v:
                continue
            wd[s] = v
            waits.append((s, v))
        self.ops[eng].append((waits, fn, ev, dma))
        for r in reads:
            if r.readers.get(ev[0], 0) < ev[1]:
                r.readers[ev[0]] = ev[1]
        for w in writes:
            w.last_w = ev
            w.readers = {}
        return ev

    def barrier(self):
        allev = []
        for e in ENGS:
            if self.seq[e] > 0:
                allev.append(("c_" + e, self.seq[e]))
        for i in range(self.n_dma):
            if self.dma_uses[i] > 0:
                allev.append(("d%d" % i, 16 * self.dma_uses[i]))
        for e in ENGS:
            waits = []
            wd = self.waited[e]
            for s, v in allev:
                if s == "c_" + e:
                    continue
                if wd.get(s, 0) >= v:
                    continue
                wd[s] = v
                waits.append((s, v))
            if waits:
                self.ops[e].append((waits, None, None, False))

    def replay(self, eng, e):
        for waits, fn, ev, dma in self.ops[eng]:
            for s, v in waits:
                e.wait_ge(self.sems[s], v)
            if fn is None:
                continue
            inst = fn(e)
            inst.then_inc(self.sems[ev[0]], 16 if dma else 1)

    def run(self):
        nc = self.nc
        with nc.Block() as block:
            @block.tensor
            def _(e):
                self.replay("pe", e)

            @block.scalar
            def _(e):
                self.replay("act", e)

            @block.vector
            def _(e):
                self.replay("dve", e)

            @block.gpsimd
            def _(e):
                self.replay("pool", e)

            @block.sync
            def _(e):
                self.replay("sp", e)
        self.ops = {e: [] for e in ENGS}

    def flush(self):
        self.barrier()
        self.run()


def bc(ap, shape):
    return ap.broadcast_to(list(shape))


def build_nc(debug=False):
    nc = bass.Bass("TRN2", target_bir_lowering=False)

    def din(name, shape, dt=F32):
        return nc.dram_tensor(name, list(shape), dt, kind="ExternalInput").ap()

    def dscr(name, shape, dt=F32, out=False):
        return nc.dram_tensor(name, list(shape), dt, kind="ExternalOutput" if out else "Internal").ap()

    x = din("x", [S, D])
    c_in = din("c", [D])
    ada_w = din("ada_w", [D, 6 * D])
    ada_b = din("ada_b", [6 * D])
    norm1_g = din("norm1_g", [D])
    w_in = din("w_in", [D, IN_COLS])
    lb_table = din("lb_table", [2, 2, 512])
    hg_norm_g = din("hg_norm_g", [4, 128])
    w_o_hg = din("w_o_hg", [512, D])
    dw_w = din("dw_w", [31, 512])
    dw_b = din("dw_b", [512])
    cv_ln_g = din("cv_ln_g", [512])
    cv_ln_b = din("cv_ln_b", [512])
    w_o_cv = din("w_o_cv", [512, D])
    b_o_cv = din("b_o_cv", [D])
    w_out = din("w_out", [D, D])
    norm2_g = din("norm2_g", [D])
    router_w = din("router_w", [D, E])
    router_b = din("router_b", [E])
    w_gu = din("w_gate_up", [E * D, 2 * D])
    b_gu = din("b_gate_up", [E, 2 * D])
    w_dn = din("w_down", [E * D, D])
    b_dn = din("b_down", [E, D])
    fin_g = din("final_norm_g", [D])
    out = nc.dram_tensor("out", [S, D], F32, kind="ExternalOutput").ap()

    mod_d = dscr("mod_d", [6, D])
    projT_d = dscr("projT_d", [IN_COLS, S])
    vtok_d = dscr("vtok_d", [S, 512], BF16)
    hres_d = dscr("hres_d", [S, D], F32, out=debug)
    h2_d = dscr("h2_d", [S + 128, D], BF16)
    slot_d = dscr("slot_d", [NSLOT, 1], I32)
    ys_d = dscr("ys_d", [NSLOT, D], F32)
    dbg_d = dscr("dbg_d", [128, 32 * 40], F32, out=True) if debug else None

    with ExitStack() as top:
        P = Prog(nc)
        P.alloc(top)
        top.enter_context(nc.allow_non_contiguous_dma(reason="small parameter / index layouts"))

        def sb(st, name, shape, dt=F32):
            return st.enter_context(nc.sbuf_tensor(name, list(shape), dt))

        def ps(st, name, shape, dt=F32):
            return st.enter_context(nc.psum_tensor(name, list(shape), dt))

        ident_f = sb(top, "ident_f", [128, 128]); r_ident = Res()
        ident_b = sb(top, "ident_b", [128, 128], BF16)
        ones_f = sb(top, "ones_f", [128, 128]); r_ones = Res()
        lstrict = sb(top, "lstrict", [128, 128])
        epsb = sb(top, "epsb", [128, 1])
        iota_p = sb(top, "iota_p", [128, 1])
        g2b = sb(top, "g2b", [128, D]); r_g2b = Res()
        fgb = sb(top, "fgb", [128, D]); r_fgb = Res()
        dest4i = sb(top, "dest4i", [128, NT, 4], I32); r_dest4 = Res()
        w4 = sb(top, "w4", [128, NT, 4]); r_w4 = Res()
        slot_sb = sb(top, "slot_sb", [128, NBLK * 4], I32); r_slot_sb = Res()
        widx = sb(top, "widx", [128, NBLK, 8], I32); r_widx = Res()
        ohb = sb(top, "ohb", [32, NBLK]); r_ohb = Res()
        r_const = Res()

        P.op("pool", lambda e: e.memset(ident_f[:], 1.0), writes=[r_ident])
        P.op("pool", lambda e: e.affine_select(out=ident_f[:], in_=ident_f[:], pattern=[[-1, 128]],
                                               compare_op=ALU.is_equal, fill=0.0, base=0, channel_multiplier=1),
             reads=[r_ident], writes=[r_ident])
        P.op("dve", lambda e: e.tensor_copy(out=ident_b[:], in_=ident_f[:]), reads=[r_ident], writes=[r_const])
        P.op("pool", lambda e: e.memset(ones_f[:], 1.0), writes=[r_ones])
        P.op("pool", lambda e: e.memset(lstrict[:], 1.0), writes=[r_const])
        P.op("pool", lambda e: e.affine_select(out=lstrict[:], in_=lstrict[:], pattern=[[1, 128]],
                                               compare_op=ALU.is_ge, fill=0.0, base=-1, channel_multiplier=-1),
             reads=[r_const], writes=[r_const])
        P.op("pool", lambda e: e.memset(epsb[:], EPS), writes=[r_const])
        P.op("pool", lambda e: e.iota(iota_p[:], pattern=[[0, 1]], base=0, channel_multiplier=1,
                                      allow_small_or_imprecise_dtypes=True), writes=[r_const])

        r_mod = Res()
        r_projT = {}
        r_vtok = [Res() for _ in range(NT)]
        r_hres = [Res() for _ in range(NT)]
        r_h2d = [Res() for _ in range(NT + 1)]
        r_slotd = Res()
        r_ysd = Res()

        with ExitStack() as st:
            c_sb = sb(st, "c_sb", [128, 8]); r_c = Res()
            adw = [sb(st, "adw%d" % i, [128, 8, 512]) for i in range(2)]; r_adw = [Res(), Res()]
            modrow = sb(st, "modrow", [1, 6 * D]); r_modrow = Res()
            adb = sb(st, "adb", [1, 6 * D]); r_adb = Res()
            pmod = [ps(st, "pmod%d" % i, [128, 512]) for i in range(2)]; r_pmod = [Res(), Res()]
            P.op("sp", lambda e: e.dma_start(out=c_sb[:], in_=c_in.rearrange("(c p) -> p c", p=128)),
                 writes=[r_c], dma=True)
            P.op("sp", lambda e: e.dma_start(out=adb[:], in_=ada_b.rearrange("(o n) -> o n", o=1)),
                 writes=[r_adb], dma=True)
            P.op("act", lambda e: e.activation(out=c_sb[:], in_=c_sb[:], func=AF.Silu), reads=[r_c], writes=[r_c])
            adw_v = ada_w.rearrange("(c p) n -> p c n", p=128)
            for n in range(12):
                bi = n % 2
                P.op("sp", lambda e, n=n, bi=bi: e.dma_start(out=adw[bi][:], in_=adw_v[:, :, n * 512:(n + 1) * 512]),
                     writes=[r_adw[bi]], dma=True)
                for kc in range(8):
                    P.op("pe", lambda e, bi=bi, kc=kc: e.matmul(pmod[bi][0:1, :], lhsT=c_sb[:, kc:kc + 1],
                                                                rhs=adw[bi][:, kc, :], start=(kc == 0), stop=(kc == 7)),
                         reads=[r_c, r_adw[bi]], writes=[r_pmod[bi]])
                P.op("dve", lambda e, n=n, bi=bi: e.tensor_tensor(out=modrow[0:1, n * 512:(n + 1) * 512],
                                                                   in0=pmod[bi][0:1, :],
                                                                   in1=adb[0:1, n * 512:(n + 1) * 512], op=ALU.add),
                     reads=[r_pmod[bi], r_adb], writes=[r_modrow])
            P.op("sp", lambda e: e.dma_start(out=mod_d.rearrange("(o k) d -> o (k d)", o=1), in_=modrow[:]),
                 reads=[r_modrow], writes=[r_mod], dma=True)
            P.flush()

        stR = ExitStack()
        mask_all = sb(stR, "mask_all", [128, NT, E]); r_maskall = [Res() for _ in range(NT)]
        gw_all = sb(stR, "gw_all", [128, NT, E])
        rank_all = sb(stR, "rank_all", [128, NT, E]); r_rankall = [Res() for _ in range(NT)]
        cummask = sb(stR, "cummask", [128, E]); r_cum = Res()
        cnt_b = sb(stR, "cnt_b", [128, E]); r_cnt = Res()
        with ExitStack() as stA:

            with ExitStack() as st:
                hT = sb(st, "hT", [128, 8, S], BF16)
                r_hT = [Res() for _ in range(NT)]
                a1 = sb(st, "a1", [128, 8]); sh1 = sb(st, "sh1", [128, 8]); n1f = sb(st, "n1f", [128, 8])
                r_a1 = Res()
                A1 = sb(st, "A1", [128, 8, 128]); SH1 = sb(st, "SH1", [128, 8, 128]); r_A1 = Res()
                P.op("sp", lambda e: e.dma_start(out=sh1[:], in_=mod_d[0].rearrange("(c p) -> p c", p=128)),
                     reads=[r_mod], writes=[r_a1], dma=True)
                P.op("sp", lambda e: e.dma_start(out=a1[:], in_=mod_d[1].rearrange("(c p) -> p c", p=128)),
                     reads=[r_mod], writes=[r_a1], dma=True)
                P.op("sp", lambda e: e.dma_start(out=n1f[:], in_=norm1_g.rearrange("(c p) -> p c", p=128)),
                     writes=[r_a1], dma=True)
                P.op("dve", lambda e: e.scalar_tensor_tensor(out=a1[:], in0=a1[:], scalar=1.0, in1=n1f[:],
                                                             op0=ALU.add, op1=ALU.mult), reads=[r_a1], writes=[r_a1])
                P.op("dve", lambda e: e.tensor_copy(out=A1[:], in_=bc(a1[:, :].unsqueeze(2), [128, 8, 128])),
                     reads=[r_a1], writes=[r_A1])
                P.op("dve", lambda e: e.tensor_copy(out=SH1[:], in_=bc(sh1[:, :].unsqueeze(2), [128, 8, 128])),
                     reads=[r_a1], writes=[r_A1])
                NXB = 3
                xt = [sb(st, "xt%d" % i, [128, D]) for i in range(NXB)]; r_xt = [Res() for _ in range(NXB)]
                sq = sb(st, "sq", [128, D], BF16); r_sq = Res()
                ss = sb(st, "ss", [128, NT]); r_ss = [Res() for _ in range(NT)]
                rs = sb(st, "rs", [128, NT])
                xn = [sb(st, "xn%d" % i, [128, D], BF16) for i in range(2)]; r_xn = [Res(), Res()]
                ptr = [ps(st, "ptr%d" % i, [128, 8, 128], BF16) for i in range(2)]; r_ptr = [Res(), Res()]
                tmp = [sb(st, "tmp%d" % i, [128, 8, 128]) for i in range(2)]; r_tmp = [Res(), Res()]
                for i in range(NT):
                    xb = i % NXB
                    b2 = i % 2
                    P.op("sp", lambda e, i=i, xb=xb: e.dma_start(out=xt[xb][:], in_=x[i * 128:(i + 1) * 128, :]),
                         writes=[r_xt[xb]], dma=True)
                    P.op("act", lambda e, i=i, xb=xb: e.activation(out=sq[:], in_=xt[xb][:], func=AF.Square,
                                                                    accum_out=ss[:, i:i + 1]),
                         reads=[r_xt[xb]], writes=[r_sq, r_ss[i]])
                    P.op("act", lambda e, i=i: e.activation(out=rs[:, i:i + 1], in_=ss[:, i:i + 1], func=AF.Sqrt,
                                                            scale=1.0 / D, bias=epsb[:]),
                         reads=[r_ss[i], r_const], writes=[r_ss[i]])
                    P.op("dve", lambda e, i=i: e.reciprocal(out=rs[:, i:i + 1], in_=rs[:, i:i + 1]),
                         reads=[r_ss[i]], writes=[r_ss[i]])
                    P.op("dve", lambda e, i=i, xb=xb, b2=b2: e.tensor_scalar_mul(out=xn[b2][:], in0=xt[xb][:],
                                                                                  scalar1=rs[:, i:i + 1]),
                         reads=[r_xt[xb], r_ss[i]], writes=[r_xn[b2]])
                    for kc in range(8):
                        P.op("pe", lambda e, kc=kc, b2=b2: e.transpose(out=ptr[b2][:, kc, :],
                                                                        in_=xn[b2][:, kc * 128:(kc + 1) * 128],
                                                                        identity=ident_b[:]),
                             reads=[r_xn[b2], r_const], writes=[r_ptr[b2]])
                    P.op("dve", lambda e, b2=b2: e.tensor_tensor(out=tmp[b2][:], in0=ptr[b2][:], in1=A1[:], op=ALU.mult),
                         reads=[r_ptr[b2], r_A1], writes=[r_tmp[b2]])
                    P.op("pool", lambda e, i=i, b2=b2: e.tensor_tensor(out=hT[:, :, i * 128:(i + 1) * 128],
                                                                        in0=tmp[b2][:], in1=SH1[:], op=ALU.add),
                         reads=[r_tmp[b2], r_A1], writes=[r_hT[i]])

                wv = w_in.rearrange("(c p) n -> p c n", p=128)
                wt = [sb(st, "wt%d" % i, [128, 8, 512], BF16) for i in range(2)]; r_wt = [Res(), Res()]
                stg = [sb(st, "stg%d" % i, [128, S]) for i in range(2)]; r_stg = [Res(), Res()]
                vst = [sb(st, "vst%d" % i, [128, 512], BF16) for i in range(2)]; r_vst = [Res(), Res()]
                pp = [ps(st, "pp%d" % i, [128, 512]) for i in range(4)]; r_pp = [Res() for _ in range(4)]
                ppi = 0
                sgi = 0
                for g in range(11):
                    bi = g % 2
                    P.op("pool", lambda e, g=g, bi=bi: e.dma_start(out=wt[bi][:], in_=wv[:, :, g * 512:(g + 1) * 512]),
                         writes=[r_wt[bi]], dma=True)
                    if g == 3:
                        for i in range(NT):
                            pi = ppi % 4; ppi += 1
                            for kc in range(8):
                                P.op("pe", lambda e, i=i, kc=kc, pi=pi, bi=bi: e.matmul(
                                    pp[pi][:], lhsT=hT[:, kc, i * 128:(i + 1) * 128], rhs=wt[bi][:, kc, :],
                                    start=(kc == 0), stop=(kc == 7)),
                                    reads=[r_hT[i], r_wt[bi]], writes=[r_pp[pi]])
                            vb = i % 2
                            P.op("act", lambda e, pi=pi, vb=vb: e.copy(out=vst[vb][:], in_=pp[pi][:]),
                                 reads=[r_pp[pi]], writes=[r_vst[vb]])
                            P.op("sp", lambda e, i=i, vb=vb: e.dma_start(out=vtok_d[i * 128:(i + 1) * 128, :],
                                                                         in_=vst[vb][:]),
                                 reads=[r_vst[vb]], writes=[r_vtok[i]], dma=True)
                        continue
                    for mm in range(4):
                        sg = sgi % 2; sgi += 1
                        for tc in range(NCH):
                            pi = ppi % 4; ppi += 1
                            for kc in range(8):
                                P.op("pe", lambda e, mm=mm, tc=tc, kc=kc, pi=pi, bi=bi: e.matmul(
                                    pp[pi][:], lhsT=wt[bi][:, kc, mm * 128:(mm + 1) * 128],
                                    rhs=hT[:, kc, tc * 512:(tc + 1) * 512], start=(kc == 0), stop=(kc == 7)),
                                    reads=[r_wt[bi]] + r_hT[tc * 4:(tc + 1) * 4], writes=[r_pp[pi]])
                            if tc % 2 == 0:
                                P.op("act", lambda e, tc=tc, pi=pi, sg=sg: e.copy(
                                    out=stg[sg][:, tc * 512:(tc + 1) * 512], in_=pp[pi][:]),
                                    reads=[r_pp[pi]], writes=[r_stg[sg]])
                            else:
                                P.op("dve", lambda e, tc=tc, pi=pi, sg=sg: e.tensor_copy(
                                    out=stg[sg][:, tc * 512:(tc + 1) * 512], in_=pp[pi][:]),
                                    reads=[r_pp[pi]], writes=[r_stg[sg]])
                        row0 = g * 512 + mm * 128
                        r_projT[row0] = Res()
                        P.op("sp", lambda e, row0=row0, sg=sg: e.dma_start(out=projT_d[row0:row0 + 128, :],
                                                                           in_=stg[sg][:]),
                             reads=[r_stg[sg]], writes=[r_projT[row0]], dma=True)
                P.flush()

            hgT = sb(stA, "hgT", [128, 4, S], BF16)
            r_hgT = [Res() for _ in range(4)]
            with ExitStack() as st:
                lbt = sb(st, "lbt", [128, 2, 2, 4]); r_lb = Res()
                lbv = sb(st, "lbv", [128, 2, 4]); oml = sb(st, "oml", [128, 2, 4])
                ngf = sb(st, "ngf", [128, 4])
                P.op("sp", lambda e: e.dma_start(out=lbt[:, 0], in_=lb_table[0].rearrange("d (h p) -> p d h", p=128)),
                     writes=[r_lb], dma=True)
                P.op("sp", lambda e: e.dma_start(out=lbt[:, 1], in_=lb_table[1].rearrange("d (h p) -> p d h", p=128)),
                     writes=[r_lb], dma=True)
                P.op("sp", lambda e: e.dma_start(out=ngf[:], in_=hg_norm_g.rearrange("h p -> p h")),
                     writes=[r_lb], dma=True)
                P.op("dve", lambda e: e.tensor_tensor(out=lbv[:], in0=lbt[:, 0], in1=lbt[:, 1], op=ALU.subtract),
                     reads=[r_lb], writes=[r_lb])
                P.op("act", lambda e: e.activation(out=lbv[:], in_=lbv[:], func=AF.Sigmoid), reads=[r_lb], writes=[r_lb])
                P.op("dve", lambda e: e.tensor_scalar(out=oml[:], in0=lbv[:], scalar1=-1.0, scalar2=1.0,
                                                       op0=ALU.mult, op1=ALU.add), reads=[r_lb], writes=[r_lb])
                H = 2048
                ones_h = sb(st, "ones_h", [128, H]); r_onesh = Res()
                P.op("pool", lambda e: e.memset(ones_h[:], 1.0), writes=[r_onesh])
                mask_f = sb(st, "mask_f", [128, 128]); mask_b = sb(st, "mask_b", [128, 128]); r_mask = Res()
                for mk, sgn in ((mask_f, 1), (mask_b, -1)):
                    P.op("pool", lambda e, mk=mk: e.memset(mk[:], 1.0), writes=[r_mask])
                    P.op("pool", lambda e, mk=mk, sgn=sgn: e.affine_select(
                        out=mk[:], in_=mk[:], pattern=[[sgn, 128]], compare_op=ALU.is_ge, fill=0.0, base=0,
                        channel_multiplier=-sgn), reads=[r_mask], writes=[r_mask])
                    P.op("pool", lambda e, mk=mk: e.memset(mk[0:64, 64:128], 0.0), reads=[r_mask], writes=[r_mask])
                    P.op("pool", lambda e, mk=mk: e.memset(mk[64:128, 0:64], 0.0), reads=[r_mask], writes=[r_mask])

                T1 = sb(st, "T1", [128, H]); T2 = sb(st, "T2", [128, H]); T3 = sb(st, "T3", [128, H])
                TQ = sb(st, "TQ", [128, H])
                r_T1, r_T2, r_T3, r_TQ = Res(), Res(), Res(), Res()
                Bext = sb(st, "Bext", [128, S + 1]); r_B = Res()
                qt = [sb(st, "qt%d" % d, [128, S], BF16) for d in range(2)]
                kt = [sb(st, "kt%d" % d, [128, S], BF16) for d in range(2)]
                ktok = [sb(st, "ktok%d" % d, [128, NT, 128], BF16) for d in range(2)]
                dec = [sb(st, "dec%d" % d, [128, 64]) for d in range(2)]
                r_qk = [Res(), Res()]
                r_ktok = [Res(), Res()]
                vtok = sb(st, "vtok", [128, NT, 128], BF16); r_vt = Res()
                o_h = sb(st, "o_h", [128, S]); r_oh = [Res() for _ in range(NT)]
                Sf = [sb(st, "Sf%d" % d, [128, 128]) for d in range(2)]
                Sb = [sb(st, "Sb%d" % d, [128, 128], BF16) for d in range(2)]
                Stmp = [sb(st, "Stmp%d" % d, [128, 128]) for d in range(2)]
                r_S = [Res(), Res()]
                r_Sf = [Res(), Res()]
                r_Stmp = [Res(), Res()]
                sT = [sb(st, "sT%d" % d, [128, 128], BF16) for d in range(2)]; r_sT = [Res(), Res()]
                p_sc = [ps(st, "p_sc%d" % d, [128, 512])[:, 0:128] for d in range(2)]; r_psc = [Res(), Res()]
                p_o = [ps(st, "p_o%d" % d, [128, 512])[:, 0:128] for d in range(2)]; r_po = [Res(), Res()]
                p_P = [ps(st, "p_P%d" % d, [128, 512])[:, 0:128] for d in range(2)]; r_pP = [Res(), Res()]
                p_kt = ps(st, "p_kt", [128, 8, 128], BF16); r_pkt = Res()
                p_st = ps(st, "p_st", [128, 512]); r_pst = Res()

                for h in range(4):
                    P.op("sp", lambda e, h=h: e.dma_start(
                        out=vtok[:], in_=vtok_d[:, h * 128:(h + 1) * 128].rearrange("(i p) v -> p i v", p=128)),
                        reads=r_vtok, writes=[r_vt], dma=True)
                    P.op("pool", lambda e: e.memset(o_h[:], 0.0), writes=r_oh)
                    for d in range(2):
                        zrow = 512 + d * 512 + h * 128
                        B3 = Bext[:, 1:S + 1].rearrange("p (c j) -> p c j", j=64)
                        B0 = Bext[:, 0:S].rearrange("p (c j) -> p c j", j=64)
                        P.op("pool", lambda e: e.memset(Bext[:, 0:1], 0.0), writes=[r_B])
                        for hf in range(2):
                            c0 = hf * H
                            P.op("sp", lambda e, zrow=zrow, c0=c0: e.dma_start(
                                out=T1[:], in_=projT_d[zrow:zrow + 128, c0:c0 + H]),
                                reads=[r_projT[zrow]], writes=[r_T1], dma=True)
                            P.op("act", lambda e: e.activation(out=T1[:], in_=T1[:], func=AF.Sigmoid),
                                 reads=[r_T1], writes=[r_T1])
                            P.op("dve", lambda e, d=d, h=h: e.tensor_scalar(
                                out=T1[:], in0=T1[:], scalar1=oml[:, d, h:h + 1], scalar2=lbv[:, d, h:h + 1],
                                op0=ALU.mult, op1=ALU.add), reads=[r_T1, r_lb], writes=[r_T1])
                            P.op("act", lambda e: e.activation(out=T2[:], in_=T1[:], func=AF.Ln),
                                 reads=[r_T1], writes=[r_T2])
                            P.op("dve", lambda e, c0=c0: e.tensor_tensor_scan(
                                out=Bext[:, 1 + c0:1 + c0 + H], data0=ones_h[:], data1=T2[:],
                                initial=Bext[:, c0:c0 + 1], op0=ALU.mult, op1=ALU.add),
                                reads=[r_T2, r_onesh, r_B], writes=[r_B])
                            P.op("pool", lambda e: e.tensor_scalar(out=T1[:], in0=T1[:], scalar1=-1.0, scalar2=1.0,
                                                                   op0=ALU.mult, op1=ALU.add),
                                 reads=[r_T1], writes=[r_T1])
                            P.op("act", lambda e, d=d, c0=c0: e.copy(out=kt[d][:, c0:c0 + H], in_=T1[:]),
                                 reads=[r_T1], writes=[r_qk[d]])
                        for hf in range(2):
                            c0 = hf * H
                            cs = slice(hf * 32, (hf + 1) * 32)
                            T2v = T2[:].rearrange("p (c j) -> p c j", j=64)
                            if d == 0:
                                P.op("dve", lambda e, cs=cs: e.tensor_tensor(
                                    out=T2v, in0=B3[:, cs, :], in1=bc(B0[:, cs, 0:1], [128, 32, 64]),
                                    op=ALU.subtract), reads=[r_B], writes=[r_T2])
                            else:
                                P.op("dve", lambda e, cs=cs: e.tensor_tensor(
                                    out=T2v, in0=bc(B3[:, cs, 63:64], [128, 32, 64]), in1=B0[:, cs, :],
                                    op=ALU.subtract), reads=[r_B], writes=[r_T2])
                            P.op("act", lambda e: e.activation(out=T3[:], in_=T2[:], func=AF.Exp),
                                 reads=[r_T2], writes=[r_T3])
                            T3v = T3[:].rearrange("p (c j) -> p c j", j=64)
                            jj = 63 if d == 0 else 0
                            P.op("pool", lambda e, d=d, cs=cs, jj=jj: e.tensor_copy(
                                out=dec[d][:, cs].unsqueeze(2), in_=T3v[:, :, jj:jj + 1]),
                                reads=[r_T3], writes=[r_qk[d]])
                            qrow = h * 128
                            P.op("sp", lambda e, qrow=qrow, c0=c0: e.dma_start(
                                out=TQ[:], in_=projT_d[qrow:qrow + 128, c0:c0 + H]),
                                reads=[r_projT[qrow]], writes=[r_TQ], dma=True)
                            P.op("dve", lambda e, d=d, c0=c0: e.tensor_tensor(
                                out=qt[d][:, c0:c0 + H], in0=TQ[:], in1=T3[:], op=ALU.mult),
                                reads=[r_TQ, r_T3], writes=[r_qk[d]])
                            P.op("act", lambda e: e.activation(out=T3[:], in_=T2[:], func=AF.Exp, scale=-1.0),
                                 reads=[r_T2], writes=[r_T3])
                            P.op("pool", lambda e, d=d, c0=c0: e.tensor_tensor(
                                out=kt[d][:, c0:c0 + H], in0=kt[d][:, c0:c0 + H], in1=T3[:], op=ALU.mult),
                                reads=[r_T3, r_qk[d]], writes=[r_qk[d]])
                        for g8 in range(4):
                            for j in range(8):
                                i = g8 * 8 + j
                                P.op("pe", lambda e, d=d, i=i, j=j: e.transpose(
                                    out=p_kt[:, j, :], in_=kt[d][:, i * 128:(i + 1) * 128], identity=ident_b[:]),
                                    reads=[r_qk[d], r_const], writes=[r_pkt])
                            P.op("act", lambda e, d=d, g8=g8: e.copy(out=ktok[d][:, g8 * 8:(g8 + 1) * 8, :],
                                                                     in_=p_kt[:]),
                                 reads=[r_pkt], writes=[r_ktok[d]])
                        P.op("pool", lambda e, d=d: e.memset(Sf[d][:], 0.0), writes=[r_Sf[d]])
                        P.op("pool", lambda e, d=d: e.memset(Sb[d][:], 0.0), writes=[r_S[d]])

                    for step in range(NT):
                        for d in range(2):
                            i = step if d == 0 else NT - 1 - step
                            t0 = i * 128
                            mk = mask_f if d == 0 else mask_b
                            P.op("pe", lambda e, d=d, t0=t0: e.matmul(
                                p_sc[d][:], lhsT=kt[d][:, t0:t0 + 128], rhs=qt[d][:, t0:t0 + 128],
                                start=True, stop=True), reads=[r_qk[d]], writes=[r_psc[d]])
                            P.op("dve", lambda e, d=d, mk=mk: e.tensor_tensor(
                                out=sT[d][:], in0=p_sc[d][:], in1=mk[:], op=ALU.mult),
                                reads=[r_psc[d], r_mask], writes=[r_sT[d]])
                            P.op("pe", lambda e, d=d, i=i: e.matmul(
                                p_o[d][:], lhsT=vtok[:, i, :], rhs=sT[d][:], start=True, stop=False),
                                reads=[r_vt, r_sT[d]], writes=[r_po[d]])
                            order = (0, 1) if d == 0 else (1, 0)
                            for n_, half in enumerate(order):
                                c = i * 2 + half
                                hs = slice(half * 64, half * 64 + 64)
                                P.op("pe", lambda e, d=d, t0=t0, hs=hs, n_=n_: e.matmul(
                                    p_o[d][:, hs], lhsT=Sb[d][:], rhs=qt[d][:, t0 + hs.start:t0 + hs.stop],
                                    start=False, stop=(n_ == 1)),
                                    reads=[r_S[d], r_qk[d]], writes=[r_po[d]])
                                P.op("pe", lambda e, d=d, i=i, hs=hs: e.matmul(
                                    p_P[d][:], lhsT=ktok[d][hs, i, :], rhs=vtok[hs, i, :], start=True, stop=True),
                                    reads=[r_ktok[d], r_vt], writes=[r_pP[d]])
                                P.op("dve", lambda e, d=d: e.tensor_tensor(
                                    out=Stmp[d][:], in0=p_P[d][:], in1=Sf[d][:], op=ALU.add),
                                    reads=[r_pP[d], r_Sf[d]], writes=[r_Stmp[d]])
                                P.op("act", lambda e, d=d, c=c: e.activation(
                                    out=Sb[d][:], in_=Stmp[d][:], func=AF.Copy, scale=dec[d][:, c:c + 1]),
                                    reads=[r_Stmp[d], r_qk[d]], writes=[r_S[d]])
                                P.op("dve", lambda e, d=d, c=c: e.tensor_scalar_mul(
                                    out=Sf[d][:], in0=Stmp[d][:], scalar1=dec[d][:, c:c + 1]),
                                    reads=[r_Stmp[d], r_qk[d]], writes=[r_Sf[d]])
                            P.op("dve", lambda e, d=d, t0=t0: e.tensor_tensor(
                                out=o_h[:, t0:t0 + 128], in0=p_o[d][:], in1=o_h[:, t0:t0 + 128], op=ALU.add),
                                reads=[r_po[d], r_oh[i]], writes=[r_oh[i]])

                    for tc in range(NCH):
                        cs = slice(tc * 512, (tc + 1) * 512)
                        w0 = (tc % 4) * 512
                        P.op("act", lambda e, cs=cs, w0=w0: e.activation(out=T1[:, w0:w0 + 512], in_=o_h[:, cs],
                                                                         func=AF.Square),
                             reads=r_oh[tc * 4:(tc + 1) * 4], writes=[r_T1])
                        P.op("pe", lambda e, w0=w0: e.matmul(p_st[:], lhsT=ones_f[:], rhs=T1[:, w0:w0 + 512],
                                                            start=True, stop=True),
                             reads=[r_T1, r_ones], writes=[r_pst])
                        P.op("act", lambda e, w0=w0: e.activation(out=T2[:, w0:w0 + 512], in_=p_st[:], func=AF.Sqrt,
                                                                  scale=1.0 / 128, bias=epsb[:]),
                             reads=[r_pst, r_const], writes=[r_T2])
                        P.op("dve", lambda e, w0=w0: e.reciprocal(out=T2[:, w0:w0 + 512], in_=T2[:, w0:w0 + 512]),
                             reads=[r_T2], writes=[r_T2])
                        P.op("dve", lambda e, cs=cs, w0=w0: e.tensor_tensor(
                            out=T2[:, w0:w0 + 512], in0=T2[:, w0:w0 + 512], in1=o_h[:, cs], op=ALU.mult),
                            reads=[r_T2] + r_oh[tc * 4:(tc + 1) * 4], writes=[r_T2])
                        grow = 2048 + h * 128
                        P.op("sp", lambda e, grow=grow, cs=cs, w0=w0: e.dma_start(
                            out=T3[:, w0:w0 + 512], in_=projT_d[grow:grow + 128, cs]),
                            reads=[r_projT[grow]], writes=[r_T3], dma=True)
                        P.op("act", lambda e, w0=w0: e.activation(out=T3[:, w0:w0 + 512], in_=T3[:, w0:w0 + 512],
                                                                  func=AF.Silu), reads=[r_T3], writes=[r_T3])
                        P.op("dve", lambda e, h=h, cs=cs, w0=w0: e.scalar_tensor_tensor(
                            out=hgT[:, h, cs], in0=T2[:, w0:w0 + 512], scalar=ngf[:, h:h + 1],
                            in1=T3[:, w0:w0 + 512], op0=ALU.mult, op1=ALU.mult),
                            reads=[r_T2, r_T3, r_lb], writes=[r_hgT[h]])
                P.flush()

            cvT = sb(stA, "cvT", [128, 4, S], BF16)
            r_cvT = [Res() for _ in range(4)]
            with ExitStack() as st:
                dww = sb(st, "dww", [128, 4, 31]); dwb = sb(st, "dwb", [128, 4])
                lng = sb(st, "lng", [128, 4]); lnb = sb(st, "lnb", [128, 4]); r_cp = Res()
                for cc in range(4):
                    P.op("sp", lambda e, cc=cc: e.dma_start(out=dww[:, cc, :],
                                                            in_=dw_w[:, cc * 128:(cc + 1) * 128].rearrange("j p -> p j")),
                         writes=[r_cp], dma=True)
                for t_, src_ in ((dwb, dw_b), (lng, cv_ln_g), (lnb, cv_ln_b)):
                    P.op("sp", lambda e, t_=t_, src_=src_: e.dma_start(out=t_[:], in_=src_.rearrange("(c p) -> p c", p=128)),
                         writes=[r_cp], dma=True)
                ub = sb(st, "ub", [128, 4, S + 30], BF16); r_ub = [Res() for _ in range(4)]
                for cc in range(4):
                    P.op("pool", lambda e, cc=cc: e.memset(ub[:, cc, 0:15], 0.0), writes=[r_ub[cc]])
                    P.op("pool", lambda e, cc=cc: e.memset(ub[:, cc, S + 15:S + 30], 0.0), writes=[r_ub[cc]])
                HH = 2048
                with ExitStack() as st2:
                    vt = [sb(st2, "vt%d" % i, [128, HH]) for i in range(2)]; r_vt4 = [Res(), Res()]
                    gt = [sb(st2, "gt%d" % i, [128, HH]) for i in range(2)]; r_gt4 = [Res(), Res()]
                    n_it = 0
                    for cc in range(4):
                        vrow = 2560 + cc * 128
                        grow = 3072 + cc * 128
                        for hf in range(2):
                            b2 = n_it % 2; n_it += 1
                            c0 = hf * HH
                            P.op("sp", lambda e, vrow=vrow, c0=c0, b2=b2: e.dma_start(
                                out=vt[b2][:], in_=projT_d[vrow:vrow + 128, c0:c0 + HH]),
                                reads=[r_projT[vrow]], writes=[r_vt4[b2]], dma=True)
                            P.op("sp", lambda e, grow=grow, c0=c0, b2=b2: e.dma_start(
                                out=gt[b2][:], in_=projT_d[grow:grow + 128, c0:c0 + HH]),
                                reads=[r_projT[grow]], writes=[r_gt4[b2]], dma=True)
                            P.op("act", lambda e, b2=b2: e.activation(out=gt[b2][:], in_=gt[b2][:], func=AF.Sigmoid),
                                 reads=[r_gt4[b2]], writes=[r_gt4[b2]])
                            P.op("dve", lambda e, cc=cc, c0=c0, b2=b2: e.tensor_tensor(
                                out=ub[:, cc, 15 + c0:15 + c0 + HH], in0=vt[b2][:], in1=gt[b2][:], op=ALU.mult),
                                reads=[r_vt4[b2], r_gt4[b2]], writes=[r_ub[cc]])
                    P.flush()
                dg = sb(st, "dg", [128, 4, 31, 128], BF16); r_dg = [Res() for _ in range(4)]
                n_it = 0
                for cc in range(4):
                    for j in range(31):
                        if n_it % 2 == 0:
                            P.op("dve", lambda e, cc=cc, j=j: e.tensor_scalar_mul(
                                out=dg[:, cc, j, :], in0=ident_f[:], scalar1=dww[:, cc, j:j + 1]),
                                reads=[r_ident, r_cp], writes=[r_dg[cc]])
                        else:
                            P.op("act", lambda e, cc=cc, j=j: e.activation(
                                out=dg[:, cc, j, :], in_=ident_f[:], func=AF.Copy, scale=dww[:, cc, j:j + 1]),
                                reads=[r_ident, r_cp], writes=[r_dg[cc]])
                        n_it += 1
                ones_b = sb(st, "ones_b", [128, 128], BF16); r_ob = Res()
                P.op("dve", lambda e: e.tensor_copy(out=ones_b[:], in_=ones_f[:]), reads=[r_ones], writes=[r_ob])
                uc = [sb(st, "uc%d" % i, [128, 4, 512], BF16) for i in range(2)]; r_uc = [[Res() for _ in range(4)] for _ in range(2)]
                usq = sb(st, "usq", [128, 4, 512], BF16); r_usq = Res()
                p_cv = [ps(st, "p_cv%d" % i, [128, 512]) for i in range(4)]; r_pcv = [Res() for _ in range(4)]
                p_s1 = ps(st, "p_s1", [128, 512]); p_s2 = ps(st, "p_s2", [128, 512]); r_ps1 = Res(); r_ps2 = Res()
                mean = sb(st, "mean", [128, 512]); msq = sb(st, "msq", [128, 512]); rstd = sb(st, "rstd", [128, 512])
                r_mean = Res(); r_rstd = Res()
                tt = [sb(st, "tt%d" % i, [128, 512]) for i in range(2)]; r_tt = [Res(), Res()]
                for tc in range(NCH):
                    cs = slice(tc * 512, (tc + 1) * 512)
                    ub_ = tc % 2
                    for cc in range(4):
                        for j in range(31):
                            P.op("pe", lambda e, cc=cc, j=j, tc=tc: e.matmul(
                                p_cv[cc][:], lhsT=dg[:, cc, j, :], rhs=ub[:, cc, tc * 512 + j:tc * 512 + j + 512],
                                start=(j == 0), stop=(j == 30)), reads=[r_dg[cc], r_ub[cc]], writes=[r_pcv[cc]])
                        P.op("act", lambda e, cc=cc, ub_=ub_: e.activation(
                            out=uc[ub_][:, cc, :], in_=p_cv[cc][:], func=AF.Identity, bias=dwb[:, cc:cc + 1]),
                            reads=[r_pcv[cc], r_cp], writes=[r_uc[ub_][cc]])
                    for cc in range(4):
                        P.op("act", lambda e, cc=cc, ub_=ub_: e.activation(out=usq[:, cc, :], in_=uc[ub_][:, cc, :],
                                                                          func=AF.Square),
                             reads=[r_uc[ub_][cc]], writes=[r_usq])
                    for cc in range(4):
                        P.op("pe", lambda e, cc=cc, ub_=ub_: e.matmul(p_s1[:], lhsT=ones_b[:], rhs=uc[ub_][:, cc, :],
                                                                      start=(cc == 0), stop=(cc == 3)),
                             reads=[r_uc[ub_][cc], r_ob], writes=[r_ps1])
                    for cc in range(4):
                        P.op("pe", lambda e, cc=cc: e.matmul(p_s2[:], lhsT=ones_b[:], rhs=usq[:, cc, :],
                                                             start=(cc == 0), stop=(cc == 3)),
                             reads=[r_usq, r_ob], writes=[r_ps2])
                    P.op("act", lambda e: e.activation(out=mean[:], in_=p_s1[:], func=AF.Copy, scale=1.0 / 512),
                         reads=[r_ps1], writes=[r_mean])
                    P.op("dve", lambda e: e.tensor_tensor(out=msq[:], in0=mean[:], in1=mean[:], op=ALU.mult),
                         reads=[r_mean], writes=[r_rstd])
                    P.op("dve", lambda e: e.scalar_tensor_tensor(out=rstd[:], in0=p_s2[:], scalar=1.0 / 512, in1=msq[:],
                                                                 op0=ALU.mult, op1=ALU.subtract),
                         reads=[r_ps2, r_rstd], writes=[r_rstd])
                    P.op("dve", lambda e: e.tensor_scalar_max(out=rstd[:], in0=rstd[:], scalar1=0.0),
                         reads=[r_rstd], writes=[r_rstd])
                    P.op("act", lambda e: e.activation(out=rstd[:], in_=rstd[:], func=AF.Sqrt, bias=epsb[:]),
                         reads=[r_rstd, r_const], writes=[r_rstd])
                    P.op("dve", lambda e: e.reciprocal(out=rstd[:], in_=rstd[:]), reads=[r_rstd], writes=[r_rstd])
                    for cc in range(4):
                        b2 = cc % 2
                        P.op("dve", lambda e, cc=cc, ub_=ub_, b2=b2: e.tensor_tensor(
                            out=tt[b2][:], in0=uc[ub_][:, cc, :], in1=mean[:], op=ALU.subtract),
                            reads=[r_uc[ub_][cc], r_mean], writes=[r_tt[b2]])
                        P.op("dve", lambda e, b2=b2: e.tensor_tensor(out=tt[b2][:], in0=tt[b2][:], in1=rstd[:],
                                                                     op=ALU.mult),
                             reads=[r_tt[b2], r_rstd], writes=[r_tt[b2]])
                        P.op("act", lambda e, cc=cc, cs=cs, b2=b2: e.activation(
                            out=cvT[:, cc, cs], in_=tt[b2][:], func=AF.Silu, scale=lng[:, cc:cc + 1],
                            bias=lnb[:, cc:cc + 1]), reads=[r_tt[b2], r_cp], writes=[r_cvT[cc]])
                P.flush()

            with ExitStack() as st:
                wohg = sb(st, "wohg", [128, 4, D], BF16); wocv = sb(st, "wocv", [128, 4, D], BF16)
                wo = sb(st, "wo", [128, 8, D], BF16); r_w5 = Res()
                rw = sb(st, "rw", [128, 8, E]); rbb = sb(st, "rbb", [128, E]); bocv = sb(st, "bocv", [128, 8])
                g1b = sb(st, "g1b", [128, D]); a2b = sb(st, "a2b", [128, D]); sh2b = sb(st, "sh2b", [128, D])
                r_bt = Res()
                P.op("pool", lambda e: e.dma_start(out=wohg[:], in_=w_o_hg.rearrange("(c p) n -> p c n", p=128)),
                     writes=[r_w5], dma=True)
                P.op("pool", lambda e: e.dma_start(out=wocv[:], in_=w_o_cv.rearrange("(c p) n -> p c n", p=128)),
                     writes=[r_w5], dma=True)
                P.op("pool", lambda e: e.dma_start(out=wo[:], in_=w_out.rearrange("(c p) n -> p c n", p=128)),
                     writes=[r_w5], dma=True)
                P.op("sp", lambda e: e.dma_start(out=rw[:], in_=router_w.rearrange("(c p) n -> p c n", p=128)),
                     writes=[r_w5], dma=True)
                P.op("sp", lambda e: e.dma_start(out=rbb[:], in_=router_b.partition_broadcast(128)),
                     writes=[r_w5], dma=True)
                P.op("sp", lambda e: e.dma_start(out=bocv[:], in_=b_o_cv.rearrange("(c p) -> p c", p=128)),
                     writes=[r_w5], dma=True)
                P.op("sp", lambda e: e.dma_start(out=g1b[:], in_=mod_d[2, :].partition_broadcast(128)),
                     reads=[r_mod], writes=[r_bt], dma=True)
                P.op("sp", lambda e: e.dma_start(out=sh2b[:], in_=mod_d[3, :].partition_broadcast(128)),
                     reads=[r_mod], writes=[r_bt], dma=True)
                P.op("sp", lambda e: e.dma_start(out=a2b[:], in_=mod_d[4, :].partition_broadcast(128)),
                     reads=[r_mod], writes=[r_bt], dma=True)
                P.op("sp", lambda e: e.dma_start(out=g2b[:], in_=mod_d[5, :].partition_broadcast(128)),
                     reads=[r_mod], writes=[r_g2b], dma=True)
                P.op("sp", lambda e: e.dma_start(out=fgb[:], in_=fin_g.partition_broadcast(128)),
                     writes=[r_fgb], dma=True)

                gh = [sb(st, "gh%d" % i, [128, 512]) for i in range(2)]; r_gh = [Res(), Res()]
                gc = [sb(st, "gc%d" % i, [128, 512]) for i in range(2)]; r_gc = [Res(), Res()]
                mT = sb(st, "mT", [128, 8, 512], BF16); r_mT = Res()
                m1 = [sb(st, "m1_%d" % i, [128, 512]) for i in range(2)]
                m2 = [sb(st, "m2_%d" % i, [128, 512]) for i in range(2)]
                r_m1 = [Res(), Res()]; r_m2 = [Res(), Res()]
                p_yh = [ps(st, "p_yh%d" % i, [128, 512]) for i in range(2)]; r_pyh = [Res(), Res()]
                p_yc = [ps(st, "p_yc%d" % i, [128, 512]) for i in range(2)]; r_pyc = [Res(), Res()]
                p_o5 = [ps(st, "p_o5%d" % i, [128, 512]) for i in range(2)]; r_po5 = [Res(), Res()]
                p_tr = ps(st, "p_tr", [128, 8, 128], BF16); r_ptr5 = Res()
                p_lg = ps(st, "p_lg", [128, 16, E]); r_plg = Res()
                xr = [sb(st, "xr%d" % i, [128, D]) for i in range(2)]; r_xr = [Res(), Res()]
                hr = [sb(st, "hr%d" % i, [128, D]) for i in range(2)]; r_hr = [Res(), Res()]
                h2f = [sb(st, "h2f%d" % i, [128, D]) for i in range(2)]; r_h2f = [Res(), Res()]
                P.op("sp", lambda e: e.dma_start(out=h2f[0][:], in_=norm2_g.partition_broadcast(128)),
                     writes=[r_h2f[0]], dma=True)
                P.op("dve", lambda e: e.scalar_tensor_tensor(out=a2b[:], in0=a2b[:], scalar=1.0, in1=h2f[0][:],
                                                             op0=ALU.add, op1=ALU.mult), reads=[r_bt, r_h2f[0]], writes=[r_bt])
                h2b = [sb(st, "h2b%d" % i, [128, D], BF16) for i in range(2)]; r_h2b = [Res(), Res()]
                junk = sb(st, "junk", [128, D], BF16); r_junk = Res()
                ss2 = sb(st, "ss2", [128, NT]); r_ss2 = [Res() for _ in range(NT)]
                h2Th = sb(st, "h2Th", [128, 8, 128], BF16); r_h2Th = Res()
                h2Tl = sb(st, "h2Tl", [128, 8, 128], BF16); r_h2Tl = Res()
                h2l = [sb(st, "h2l%d" % i, [128, D], BF16) for i in range(2)]; r_h2l = [Res(), Res()]
                rwh = sb(st, "rwh", [128, 8, E], BF16); rwl = sb(st, "rwl", [128, 8, E], BF16)
                P.op("dve", lambda e: e.tensor_copy(out=rwh[:], in_=rw[:]), reads=[r_w5], writes=[r_w5])
                P.op("dve", lambda e: e.tensor_tensor(out=rwl[:], in0=rw[:], in1=rwh[:], op=ALU.subtract),
                     reads=[r_w5], writes=[r_w5])
                lg_all = sb(st, "lg_all", [128, NT, E]); r_lgall = [Res() for _ in range(NT)]
                m8a = sb(st, "m8a", [128, NT, 8]); r_m8a = Res()
                den_all = sb(st, "den_all", [128, NT]); r_den = Res(); r_gwall = Res()
                P.op("pool", lambda e: e.memset(junk[:], 0.0), writes=[r_junk])
                P.op("sp", lambda e: e.dma_start(out=h2_d[S:S + 128, :], in_=junk[:]), reads=[r_junk], writes=[r_h2d[NT]],
                     dma=True)
                pending_b = []
                for tc in range(NCH):
                    cs = slice(tc * 512, (tc + 1) * 512)
                    for dch in range(8):
                        b2 = dch % 2
                        rh = 3584 + dch * 128
                        rc = 4608 + dch * 128
                        P.op("sp", lambda e, cs=cs, rh=rh, b2=b2: e.dma_start(out=gh[b2][:], in_=projT_d[rh:rh + 128, cs]),
                             reads=[r_projT[rh]], writes=[r_gh[b2]], dma=True)
                        P.op("sp", lambda e, cs=cs, rc=rc, b2=b2: e.dma_start(out=gc[b2][:], in_=projT_d[rc:rc + 128, cs]),
                             reads=[r_projT[rc]], writes=[r_gc[b2]], dma=True)
                        P.op("act", lambda e, b2=b2: e.activation(out=gh[b2][:], in_=gh[b2][:], func=AF.Sigmoid),
                             reads=[r_gh[b2]], writes=[r_gh[b2]])
                        P.op("act", lambda e, b2=b2: e.activation(out=gc[b2][:], in_=gc[b2][:], func=AF.Sigmoid),
                             reads=[r_gc[b2]], writes=[r_gc[b2]])
                        for kc in range(4):
                            P.op("pe", lambda e, dch=dch, kc=kc, b2=b2, cs=cs: e.matmul(
                                p_yh[b2][:], lhsT=wohg[:, kc, dch * 128:(dch + 1) * 128], rhs=hgT[:, kc, cs],
                                start=(kc == 0), stop=(kc == 3)), reads=[r_w5, r_hgT[kc]], writes=[r_pyh[b2]])
                        for kc in range(4):
                            P.op("pe", lambda e, dch=dch, kc=kc, b2=b2, cs=cs: e.matmul(
                                p_yc[b2][:], lhsT=wocv[:, kc, dch * 128:(dch + 1) * 128], rhs=cvT[:, kc, cs],
                                start=(kc == 0), stop=(kc == 3)), reads=[r_w5, r_cvT[kc]], writes=[r_pyc[b2]])
                        P.op("dve", lambda e, dch=dch, b2=b2: e.tensor_tensor(
                            out=m1[b2][:], in0=p_yh[b2][:], in1=gh[b2][:], op=ALU.mult),
                            reads=[r_pyh[b2], r_gh[b2]], writes=[r_m1[b2]])
                        P.op("dve", lambda e, dch=dch, b2=b2: e.scalar_tensor_tensor(
                            out=m2[b2][:], in0=p_yc[b2][:], scalar=bocv[:, dch:dch + 1], in1=gc[b2][:],
                            op0=ALU.add, op1=ALU.mult), reads=[r_pyc[b2], r_gc[b2], r_w5], writes=[r_m2[b2]])
                        P.op("dve", lambda e, dch=dch, b2=b2: e.tensor_tensor(
                            out=mT[:, dch, :], in0=m1[b2][:], in1=m2[b2][:], op=ALU.add),
                            reads=[r_m1[b2], r_m2[b2]], writes=[r_mT])
                    for q in range(4):
                        i = tc * 4 + q
                        b2 = i % 2
                        P.op("sp", lambda e, i=i, b2=b2: e.dma_start(out=xr[b2][:], in_=x[i * 128:(i + 1) * 128, :]),
                             writes=[r_xr[b2]], dma=True)
                        for dh in range(2):
                            for kc in range(8):
                                P.op("pe", lambda e, q=q, dh=dh, kc=kc: e.matmul(
                                    p_o5[dh][:], lhsT=mT[:, kc, q * 128:(q + 1) * 128],
                                    rhs=wo[:, kc, dh * 512:(dh + 1) * 512], start=(kc == 0), stop=(kc == 7)),
                                    reads=[r_mT, r_w5], writes=[r_po5[dh]])
                            ds_ = slice(dh * 512, (dh + 1) * 512)
                            P.op("dve", lambda e, dh=dh, ds_=ds_, b2=b2: e.tensor_tensor(
                                out=hr[b2][:, ds_], in0=p_o5[dh][:], in1=g1b[:, ds_], op=ALU.mult),
                                reads=[r_po5[dh], r_bt], writes=[r_hr[b2]])
                        P.op("dve", lambda e, b2=b2: e.tensor_tensor(out=hr[b2][:], in0=hr[b2][:], in1=xr[b2][:],
                                                                     op=ALU.add),
                             reads=[r_hr[b2], r_xr[b2]], writes=[r_hr[b2]])
                        P.op("sp", lambda e, i=i, b2=b2: e.dma_start(out=hres_d[i * 128:(i + 1) * 128, :], in_=hr[b2][:]),
                             reads=[r_hr[b2]], writes=[r_hres[i]], dma=True)
                        P.op("act", lambda e, i=i, b2=b2: e.activation(out=junk[:], in_=hr[b2][:], func=AF.Square,
                                                                        accum_out=ss2[:, i:i + 1]),
                             reads=[r_hr[b2]], writes=[r_junk, r_ss2[i]])
                        P.op("act", lambda e, i=i: e.activation(out=ss2[:, i:i + 1], in_=ss2[:, i:i + 1], func=AF.Sqrt,
                                                                scale=1.0 / D, bias=epsb[:]),
                             reads=[r_ss2[i], r_const], writes=[r_ss2[i]])
                        P.op("dve", lambda e, i=i: e.reciprocal(out=ss2[:, i:i + 1], in_=ss2[:, i:i + 1]),
                             reads=[r_ss2[i]], writes=[r_ss2[i]])
                        P.op("dve", lambda e, i=i, b2=b2: e.scalar_tensor_tensor(
                            out=h2f[b2][:], in0=hr[b2][:], scalar=ss2[:, i:i + 1], in1=a2b[:],
                            op0=ALU.mult, op1=ALU.mult), reads=[r_hr[b2], r_ss2[i], r_bt], writes=[r_h2f[b2]])
                        P.op("dve", lambda e, b2=b2: e.tensor_tensor(out=h2f[b2][:], in0=h2f[b2][:], in1=sh2b[:],
                                                                     op=ALU.add),
                             reads=[r_h2f[b2], r_bt], writes=[r_h2f[b2]])
                        P.op("act", lambda e, b2=b2: e.copy(out=h2b[b2][:], in_=h2f[b2][:]),
                             reads=[r_h2f[b2]], writes=[r_h2b[b2]])
                        P.op("sp", lambda e, i=i, b2=b2: e.dma_start(out=h2_d[i * 128:(i + 1) * 128, :], in_=h2b[b2][:]),
                             reads=[r_h2b[b2]], writes=[r_h2d[i]], dma=True)
                        def stage_b(i=i, b2=b2):
                            P.op("dve", lambda e, b2=b2: e.tensor_tensor(out=h2l[b2][:], in0=h2f[b2][:], in1=h2b[b2][:],
                                                                          op=ALU.subtract),
                                 reads=[r_h2f[b2], r_h2b[b2]], writes=[r_h2l[b2]])
                            for part, (srcT, r_src, dstT, r_dst) in enumerate(((h2b[b2], r_h2b[b2], h2Th, r_h2Th),
                                                                              (h2l[b2], r_h2l[b2], h2Tl, r_h2Tl))):
                                for kc in range(8):
                                    P.op("pe", lambda e, kc=kc, srcT=srcT: e.transpose(
                                        out=p_tr[:, kc, :], in_=srcT[:, kc * 128:(kc + 1) * 128], identity=ident_b[:]),
                                        reads=[r_src, r_const], writes=[r_ptr5])
                                if part == 0:
                                    P.op("act", lambda e, dstT=dstT: e.copy(out=dstT[:], in_=p_tr[:]),
                                         reads=[r_ptr5], writes=[r_dst])
                                else:
                                    P.op("dve", lambda e, dstT=dstT: e.tensor_copy(out=dstT[:], in_=p_tr[:]),
                                         reads=[r_ptr5], writes=[r_dst])
                            n_acc = 0
                            for (aT, r_a, wpart) in ((h2Th, r_h2Th, rwh), (h2Tl, r_h2Tl, rwh), (h2Th, r_h2Th, rwl)):
                                for kc in range(8):
                                    P.op("pe", lambda e, kc=kc, aT=aT, wpart=wpart, n_acc=n_acc: e.matmul(
                                        p_lg[:, 0, :], lhsT=aT[:, kc, :], rhs=wpart[:, kc, :],
                                        start=(n_acc == 0), stop=(n_acc == 23)),
                                        reads=[r_a, r_w5], writes=[r_plg])
                                    n_acc += 1
                            P.op("dve", lambda e, i=i: e.tensor_tensor(out=lg_all[:, i, :], in0=p_lg[:, 0, :], in1=rbb[:],
                                                                        op=ALU.add),
                                 reads=[r_plg, r_w5], writes=[r_lgall[i]])

                        if pending_b:
                            pending_b.pop()()
                        pending_b.append(stage_b)
                if pending_b and tc == NCH - 1:
                    pending_b.pop()()
                tot_all = xr[0][:].rearrange("p (a b) -> p a b", a=NT); r_tot = r_xr[0]
                cum_all = xr[1][:].rearrange("p (a b) -> p a b", a=NT); r_cumall = r_xr[1]
                for i in range(NT):
                    P.op("dve", lambda e, i=i: e.max(out=m8a[:, i, :], in_=lg_all[:, i, :]),
                         reads=[r_lgall[i]], writes=[r_m8a])
                P.op("dve", lambda e: e.tensor_tensor(out=mask_all[:], in0=lg_all[:],
                                                      in1=bc(m8a[:, :, 3:4], [128, NT, E]), op=ALU.is_ge),
                     reads=r_lgall + [r_m8a], writes=r_maskall)
                P.op("dve", lambda e: e.tensor_tensor(out=lg_all[:], in0=lg_all[:],
                                                      in1=bc(m8a[:, :, 0:1], [128, NT, E]), op=ALU.subtract),
                     reads=r_lgall + [r_m8a], writes=r_lgall)
                P.op("act", lambda e: e.activation(out=lg_all[:], in_=lg_all[:], func=AF.Exp),
                     reads=r_lgall, writes=r_lgall)
                P.op("dve", lambda e: e.tensor_tensor(out=lg_all[:], in0=lg_all[:], in1=mask_all[:], op=ALU.mult),
                     reads=r_lgall + r_maskall, writes=r_lgall)
                P.op("dve", lambda e: e.reduce_sum(out=den_all[:], in_=lg_all[:], axis=AX.X), reads=r_lgall, writes=[r_den])
                P.op("dve", lambda e: e.reciprocal(out=den_all[:], in_=den_all[:]), reads=[r_den], writes=[r_den])
                P.op("dve", lambda e: e.tensor_tensor(out=gw_all[:], in0=lg_all[:],
                                                      in1=bc(den_all[:, :].unsqueeze(2), [128, NT, E]), op=ALU.mult),
                     reads=r_lgall + [r_den], writes=[r_gwall])
                for i in range(NT):
                    P.op("pe", lambda e, i=i: e.matmul(p_yh[i // 16][:, (i % 16) * E:(i % 16 + 1) * E], lhsT=ones_f[:],
                                                       rhs=mask_all[:, i, :], start=True, stop=True),
                         reads=[r_maskall[i], r_ones], writes=[r_pyh[i // 16]])
                    P.op("pe", lambda e, i=i: e.matmul(p_yc[i // 16][:, (i % 16) * E:(i % 16 + 1) * E], lhsT=lstrict[:],
                                                       rhs=mask_all[:, i, :], start=True, stop=True),
                         reads=[r_maskall[i], r_const], writes=[r_pyc[i // 16]])
                for hf in range(2):
                    P.op("act", lambda e, hf=hf: e.copy(out=tot_all[:, hf * 16:(hf + 1) * 16, :].rearrange("p a b -> p (a b)"),
                                                        in_=p_yh[hf][:]), reads=[r_pyh[hf]], writes=[r_tot])
                P.op("pool", lambda e: e.memset(cum_all[:, 0, :], 0.0), writes=[r_cumall])
                for i in range(1, NT):
                    P.op("dve", lambda e, i=i: e.tensor_tensor(out=cum_all[:, i, :], in0=cum_all[:, i - 1, :],
                                                                in1=tot_all[:, i - 1, :], op=ALU.add),
                         reads=[r_cumall, r_tot], writes=[r_cumall])
                P.op("dve", lambda e: e.tensor_tensor(out=cnt_b[:], in0=cum_all[:, NT - 1, :], in1=tot_all[:, NT - 1, :],
                                                      op=ALU.add), reads=[r_cumall, r_tot], writes=[r_cnt])
                for hf in range(2):
                    P.op("dve", lambda e, hf=hf: e.tensor_tensor(
                        out=rank_all[:, hf * 16:(hf + 1) * 16, :].rearrange("p a b -> p (a b)"), in0=p_yc[hf][:],
                        in1=cum_all[:, hf * 16:(hf + 1) * 16, :].rearrange("p a b -> p (a b)"), op=ALU.add),
                        reads=[r_pyc[hf], r_cumall], writes=r_rankall)
                P.flush()

        with ExitStack() as st:
            padb = sb(st, "padb", [128, E]); endb = sb(st, "endb", [128, E]); startb = sb(st, "startb", [128, E])
            r_rt = Res()
            P.op("pool", lambda e: e.memset(padb[:], 0.0), writes=[r_rt])
            for k in range(8):
                P.op("dve", lambda e, k=k: e.scalar_tensor_tensor(out=padb[:], in0=cnt_b[:], scalar=float(512 * k + 1),
                                                                  in1=padb[:], op0=ALU.is_ge, op1=ALU.add),
                     reads=[r_cnt, r_rt], writes=[r_rt])
            P.op("dve", lambda e: e.tensor_scalar_mul(out=padb[:], in0=padb[:], scalar1=512.0), reads=[r_rt], writes=[r_rt])
            P.op("dve", lambda e: e.tensor_tensor_scan(out=endb[:], data0=ones_f[:, 0:E], data1=padb[:], initial=0.0,
                                                       op0=ALU.mult, op1=ALU.add), reads=[r_rt, r_ones], writes=[r_rt])
            P.op("dve", lambda e: e.tensor_tensor(out=startb[:], in0=endb[:], in1=padb[:], op=ALU.subtract),
                 reads=[r_rt], writes=[r_rt])
            dsel = sb(st, "dsel", [128, NT, E]); r_dsel = Res()
            P.op("dve", lambda e: e.tensor_tensor(out=dsel[:], in0=rank_all[:], in1=bc(startb[:, :].unsqueeze(1), [128, NT, E]),
                                                  op=ALU.add), reads=r_rankall + [r_rt], writes=[r_dsel])
            P.op("dve", lambda e: e.scalar_tensor_tensor(out=dsel[:], in0=dsel[:], scalar=1.0, in1=mask_all[:],
                                                         op0=ALU.add, op1=ALU.mult),
                 reads=[r_dsel] + r_maskall, writes=[r_dsel])
            t8 = sb(st, "t8", [128, NT, 8]); r_t8 = Res()
            oh = sb(st, "oh", [128, E]); r_oh_ = Res()
            d4f = sb(st, "d4f", [128, NT, 4]); r_d4f = Res()
            junk2 = sb(st, "junk2", [128, E])
            for i in range(NT):
                P.op("dve", lambda e, i=i: e.max(out=t8[:, i, :], in_=dsel[:, i, :]), reads=[r_dsel], writes=[r_t8])
                for k in range(4):
                    P.op("dve", lambda e, i=i, k=k: e.tensor_scalar(out=oh[:], in0=dsel[:, i, :], scalar1=t8[:, i, k:k + 1],
                                                                     scalar2=None, op0=ALU.is_equal),
                         reads=[r_dsel, r_t8], writes=[r_oh_])
                    P.op("dve", lambda e, i=i: e.tensor_tensor(out=junk2[:], in0=oh[:], in1=gw_all[:, i, :], op=ALU.mult),
                         reads=[r_oh_, r_gwall], writes=[r_oh_])
                    P.op("dve", lambda e, i=i, k=k: e.reduce_sum(out=w4[:, i, k:k + 1], in_=junk2[:], axis=AX.X),
                         reads=[r_oh_], writes=[r_w4])
            P.op("dve", lambda e: e.tensor_scalar_add(out=d4f[:], in0=t8[:, :, 0:4], scalar1=-1.0),
                 reads=[r_t8], writes=[r_d4f])
            P.op("dve", lambda e: e.tensor_copy(out=dest4i[:], in_=d4f[:]), reads=[r_d4f], writes=[r_dest4])
            fill = sb(st, "fill", [128, NBLK * 4], I32); r_fill = Res()
            tokid = sb(st, "tokid", [128, NT], I32); r_tok = Res()
            P.op("pool", lambda e: e.iota(fill[:], pattern=[[0, NBLK * 4]], base=S, channel_multiplier=0), writes=[r_fill])
            P.op("pool", lambda e: e.iota(tokid[:], pattern=[[128, NT]], base=0, channel_multiplier=1), writes=[r_tok])
            P.op("sp", lambda e: e.dma_start(out=slot_d.rearrange("(p c) o -> p (c o)", p=128), in_=fill[:]),
                 reads=[r_fill], writes=[r_slotd], dma=True)
            r_sc = [Res() for _ in range(NT * 4)]
            for i in range(NT):
                for k in range(4):
                    P.op("pool", lambda e, i=i, k=k: e.indirect_dma_start(
                        out=slot_d[:, :], out_offset=bass.IndirectOffsetOnAxis(ap=dest4i[:, i, k:k + 1], axis=0),
                        in_=tokid[:, i:i + 1], in_offset=None),
                        reads=[r_dest4, r_tok, r_slotd], writes=[r_sc[i * 4 + k]], dma=True)
            for g in range(8):
                P.op("sp", lambda e, g=g: e.dma_start(
                    out=slot_sb[:, g * 32:(g + 1) * 32],
                    in_=slot_d[g * 4096:(g + 1) * 4096, :].rearrange("(c p) o -> p (c o)", p=128)),
                    reads=[r_slotd] + r_sc, writes=[r_slot_sb], dma=True)
            thr = sb(st, "thr", [128, NBLK]); cmp = sb(st, "cmp", [128, NBLK, E]); beb = sb(st, "beb", [128, NBLK])
            iokc = sb(st, "iokc", [128, 8]); wif = sb(st, "wif", [128, NBLK, 8]); r_be = Res()
            P.op("pool", lambda e: e.iota(thr[:], pattern=[[512, NBLK]], base=0, channel_multiplier=0,
                                          allow_small_or_imprecise_dtypes=True), writes=[r_be])
            P.op("pool", lambda e: e.iota(iokc[:], pattern=[[128, 8]], base=0, channel_multiplier=1,
                                          allow_small_or_imprecise_dtypes=True), writes=[r_be])
            P.op("dve", lambda e: e.tensor_tensor(out=cmp[:], in0=bc(thr[:, :].unsqueeze(2), [128, NBLK, E]),
                                                  in1=bc(endb[:, :].unsqueeze(1), [128, NBLK, E]), op=ALU.is_ge),
                 reads=[r_rt, r_be], writes=[r_be])
            P.op("dve", lambda e: e.reduce_sum(out=beb[:], in_=cmp[:], axis=AX.X), reads=[r_be], writes=[r_be])
            P.op("dve", lambda e: e.tensor_scalar_min(out=beb[:], in0=beb[:], scalar1=float(E - 1)), reads=[r_be], writes=[r_be])
            P.op("dve", lambda e: e.tensor_scalar(out=ohb[:], in0=beb[0:32, :], scalar1=iota_p[0:32, 0:1], scalar2=None,
                                                   op0=ALU.is_equal), reads=[r_be, r_const], writes=[r_ohb])
            P.op("dve", lambda e: e.scalar_tensor_tensor(out=wif[:], in0=bc(beb[:, :].unsqueeze(2), [128, NBLK, 8]),
                                                         scalar=1024.0, in1=bc(iokc[:, :].unsqueeze(1), [128, NBLK, 8]),
                                                         op0=ALU.mult, op1=ALU.add), reads=[r_be], writes=[r_be])
            same = sb(st, "same", [128, NBLK]); pm1 = sb(st, "pm1", [128, 1])
            P.op("dve", lambda e: e.memset(same[:, 0:1], 0.0), reads=[r_be], writes=[r_be])
            P.op("dve", lambda e: e.tensor_tensor(out=same[:, 1:NBLK], in0=beb[:, 1:NBLK], in1=beb[:, 0:NBLK - 1],
                                                  op=ALU.is_equal), reads=[r_be], writes=[r_be])
            P.op("dve", lambda e: e.tensor_scalar(out=pm1[:], in0=iota_p[:], scalar1=1.0, scalar2=1048576.0,
                                                   op0=ALU.min, op1=ALU.mult), reads=[r_be, r_const], writes=[r_be])
            P.op("dve", lambda e: e.tensor_scalar_mul(out=same[:], in0=same[:], scalar1=pm1[:, 0:1]),
                 reads=[r_be], writes=[r_be])
            P.op("dve", lambda e: e.tensor_tensor(out=wif[:], in0=wif[:], in1=bc(same[:, :].unsqueeze(2), [128, NBLK, 8]),
                                                  op=ALU.add), reads=[r_be], writes=[r_be])
            P.op("dve", lambda e: e.tensor_copy(out=widx[:], in_=wif[:]), reads=[r_be], writes=[r_widx])
            if debug:
                P.op("sp", lambda e: e.dma_start(out=dbg_d[:, 0:NT * E], in_=gw_all[:].rearrange("p a b -> p (a b)")),
                     reads=r_maskall, writes=[Res()], dma=True)
                P.op("sp", lambda e: e.dma_start(out=dbg_d[:, NT * E:NT * E + NBLK], in_=beb[:]),
                     reads=[r_be], writes=[Res()], dma=True)
                P.op("sp", lambda e: e.dma_start(out=dbg_d[:, NT * E + NBLK:NT * E + NBLK + NT * 4],
                                                 in_=d4f[:].rearrange("p a b -> p (a b)")),
                     reads=[r_d4f], writes=[Res()], dma=True)
            P.flush()
        stR.close()

        with ExitStack() as st:
            bgu = sb(st, "bgu", [E, 2 * D], BF16); bdn = sb(st, "bdn", [E, D], BF16); r_bias = Res()
            P.op("pool", lambda e: e.dma_start(out=bgu[:], in_=b_gu[:, :]), writes=[r_bias], dma=True)
            P.op("pool", lambda e: e.dma_start(out=bdn[:], in_=b_dn[:, :]), writes=[r_bias], dma=True)
            wgu = [sb(st, "wgu%d" % i, [128, 8, 2 * D], BF16) for i in range(2)]; r_wgu = [[Res() for _ in range(8)] for _ in range(2)]
            wdn = [sb(st, "wdn%d" % i, [128, 8, D], BF16) for i in range(2)]; r_wdn = [[Res() for _ in range(8)] for _ in range(2)]
            xg = [sb(st, "xg%d" % i, [128, 4, D], BF16) for i in range(2)]; r_xg = [[Res() for _ in range(4)] for _ in range(2)]
            xT = sb(st, "xT", [128, 8, 512], BF16); r_xT = Res()
            aT = sb(st, "aT", [128, 8, 512], BF16); r_aT = Res()
            ohj = [sb(st, "ohj%d" % i, [E, 512], BF16) for i in range(2)]; r_ohj = [Res(), Res()]
            yst = sb(st, "yst", [128, 4, D]); r_yst = Res()
            gp = [sb(st, "gp%d" % i, [128, 512]) for i in range(2)]; r_gp = [Res(), Res()]
            sg_ = [sb(st, "sg%d" % i, [128, 512]) for i in range(2)]; r_sg = [Res(), Res()]
            up = [sb(st, "up%d" % i, [128, 512]) for i in range(2)]; r_up7 = [Res(), Res()]
            p_x = [ps(st, "p_x%d" % i, [128, 8, 128], BF16) for i in range(2)]; r_px = [Res(), Res()]
            p_g = [ps(st, "p_g%d" % i, [128, 512]) for i in range(2)]; r_pg = [Res(), Res()]
            p_u = [ps(st, "p_u%d" % i, [128, 512]) for i in range(2)]; r_pu = [Res(), Res()]
            p_y = [ps(st, "p_y%d" % i, [128, 512]) for i in range(2)]; r_py = [Res(), Res()]

            bnd_reg = []

            def get_bnd(e):
                if not bnd_reg:
                    r = e.alloc_register("bnd_reg")
                    e.reg_mov(r, E * D - 1)
                    bnd_reg.append(r)
                return bnd_reg[0]

            def load_block(j):
                bi = j % 2
                if j >= 1:
                    P.op("dve", lambda e, bi=bi: e.tensor_copy(out=wgu[bi][:], in_=wgu[1 - bi][:]),
                         reads=r_wgu[1 - bi], writes=r_wgu[bi])
                    P.op("dve", lambda e, bi=bi: e.tensor_copy(out=wdn[bi][:], in_=wdn[1 - bi][:]),
                         reads=r_wdn[1 - bi], writes=r_wdn[bi])
                for kc in range(8):
                    P.op("pool", lambda e, j=j, kc=kc, bi=bi: e.indirect_dma_start(
                        out=wgu[bi][:, kc, :], out_offset=None, in_=w_gu[:, :],
                        in_offset=bass.IndirectOffsetOnAxis(ap=widx[:, j, kc:kc + 1], axis=0),
                        bounds_check=get_bnd(e), oob_is_err=False),
                        reads=[r_widx], writes=[r_wgu[bi][kc]], dma=True)
                for kc in range(8):
                    P.op("pool", lambda e, j=j, kc=kc, bi=bi: e.indirect_dma_start(
                        out=wdn[bi][:, kc, :], out_offset=None, in_=w_dn[:, :],
                        in_offset=bass.IndirectOffsetOnAxis(ap=widx[:, j, kc:kc + 1], axis=0),
                        bounds_check=get_bnd(e), oob_is_err=False),
                        reads=[r_widx], writes=[r_wdn[bi][kc]], dma=True)
                for q in range(4):
                    P.op("pool", lambda e, j=j, q=q, bi=bi: e.indirect_dma_start(
                        out=xg[bi][:, q, :], out_offset=None, in_=h2_d[:, :],
                        in_offset=bass.IndirectOffsetOnAxis(ap=slot_sb[:, j * 4 + q:j * 4 + q + 1], axis=0)),
                        reads=[r_slot_sb] + r_h2d, writes=[r_xg[bi][q]], dma=True)
                P.op("dve", lambda e, j=j, bi=bi: e.tensor_copy(out=ohj[bi][:], in_=bc(ohb[:, j:j + 1], [E, 512])),
                     reads=[r_ohb], writes=[r_ohj[bi]])

            load_block(0)
            pxi = 0
            for j in range(NBLK):
                bi = j % 2
                if j + 1 < NBLK:
                    load_block(j + 1)
                for kc in range(8):
                    pb = pxi % 2; pxi += 1
                    for q in range(4):
                        P.op("pe", lambda e, kc=kc, q=q, pb=pb, bi=bi: e.transpose(
                            out=p_x[pb][:, q, :], in_=xg[bi][:, q, kc * 128:(kc + 1) * 128], identity=ident_b[:]),
                            reads=[r_xg[bi][q], r_const], writes=[r_px[pb]])
                    P.op("act", lambda e, kc=kc, pb=pb: e.copy(out=xT[:, kc, :].rearrange("p (a b) -> p a b", a=4), in_=p_x[pb][:, 0:4, :]),
                         reads=[r_px[pb]], writes=[r_xT])
                for m in range(8):
                    b2 = m % 2
                    for (pt_, rp, col0) in ((p_g[b2], r_pg[b2], m * 128), (p_u[b2], r_pu[b2], D + m * 128)):
                        for kc in range(8):
                            P.op("pe", lambda e, pt_=pt_, kc=kc, col0=col0, bi=bi: e.matmul(
                                pt_[:], lhsT=wgu[bi][:, kc, col0:col0 + 128], rhs=xT[:, kc, :],
                                start=(kc == 0), stop=False), reads=[r_wgu[bi][kc], r_xT], writes=[rp])
                        P.op("pe", lambda e, pt_=pt_, col0=col0, bi=bi: e.matmul(
                            pt_[:], lhsT=bgu[:, col0:col0 + 128], rhs=ohj[bi][:], start=False, stop=True),
                            reads=[r_bias, r_ohj[bi]], writes=[rp])
                    P.op("dve", lambda e, b2=b2: e.tensor_scalar_min(out=gp[b2][:], in0=p_g[b2][:], scalar1=7.0),
                         reads=[r_pg[b2]], writes=[r_gp[b2]])
                    P.op("act", lambda e, b2=b2: e.activation(out=sg_[b2][:], in_=gp[b2][:], func=AF.Sigmoid, scale=1.702),
                         reads=[r_gp[b2]], writes=[r_sg[b2]])
                    P.op("dve", lambda e, b2=b2: e.tensor_scalar(out=up[b2][:], in0=p_u[b2][:], scalar1=-7.0, scalar2=7.0,
                                                                  op0=ALU.max, op1=ALU.min),
                         reads=[r_pu[b2]], writes=[r_up7[b2]])
                    P.op("dve", lambda e, b2=b2: e.tensor_tensor(out=gp[b2][:], in0=gp[b2][:], in1=sg_[b2][:], op=ALU.mult),
                         reads=[r_gp[b2], r_sg[b2]], writes=[r_gp[b2]])
                    P.op("dve", lambda e, b2=b2, m=m: e.scalar_tensor_tensor(
                        out=aT[:, m, :], in0=up[b2][:], scalar=1.0, in1=gp[b2][:], op0=ALU.add, op1=ALU.mult),
                        reads=[r_up7[b2], r_gp[b2]], writes=[r_aT])
                for q in range(4):
                    for dh in range(2):
                        pb = (q * 2 + dh) % 2
                        for m in range(8):
                            P.op("pe", lambda e, q=q, dh=dh, m=m, pb=pb, bi=bi: e.matmul(
                                p_y[pb][:], lhsT=aT[:, m, q * 128:(q + 1) * 128], rhs=wdn[bi][:, m, dh * 512:(dh + 1) * 512],
                                start=(m == 0), stop=False), reads=[r_aT, r_wdn[bi][m]], writes=[r_py[pb]])
                        P.op("pe", lambda e, dh=dh, pb=pb, bi=bi: e.matmul(
                            p_y[pb][:], lhsT=ohj[bi][:, 0:128], rhs=bdn[:, dh * 512:(dh + 1) * 512], start=False, stop=True),
                            reads=[r_ohj[bi], r_bias], writes=[r_py[pb]])
                        if dh == 0:
                            P.op("act", lambda e, q=q, pb=pb: e.copy(out=yst[:, q, 0:512], in_=p_y[pb][:]),
                                 reads=[r_py[pb]], writes=[r_yst])
                        else:
                            P.op("dve", lambda e, q=q, pb=pb: e.tensor_copy(out=yst[:, q, 512:1024], in_=p_y[pb][:]),
                                 reads=[r_py[pb]], writes=[r_yst])
                P.op("sp", lambda e, j=j: e.dma_start(
                    out=ys_d[j * 512:(j + 1) * 512, :].rearrange("(q p) d -> p q d", p=128), in_=yst[:]),
                    reads=[r_yst], writes=[r_ysd], dma=True)
            P.flush()

        with ExitStack() as st:
            G = [[sb(st, "G%d_%d" % (b_, k), [128, D]) for k in range(4)] for b_ in range(2)]
            r_G = [[Res() for _ in range(4)] for _ in range(2)]
            hx = [sb(st, "hx%d" % i, [128, D]) for i in range(2)]; r_hx = [Res(), Res()]
            ac = [sb(st, "ac%d" % i, [128, D]) for i in range(2)]; r_ac = [Res(), Res()]
            ot = [sb(st, "ot%d" % i, [128, D]) for i in range(2)]; r_ot = [Res(), Res()]
            junk3 = sb(st, "junk3", [128, D], BF16); r_j3 = Res()
            ss3 = sb(st, "ss3", [128, NT]); r_ss3 = [Res() for _ in range(NT)]
            r_out = Res()
            for i in range(NT):
                b2 = i % 2
                for k in range(4):
                    P.op("pool", lambda e, i=i, k=k, b2=b2: e.indirect_dma_start(
                        out=G[b2][k][:], out_offset=None, in_=ys_d[:, :],
                        in_offset=bass.IndirectOffsetOnAxis(ap=dest4i[:, i, k:k + 1], axis=0)),
                        reads=[r_dest4, r_ysd], writes=[r_G[b2][k]], dma=True)
                P.op("sp", lambda e, i=i, b2=b2: e.dma_start(out=hx[b2][:], in_=hres_d[i * 128:(i + 1) * 128, :]),
                     reads=[r_hres[i]], writes=[r_hx[b2]], dma=True)
                P.op("dve", lambda e, i=i, b2=b2: e.tensor_scalar_mul(out=ac[b2][:], in0=G[b2][0][:], scalar1=w4[:, i, 0:1]),
                     reads=[r_G[b2][0], r_w4], writes=[r_ac[b2]])
                for k in range(1, 4):
                    P.op("dve", lambda e, i=i, k=k, b2=b2: e.scalar_tensor_tensor(
                        out=ac[b2][:], in0=G[b2][k][:], scalar=w4[:, i, k:k + 1], in1=ac[b2][:], op0=ALU.mult, op1=ALU.add),
                        reads=[r_G[b2][k], r_w4, r_ac[b2]], writes=[r_ac[b2]])
                P.op("dve", lambda e, b2=b2: e.tensor_tensor(out=ac[b2][:], in0=ac[b2][:], in1=g2b[:], op=ALU.mult),
                     reads=[r_ac[b2], r_g2b], writes=[r_ac[b2]])
                P.op("dve", lambda e, b2=b2: e.tensor_tensor(out=ac[b2][:], in0=ac[b2][:], in1=hx[b2][:], op=ALU.add),
                     reads=[r_ac[b2], r_hx[b2]], writes=[r_ac[b2]])
                P.op("act", lambda e, i=i, b2=b2: e.activation(out=junk3[:], in_=ac[b2][:], func=AF.Square,
                                                                accum_out=ss3[:, i:i + 1]),
                     reads=[r_ac[b2]], writes=[r_j3, r_ss3[i]])
                P.op("act", lambda e, i=i: e.activation(out=ss3[:, i:i + 1], in_=ss3[:, i:i + 1], func=AF.Sqrt,
                                                        scale=1.0 / D, bias=epsb[:]),
                     reads=[r_ss3[i], r_const], writes=[r_ss3[i]])
                P.op("dve", lambda e, i=i: e.reciprocal(out=ss3[:, i:i + 1], in_=ss3[:, i:i + 1]),
                     reads=[r_ss3[i]], writes=[r_ss3[i]])
                P.op("dve", lambda e, i=i, b2=b2: e.scalar_tensor_tensor(
                    out=ot[b2][:], in0=ac[b2][:], scalar=ss3[:, i:i + 1], in1=fgb[:], op0=ALU.mult, op1=ALU.mult),
                    reads=[r_ac[b2], r_ss3[i], r_fgb], writes=[r_ot[b2]])
                P.op("sp", lambda e, i=i, b2=b2: e.dma_start(out=out[i * 128:(i + 1) * 128, :], in_=ot[b2][:]),
                     reads=[r_ot[b2]], writes=[r_out], dma=True)
            P.flush()
    return nc


_NC_CACHE = {}


def _get_nc(debug=False):
    if debug not in _NC_CACHE:
        _NC_CACHE[debug] = build_nc(debug)
    return _NC_CACHE[debug]


def make_in_maps(inputs, cores):
    g = lambda k: np.ascontiguousarray(np.asarray(inputs[k], dtype=np.float32))
    shared = {
        "ada_w": g("ada_w")[0], "ada_b": g("ada_b")[0], "norm1_g": g("norm1_g")[0], "w_in": g("w_in")[0],
        "lb_table": g("lb_table"), "hg_norm_g": g("hg_norm_g")[0], "w_o_hg": g("w_o_hg")[0],
        "dw_w": g("dw_w")[0], "dw_b": g("dw_b")[0], "cv_ln_g": g("cv_ln_g")[0], "cv_ln_b": g("cv_ln_b")[0],
        "w_o_cv": g("w_o_cv")[0], "b_o_cv": g("b_o_cv")[0], "w_out": g("w_out")[0], "norm2_g": g("norm2_g")[0],
        "router_w": g("router_w")[0], "router_b": g("router_b")[0],
        "w_gate_up": g("w_gate_up")[0].reshape(E * D, 2 * D), "b_gate_up": g("b_gate_up")[0],
        "w_down": g("w_down")[0].reshape(E * D, D), "b_down": g("b_down")[0],
        "final_norm_g": g("final_norm_g"),
    }
    xs = g("x")
    cs = g("c")
    maps = []
    for b in cores:
        m = dict(shared)
        m["x"] = xs[b]
        m["c"] = cs[b]
        maps.append(m)
    return maps


def kernel(**inputs):
    nc = _get_nc(False)
    maps = make_in_maps(inputs, list(range(8)))
    res = run_bass_kernel_spmd(nc, maps, core_ids=list(range(8)))
    return np.stack([np.asarray(r["out"], dtype=np.float32) for r in res.results], axis=0)
```

```python
import os
from contextlib import ExitStack

import numpy as np
import concourse.bass as bass
import concourse.mybir as mybir
from concourse.bass_utils import run_bass_kernel_spmd

F32 = mybir.dt.float32
BF16 = mybir.dt.bfloat16
I32 = mybir.dt.int32
AF = mybir.ActivationFunctionType
ALU = mybir.AluOpType
AX = mybir.AxisListType

S = 4096
D = 1024
NT = 32
NCH = 8
E = 32
NBLK = 64
NSLOT = NBLK * 512
EPS = 1e-6
IN_COLS = 5632

ENGS = ("pe", "act", "dve", "pool", "sp")


class Res:
    __slots__ = ("last_w", "readers")

    def __init__(self):
        self.last_w = None
        self.readers = {}


class Prog:
    def __init__(self, nc, n_dma_sems=48):
        self.nc = nc
        self.ops = {e: [] for e in ENGS}
        self.seq = {e: 0 for e in ENGS}
        self.waited = {e: {} for e in ENGS}
        self.sems = {}
        self.n_dma = n_dma_sems
        self.dma_uses = [0] * n_dma_sems
        self.dma_rr = 0
        self.same_engine_sync = True

    def alloc(self, stack):
        for e in ENGS:
            self.sems["c_" + e] = stack.enter_context(self.nc.semaphore("c_" + e))
        for i in range(self.n_dma):
            self.sems["d%d" % i] = stack.enter_context(self.nc.semaphore("d%d" % i))

    def op(self, eng, fn, reads=(), writes=(), dma=False):
        deps = {}

        def add(s, v):
            if deps.get(s, 0) < v:
                deps[s] = v

        for r in reads:
            if r.last_w is not None:
                add(*r.last_w)
        for w in writes:
            if w.last_w is not None:
                add(*w.last_w)
            for s, v in w.readers.items():
                add(s, v)
        if dma:
            i = self.dma_rr
            self.dma_rr = (self.dma_rr + 1) % self.n_dma
            s = "d%d" % i
            if self.dma_uses[i] > 0:
                add(s, 16 * self.dma_uses[i])
            self.dma_uses[i] += 1
            ev = (s, 16 * self.dma_uses[i])
        else:
            self.seq[eng] += 1
            ev = ("c_" + eng, self.seq[eng])
        waits = []
        wd = self.waited[eng]
        for s, v in deps.items():
            if s == "c_" + eng and (eng == "pe" or not self.same_engine_sync):
                continue
            if wd.get(s, 0) >= v:
                continue
            wd[s] = v
            waits.append((s, v))
        self.ops[eng].append((waits, fn, ev, dma))
        for r in reads:
            if r.readers.get(ev[0], 0) < ev[1]:
                r.readers[ev[0]] = ev[1]
        for w in writes:
            w.last_w = ev
            w.readers = {}
        return ev

    def barrier(self):
        allev = []
        for e in ENGS:
            if self.seq[e] > 0:
                allev.append(("c_" + e, self.seq[e]))
        for i in range(self.n_dma):
            if self.dma_uses[i] > 0:
                allev.append(("d%d" % i, 16 * self.dma_uses[i]))
        for e in ENGS:
            waits = []
            wd = self.waited[e]
            for s, v in allev:
                if s == "c_" + e:
                    continue
                if wd.get(s, 0) >= v:
                    continue
                wd[s] = v
                waits.append((s, v))
            if waits:
                self.ops[e].append((waits, None, None, False))

    def replay(self, eng, e):
        for waits, fn, ev, dma in self.ops[eng]:
            for s, v in waits:
                e.wait_ge(self.sems[s], v)
            if fn is None:
                continue
            inst = fn(e)
            inst.then_inc(self.sems[ev[0]], 16 if dma else 1)

    def run(self):
        nc = self.nc
        with nc.Block() as block:
            @block.tensor
            def _(e):
                self.replay("pe", e)

            @block.scalar
            def _(e):
                self.replay("act", e)

            @block.vector
            def _(e):
                self.replay("dve", e)

            @block.gpsimd
            def _(e):
                self.replay("pool", e)

            @block.sync
            def _(e):
                self.replay("sp", e)
        self.ops = {e: [] for e in ENGS}

    def flush(self):
        self.barrier()
        self.run()


def bc(ap, shape):
    return ap.broadcast_to(list(shape))


def build_nc(debug=False):
    nc = bass.Bass("TRN2", target_bir_lowering=False)

    def din(name, shape, dt=F32):
        return nc.dram_tensor(name, list(shape), dt, kind="ExternalInput").ap()

    def dscr(name, shape, dt=F32, out=False):
        return nc.dram_tensor(name, list(shape), dt, kind="ExternalOutput" if out else "Internal").ap()

    x = din("x", [S, D])
    c_in = din("c", [D])
    ada_w = din("ada_w", [D, 6 * D])
    ada_b = din("ada_b", [6 * D])
    norm1_g = din("norm1_g", [D])
    w_in = din("w_in", [D, IN_COLS])
    lb_table = din("lb_table", [2, 2, 512])
    hg_norm_g = din("hg_norm_g", [4, 128])
    w_o_hg = din("w_o_hg", [512, D])
    dw_w = din("dw_w", [31, 512])
    dw_b = din("dw_b", [512])
    cv_ln_g = din("cv_ln_g", [512])
    cv_ln_b = din("cv_ln_b", [512])
    w_o_cv = din("w_o_cv", [512, D])
    b_o_cv = din("b_o_cv", [D])
    w_out = din("w_out", [D, D])
    norm2_g = din("norm2_g", [D])
    router_w = din("router_w", [D, E])
    router_b = din("router_b", [E])
    w_gu = din("w_gate_up", [E * D, 2 * D])
    b_gu = din("b_gate_up", [E, 2 * D])
    w_dn = din("w_down", [E * D, D])
    b_dn = din("b_down", [E, D])
    fin_g = din("final_norm_g", [D])
    out = nc.dram_tensor("out", [S, D], F32, kind="ExternalOutput").ap()

    mod_d = dscr("mod_d", [6, D])
    projT_d = dscr("projT_d", [IN_COLS, S])
    vtok_d = dscr("vtok_d", [S, 512], BF16)
    hres_d = dscr("hres_d", [S, D], F32, out=debug)
    h2_d = dscr("h2_d", [S + 128, D], BF16)
    slot_d = dscr("slot_d", [NSLOT, 1], I32)
    ys_d = dscr("ys_d", [NSLOT, D], F32)
    dbg_d = dscr("dbg_d", [128, 32 * 40], F32, out=True) if debug else None

    with ExitStack() as top:
        P = Prog(nc)
        P.alloc(top)
        top.enter_context(nc.allow_non_contiguous_dma(reason="small parameter / index layouts"))

        def sb(st, name, shape, dt=F32):
            return st.enter_context(nc.sbuf_tensor(name, list(shape), dt))

        def ps(st, name, shape, dt=F32):
            return st.enter_context(nc.psum_tensor(name, list(shape), dt))

        ident_f = sb(top, "ident_f", [128, 128]); r_ident = Res()
        ident_b = sb(top, "ident_b", [128, 128], BF16)
        ones_f = sb(top, "ones_f", [128, 128]); r_ones = Res()
        lstrict = sb(top, "lstrict", [128, 128])
        epsb = sb(top, "epsb", [128, 1])
        iota_p = sb(top, "iota_p", [128, 1])
        g2b = sb(top, "g2b", [128, D]); r_g2b = Res()
        fgb = sb(top, "fgb", [128, D]); r_fgb = Res()
        dest4i = sb(top, "dest4i", [128, NT, 4], I32); r_d4 = [Res() for _ in range(NT)]
        w4 = sb(top, "w4", [128, NT, 4]); r_w4 = Res()
        slot_sb = sb(top, "slot_sb", [128, NBLK * 4], I32); r_slot_sb = Res()
        widx = sb(top, "widx", [128, NBLK, 8], I32); r_widx = Res()
        ohb = sb(top, "ohb", [32, NBLK]); r_ohb = Res()
        r_const = Res()

        P.op("pool", lambda e: e.memset(ident_f[:], 1.0), writes=[r_ident])
        P.op("pool", lambda e: e.affine_select(out=ident_f[:], in_=ident_f[:], pattern=[[-1, 128]],
                                               compare_op=ALU.is_equal, fill=0.0, base=0, channel_multiplier=1),
             reads=[r_ident], writes=[r_ident])
        P.op("dve", lambda e: e.tensor_copy(out=ident_b[:], in_=ident_f[:]), reads=[r_ident], writes=[r_const])
        P.op("pool", lambda e: e.memset(ones_f[:], 1.0), writes=[r_ones])
        P.op("pool", lambda e: e.memset(lstrict[:], 1.0), writes=[r_const])
        P.op("pool", lambda e: e.affine_select(out=lstrict[:], in_=lstrict[:], pattern=[[1, 128]],
                                               compare_op=ALU.is_ge, fill=0.0, base=-1, channel_multiplier=-1),
             reads=[r_const], writes=[r_const])
        P.op("pool", lambda e: e.memset(epsb[:], EPS), writes=[r_const])
        P.op("pool", lambda e: e.iota(iota_p[:], pattern=[[0, 1]], base=0, channel_multiplier=1,
                                      allow_small_or_imprecise_dtypes=True), writes=[r_const])

        r_mod = Res()
        r_projT = {}
        r_vtok = [Res() for _ in range(NT)]
        r_hres = [Res() for _ in range(NT)]
        r_h2d = [Res() for _ in range(NT + 1)]
        r_slotd = Res()
        r_ysd = Res()

        with ExitStack() as st:
            c_sb = sb(st, "c_sb", [128, 8]); r_c = Res()
            adw = [sb(st, "adw%d" % i, [128, 8, 512]) for i in range(2)]; r_adw = [Res(), Res()]
            modrow = sb(st, "modrow", [1, 6 * D]); r_modrow = Res()
            adb = sb(st, "adb", [1, 6 * D]); r_adb = Res()
            pmod = [ps(st, "pmod%d" % i, [128, 512]) for i in range(2)]; r_pmod = [Res(), Res()]
            P.op("sp", lambda e: e.dma_start(out=c_sb[:], in_=c_in.rearrange("(c p) -> p c", p=128)),
                 writes=[r_c], dma=True)
            P.op("sp", lambda e: e.dma_start(out=adb[:], in_=ada_b.rearrange("(o n) -> o n", o=1)),
                 writes=[r_adb], dma=True)
            P.op("act", lambda e: e.activation(out=c_sb[:], in_=c_sb[:], func=AF.Silu), reads=[r_c], writes=[r_c])
            adw_v = ada_w.rearrange("(c p) n -> p c n", p=128)
            for n in range(12):
                bi = n % 2
                P.op("sp", lambda e, n=n, bi=bi: e.dma_start(out=adw[bi][:], in_=adw_v[:, :, n * 512:(n + 1) * 512]),
                     writes=[r_adw[bi]], dma=True)
                for kc in range(8):
                    P.op("pe", lambda e, bi=bi, kc=kc: e.matmul(pmod[bi][0:1, :], lhsT=c_sb[:, kc:kc + 1],
                                                                rhs=adw[bi][:, kc, :], start=(kc == 0), stop=(kc == 7)),
                         reads=[r_c, r_adw[bi]], writes=[r_pmod[bi]])
                P.op("dve", lambda e, n=n, bi=bi: e.tensor_tensor(out=modrow[0:1, n * 512:(n + 1) * 512],
                                                                   in0=pmod[bi][0:1, :],
                                                                   in1=adb[0:1, n * 512:(n + 1) * 512], op=ALU.add),
                     reads=[r_pmod[bi], r_adb], writes=[r_modrow])
            P.op("sp", lambda e: e.dma_start(out=mod_d.rearrange("(o k) d -> o (k d)", o=1), in_=modrow[:]),
                 reads=[r_modrow], writes=[r_mod], dma=True)
            P.flush()

        stR = ExitStack()
        mask_all = sb(stR, "mask_all", [128, NT, E]); r_maskall = [Res() for _ in range(NT)]
        gw_all = sb(stR, "gw_all", [128, NT, E])
        rank_all = sb(stR, "rank_all", [128, NT, E]); r_rankall = [Res() for _ in range(NT)]
        cummask = sb(stR, "cummask", [128, E]); r_cum = Res()
        cnt_b = sb(stR, "cnt_b", [128, E]); r_cnt = Res()
        with ExitStack() as stA:

            with ExitStack() as st:
                hT = sb(st, "hT", [128, 8, S], BF16)
                r_hT = [Res() for _ in range(NT)]
                a1 = sb(st, "a1", [128, 8]); sh1 = sb(st, "sh1", [128, 8]); n1f = sb(st, "n1f", [128, 8])
                r_a1 = Res()
                A1 = sb(st, "A1", [128, 8, 128]); SH1 = sb(st, "SH1", [128, 8, 128]); r_A1 = Res()
                P.op("sp", lambda e: e.dma_start(out=sh1[:], in_=mod_d[0].rearrange("(c p) -> p c", p=128)),
                     reads=[r_mod], writes=[r_a1], dma=True)
                P.op("sp", lambda e: e.dma_start(out=a1[:], in_=mod_d[1].rearrange("(c p) -> p c", p=128)),
                     reads=[r_mod], writes=[r_a1], dma=True)
                P.op("sp", lambda e: e.dma_start(out=n1f[:], in_=norm1_g.rearrange("(c p) -> p c", p=128)),
                     writes=[r_a1], dma=True)
                P.op("dve", lambda e: e.scalar_tensor_tensor(out=a1[:], in0=a1[:], scalar=1.0, in1=n1f[:],
                                                             op0=ALU.add, op1=ALU.mult), reads=[r_a1], writes=[r_a1])
                P.op("dve", lambda e: e.tensor_copy(out=A1[:], in_=bc(a1[:, :].unsqueeze(2), [128, 8, 128])),
                     reads=[r_a1], writes=[r_A1])
                P.op("dve", lambda e: e.tensor_copy(out=SH1[:], in_=bc(sh1[:, :].unsqueeze(2), [128, 8, 128])),
                     reads=[r_a1], writes=[r_A1])
                NXB = 3
                xt = [sb(st, "xt%d" % i, [128, D]) for i in range(NXB)]; r_xt = [Res() for _ in range(NXB)]
                sq = sb(st, "sq", [128, D], BF16); r_sq = Res()
                ss = sb(st, "ss", [128, NT]); r_ss = [Res() for _ in range(NT)]
                rs = sb(st, "rs", [128, NT])
                xn = [sb(st, "xn%d" % i, [128, D], BF16) for i in range(2)]; r_xn = [Res(), Res()]
                ptr = [ps(st, "ptr%d" % i, [128, 8, 128], BF16) for i in range(2)]; r_ptr = [Res(), Res()]
                tmp = [sb(st, "tmp%d" % i, [128, 8, 128]) for i in range(2)]; r_tmp = [Res(), Res()]
                for i in range(NT):
                    xb = i % NXB
                    b2 = i % 2
                    P.op("sp", lambda e, i=i, xb=xb: e.dma_start(out=xt[xb][:], in_=x[i * 128:(i + 1) * 128, :]),
                         writes=[r_xt[xb]], dma=True)
                    P.op("act", lambda e, i=i, xb=xb: e.activation(out=sq[:], in_=xt[xb][:], func=AF.Square,
                                                                    accum_out=ss[:, i:i + 1]),
                         reads=[r_xt[xb]], writes=[r_sq, r_ss[i]])
                    P.op("act", lambda e, i=i: e.activation(out=rs[:, i:i + 1], in_=ss[:, i:i + 1], func=AF.Sqrt,
                                                            scale=1.0 / D, bias=epsb[:]),
                         reads=[r_ss[i], r_const], writes=[r_ss[i]])
                    P.op("dve", lambda e, i=i: e.reciprocal(out=rs[:, i:i + 1], in_=rs[:, i:i + 1]),
                         reads=[r_ss[i]], writes=[r_ss[i]])
                    P.op("dve", lambda e, i=i, xb=xb, b2=b2: e.tensor_scalar_mul(out=xn[b2][:], in0=xt[xb][:],
                                                                                  scalar1=rs[:, i:i + 1]),
                         reads=[r_xt[xb], r_ss[i]], writes=[r_xn[b2]])
                    for kc in range(8):
                        P.op("pe", lambda e, kc=kc, b2=b2: e.transpose(out=ptr[b2][:, kc, :],
                                                                        in_=xn[b2][:, kc * 128:(kc + 1) * 128],
                                                                        identity=ident_b[:]),
                             reads=[r_xn[b2], r_const], writes=[r_ptr[b2]])
                    P.op("dve", lambda e, b2=b2: e.tensor_tensor(out=tmp[b2][:], in0=ptr[b2][:], in1=A1[:], op=ALU.mult),
                         reads=[r_ptr[b2], r_A1], writes=[r_tmp[b2]])
                    P.op("pool", lambda e, i=i, b2=b2: e.tensor_tensor(out=hT[:, :, i * 128:(i + 1) * 128],
                                                                        in0=tmp[b2][:], in1=SH1[:], op=ALU.add),
                         reads=[r_tmp[b2], r_A1], writes=[r_hT[i]])

                wv = w_in.rearrange("(c p) n -> p c n", p=128)
                wt = [sb(st, "wt%d" % i, [128, 8, 512], BF16) for i in range(2)]; r_wt = [Res(), Res()]
                stg = [sb(st, "stg%d" % i, [128, S]) for i in range(2)]; r_stg = [Res(), Res()]
                vst = [sb(st, "vst%d" % i, [128, 512], BF16) for i in range(2)]; r_vst = [Res(), Res()]
                pp = [ps(st, "pp%d" % i, [128, 512]) for i in range(4)]; r_pp = [Res() for _ in range(4)]
                ppi = 0
                sgi = 0
                for g in range(11):
                    bi = g % 2
                    P.op("pool", lambda e, g=g, bi=bi: e.dma_start(out=wt[bi][:], in_=wv[:, :, g * 512:(g + 1) * 512]),
                         writes=[r_wt[bi]], dma=True)
                    if g == 3:
                        for i in range(NT):
                            pi = ppi % 4; ppi += 1
                            for kc in range(8):
                                P.op("pe", lambda e, i=i, kc=kc, pi=pi, bi=bi: e.matmul(
                                    pp[pi][:], lhsT=hT[:, kc, i * 128:(i + 1) * 128], rhs=wt[bi][:, kc, :],
                                    start=(kc == 0), stop=(kc == 7)),
                                    reads=[r_hT[i], r_wt[bi]], writes=[r_pp[pi]])
                            vb = i % 2
                            P.op("act", lambda e, pi=pi, vb=vb: e.copy(out=vst[vb][:], in_=pp[pi][:]),
                                 reads=[r_pp[pi]], writes=[r_vst[vb]])
                            P.op("sp", lambda e, i=i, vb=vb: e.dma_start(out=vtok_d[i * 128:(i + 1) * 128, :],
                                                                         in_=vst[vb][:]),
                                 reads=[r_vst[vb]], writes=[r_vtok[i]], dma=True)
                        continue
                    for mm in range(4):
                        sg = sgi % 2; sgi += 1
                        for tc in range(NCH):
                            pi = ppi % 4; ppi += 1
                            for kc in range(8):
                                P.op("pe", lambda e, mm=mm, tc=tc, kc=kc, pi=pi, bi=bi: e.matmul(
                                    pp[pi][:], lhsT=wt[bi][:, kc, mm * 128:(mm + 1) * 128],
                                    rhs=hT[:, kc, tc * 512:(tc + 1) * 512], start=(kc == 0), stop=(kc == 7)),
                                    reads=[r_wt[bi]] + r_hT[tc * 4:(tc + 1) * 4], writes=[r_pp[pi]])
                            if tc % 2 == 0:
                                P.op("act", lambda e, tc=tc, pi=pi, sg=sg: e.copy(
                                    out=stg[sg][:, tc * 512:(tc + 1) * 512], in_=pp[pi][:]),
                                    reads=[r_pp[pi]], writes=[r_stg[sg]])
                            else:
                                P.op("dve", lambda e, tc=tc, pi=pi, sg=sg: e.tensor_copy(
                                    out=stg[sg][:, tc * 512:(tc + 1) * 512], in_=pp[pi][:]),
                                    reads=[r_pp[pi]], writes=[r_stg[sg]])
                        row0 = g * 512 + mm * 128
                        r_projT[row0] = Res()
                        P.op("sp", lambda e, row0=row0, sg=sg: e.dma_start(out=projT_d[row0:row0 + 128, :],
                                                                           in_=stg[sg][:]),
                             reads=[r_stg[sg]], writes=[r_projT[row0]], dma=True)
                P.flush()

            hgT = sb(stA, "hgT", [128, 4, S], BF16)
            r_hgT = [Res() for _ in range(4)]
            with ExitStack() as st:
                lbt = sb(st, "lbt", [128, 2, 2, 4]); r_lb = Res()
                lbv = sb(st, "lbv", [128, 2, 4]); oml = sb(st, "oml", [128, 2, 4])
                ngf = sb(st, "ngf", [128, 4])
                P.op("sp", lambda e: e.dma_start(out=lbt[:, 0], in_=lb_table[0].rearrange("d (h p) -> p d h", p=128)),
                     writes=[r_lb], dma=True)
                P.op("sp", lambda e: e.dma_start(out=lbt[:, 1], in_=lb_table[1].rearrange("d (h p) -> p d h", p=128)),
                     writes=[r_lb], dma=True)
                P.op("sp", lambda e: e.dma_start(out=ngf[:], in_=hg_norm_g.rearrange("h p -> p h")),
                     writes=[r_lb], dma=True)
                P.op("dve", lambda e: e.tensor_tensor(out=lbv[:], in0=lbt[:, 0], in1=lbt[:, 1], op=ALU.subtract),
                     reads=[r_lb], writes=[r_lb])
                P.op("act", lambda e: e.activation(out=lbv[:], in_=lbv[:], func=AF.Sigmoid), reads=[r_lb], writes=[r_lb])
                P.op("dve", lambda e: e.tensor_scalar(out=oml[:], in0=lbv[:], scalar1=-1.0, scalar2=1.0,
                                                       op0=ALU.mult, op1=ALU.add), reads=[r_lb], writes=[r_lb])
                H = 2048
                ones_h = sb(st, "ones_h", [128, H]); r_onesh = Res()
                P.op("pool", lambda e: e.memset(ones_h[:], 1.0), writes=[r_onesh])
                mask_f = sb(st, "mask_f", [128, 128]); mask_b = sb(st, "mask_b", [128, 128]); r_mask = Res()
                for mk, sgn in ((mask_f, 1), (mask_b, -1)):
                    P.op("pool", lambda e, mk=mk: e.memset(mk[:], 1.0), writes=[r_mask])
                    P.op("pool", lambda e, mk=mk, sgn=sgn: e.affine_select(
                        out=mk[:], in_=mk[:], pattern=[[sgn, 128]], compare_op=ALU.is_ge, fill=0.0, base=0,
                        channel_multiplier=-sgn), reads=[r_mask], writes=[r_mask])
                    P.op("pool", lambda e, mk=mk: e.memset(mk[0:64, 64:128], 0.0), reads=[r_mask], writes=[r_mask])
                    P.op("pool", lambda e, mk=mk: e.memset(mk[64:128, 0:64], 0.0), reads=[r_mask], writes=[r_mask])

                T1 = sb(st, "T1", [128, H]); T2 = sb(st, "T2", [128, H]); T3 = sb(st, "T3", [128, H])
                TQ = sb(st, "TQ", [128, H])
                r_T1, r_T2, r_T3, r_TQ = Res(), Res(), Res(), Res()
                Bext = sb(st, "Bext", [128, S + 1]); r_B = Res()
                qt = [sb(st, "qt%d" % d, [128, S], BF16) for d in range(2)]
                kt = [sb(st, "kt%d" % d, [128, S], BF16) for d in range(2)]
                ktok = [sb(st, "ktok%d" % d, [128, NT, 128], BF16) for d in range(2)]
                dec = [sb(st, "dec%d" % d, [128, 64]) for d in range(2)]
                r_qk = [Res(), Res()]
                r_ktok = [Res(), Res()]
                vtok = sb(st, "vtok", [128, NT, 128], BF16); r_vt = Res()
                o_h = sb(st, "o_h", [128, S]); r_oh = [Res() for _ in range(NT)]
                Sf = [sb(st, "Sf%d" % d, [128, 128]) for d in range(2)]
                Sb = [sb(st, "Sb%d" % d, [128, 128], BF16) for d in range(2)]
                Stmp = [sb(st, "Stmp%d" % d, [128, 128]) for d in range(2)]
                r_S = [Res(), Res()]
                r_Sf = [Res(), Res()]
                r_Stmp = [Res(), Res()]
                sT = [sb(st, "sT%d" % d, [128, 128], BF16) for d in range(2)]; r_sT = [Res(), Res()]
                p_sc = [ps(st, "p_sc%d" % d, [128, 512])[:, 0:128] for d in range(2)]; r_psc = [Res(), Res()]
                p_o = [ps(st, "p_o%d" % d, [128, 512])[:, 0:128] for d in range(2)]; r_po = [Res(), Res()]
                p_P = [ps(st, "p_P%d" % d, [128, 512])[:, 0:128] for d in range(2)]; r_pP = [Res(), Res()]
                p_kt = ps(st, "p_kt", [128, 8, 128], BF16); r_pkt = Res()
                p_st = ps(st, "p_st", [128, 512]); r_pst = Res()

                for h in range(4):
                    P.op("sp", lambda e, h=h: e.dma_start(
                        out=vtok[:], in_=vtok_d[:, h * 128:(h + 1) * 128].rearrange("(i p) v -> p i v", p=128)),
                        reads=r_vtok, writes=[r_vt], dma=True)
                    P.op("pool", lambda e: e.memset(o_h[:], 0.0), writes=r_oh)
                    for d in range(2):
                        zrow = 512 + d * 512 + h * 128
                        B3 = Bext[:, 1:S + 1].rearrange("p (c j) -> p c j", j=64)
                        B0 = Bext[:, 0:S].rearrange("p (c j) -> p c j", j=64)
                        P.op("pool", lambda e: e.memset(Bext[:, 0:1], 0.0), writes=[r_B])
                        for hf in range(2):
                            c0 = hf * H
                            P.op("sp", lambda e, zrow=zrow, c0=c0: e.dma_start(
                                out=T1[:], in_=projT_d[zrow:zrow + 128, c0:c0 + H]),
                                reads=[r_projT[zrow]], writes=[r_T1], dma=True)
                            P.op("act", lambda e: e.activation(out=T1[:], in_=T1[:], func=AF.Sigmoid),
                                 reads=[r_T1], writes=[r_T1])
                            P.op("dve", lambda e, d=d, h=h: e.tensor_scalar(
                                out=T1[:], in0=T1[:], scalar1=oml[:, d, h:h + 1], scalar2=lbv[:, d, h:h + 1],
                                op0=ALU.mult, op1=ALU.add), reads=[r_T1, r_lb], writes=[r_T1])
                            P.op("act", lambda e: e.activation(out=T2[:], in_=T1[:], func=AF.Ln),
                                 reads=[r_T1], writes=[r_T2])
                            P.op("dve", lambda e, c0=c0: e.tensor_tensor_scan(
                                out=Bext[:, 1 + c0:1 + c0 + H], data0=ones_h[:], data1=T2[:],
                                initial=Bext[:, c0:c0 + 1], op0=ALU.mult, op1=ALU.add),
                                reads=[r_T2, r_onesh, r_B], writes=[r_B])
                            P.op("pool", lambda e: e.tensor_scalar(out=T1[:], in0=T1[:], scalar1=-1.0, scalar2=1.0,
                                                                   op0=ALU.mult, op1=ALU.add),
                                 reads=[r_T1], writes=[r_T1])
                            P.op("act", lambda e, d=d, c0=c0: e.copy(out=kt[d][:, c0:c0 + H], in_=T1[:]),
                                 reads=[r_T1], writes=[r_qk[d]])
                        for hf in range(2):
                            c0 = hf * H
                            cs = slice(hf * 32, (hf + 1) * 32)
                            T2v = T2[:].rearrange("p (c j) -> p c j", j=64)
                            if d == 0:
                                P.op("dve", lambda e, cs=cs: e.tensor_tensor(
                                    out=T2v, in0=B3[:, cs, :], in1=bc(B0[:, cs, 0:1], [128, 32, 64]),
                                    op=ALU.subtract), reads=[r_B], writes=[r_T2])
                            else:
                                P.op("dve", lambda e, cs=cs: e.tensor_tensor(
                                    out=T2v, in0=bc(B3[:, cs, 63:64], [128, 32, 64]), in1=B0[:, cs, :],
                                    op=ALU.subtract), reads=[r_B], writes=[r_T2])
                            P.op("act", lambda e: e.activation(out=T3[:], in_=T2[:], func=AF.Exp),
                                 reads=[r_T2], writes=[r_T3])
                            T3v = T3[:].rearrange("p (c j) -> p c j", j=64)
                            jj = 63 if d == 0 else 0
                            P.op("pool", lambda e, d=d, cs=cs, jj=jj: e.tensor_copy(
                                out=dec[d][:, cs].unsqueeze(2), in_=T3v[:, :, jj:jj + 1]),
                                reads=[r_T3], writes=[r_qk[d]])
                            qrow = h * 128
                            P.op("sp", lambda e, qrow=qrow, c0=c0: e.dma_start(
                                out=TQ[:], in_=projT_d[qrow:qrow + 128, c0:c0 + H]),
                                reads=[r_projT[qrow]], writes=[r_TQ], dma=True)
                            P.op("dve", lambda e, d=d, c0=c0: e.tensor_tensor(
                                out=qt[d][:, c0:c0 + H], in0=TQ[:], in1=T3[:], op=ALU.mult),
                                reads=[r_TQ, r_T3], writes=[r_qk[d]])
                            P.op("act", lambda e: e.activation(out=T3[:], in_=T2[:], func=AF.Exp, scale=-1.0),
                                 reads=[r_T2], writes=[r_T3])
                            P.op("pool", lambda e, d=d, c0=c0: e.tensor_tensor(
                                out=kt[d][:, c0:c0 + H], in0=kt[d][:, c0:c0 + H], in1=T3[:], op=ALU.mult),
                                reads=[r_T3, r_qk[d]], writes=[r_qk[d]])
                        for g8 in range(4):
                            for j in range(8):
                                i = g8 * 8 + j
                                P.op("pe", lambda e, d=d, i=i, j=j: e.transpose(
                                    out=p_kt[:, j, :], in_=kt[d][:, i * 128:(i + 1) * 128], identity=ident_b[:]),
                                    reads=[r_qk[d], r_const], writes=[r_pkt])
                            P.op("act", lambda e, d=d, g8=g8: e.copy(out=ktok[d][:, g8 * 8:(g8 + 1) * 8, :],
                                                                     in_=p_kt[:]),
                                 reads=[r_pkt], writes=[r_ktok[d]])
                        P.op("pool", lambda e, d=d: e.memset(Sf[d][:], 0.0), writes=[r_Sf[d]])
                        P.op("pool", lambda e, d=d: e.memset(Sb[d][:], 0.0), writes=[r_S[d]])

                    for step in range(NT):
                        for d in range(2):
                            i = step if d == 0 else NT - 1 - step
                            t0 = i * 128
                            mk = mask_f if d == 0 else mask_b
                            P.op("pe", lambda e, d=d, t0=t0: e.matmul(
                                p_sc[d][:], lhsT=kt[d][:, t0:t0 + 128], rhs=qt[d][:, t0:t0 + 128],
                                start=True, stop=True), reads=[r_qk[d]], writes=[r_psc[d]])
                            P.op("dve", lambda e, d=d, mk=mk: e.tensor_tensor(
                                out=sT[d][:], in0=p_sc[d][:], in1=mk[:], op=ALU.mult),
                                reads=[r_psc[d], r_mask], writes=[r_sT[d]])
                            P.op("pe", lambda e, d=d, i=i: e.matmul(
                                p_o[d][:], lhsT=vtok[:, i, :], rhs=sT[d][:], start=True, stop=False),
                                reads=[r_vt, r_sT[d]], writes=[r_po[d]])
                            order = (0, 1) if d == 0 else (1, 0)
                            for n_, half in enumerate(order):
                                c = i * 2 + half
                                hs = slice(half * 64, half * 64 + 64)
                                P.op("pe", lambda e, d=d, t0=t0, hs=hs, n_=n_: e.matmul(
                                    p_o[d][:, hs], lhsT=Sb[d][:], rhs=qt[d][:, t0 + hs.start:t0 + hs.stop],
                                    start=False, stop=(n_ == 1)),
                                    reads=[r_S[d], r_qk[d]], writes=[r_po[d]])
                                P.op("pe", lambda e, d=d, i=i, hs=hs: e.matmul(
                                    p_P[d][:], lhsT=ktok[d][hs, i, :], rhs=vtok[hs, i, :], start=True, stop=True),
                                    reads=[r_ktok[d], r_vt], writes=[r_pP[d]])
                                P.op("dve", lambda e, d=d: e.tensor_tensor(
                                    out=Stmp[d][:], in0=p_P[d][:], in1=Sf[d][:], op=ALU.add),
                                    reads=[r_pP[d], r_Sf[d]], writes=[r_Stmp[d]])
                                P.op("act", lambda e, d=d, c=c: e.activation(
                                    out=Sb[d][:], in_=Stmp[d][:], func=AF.Copy, scale=dec[d][:, c:c + 1]),
                                    reads=[r_Stmp[d], r_qk[d]], writes=[r_S[d]])
                                P.op("dve", lambda e, d=d, c=c: e.tensor_scalar_mul(
                                    out=Sf[d][:], in0=Stmp[d][:], scalar1=dec[d][:, c:c + 1]),
                                    reads=[r_Stmp[d], r_qk[d]], writes=[r_Sf[d]])
                            P.op("dve", lambda e, d=d, t0=t0: e.tensor_tensor(
                                out=o_h[:, t0:t0 + 128], in0=p_o[d][:], in1=o_h[:, t0:t0 + 128], op=ALU.add),
                                reads=[r_po[d], r_oh[i]], writes=[r_oh[i]])

                    for tc in range(NCH):
                        cs = slice(tc * 512, (tc + 1) * 512)
                        w0 = (tc % 4) * 512
                        P.op("act", lambda e, cs=cs, w0=w0: e.activation(out=T1[:, w0:w0 + 512], in_=o_h[:, cs],
                                                                         func=AF.Square),
                             reads=r_oh[tc * 4:(tc + 1) * 4], writes=[r_T1])
                        P.op("pe", lambda e, w0=w0: e.matmul(p_st[:], lhsT=ones_f[:], rhs=T1[:, w0:w0 + 512],
                                                            start=True, stop=True),
                             reads=[r_T1, r_ones], writes=[r_pst])
                        P.op("act", lambda e, w0=w0: e.activation(out=T2[:, w0:w0 + 512], in_=p_st[:], func=AF.Sqrt,
                                                                  scale=1.0 / 128, bias=epsb[:]),
                             reads=[r_pst, r_const], writes=[r_T2])
                        P.op("dve", lambda e, w0=w0: e.reciprocal(out=T2[:, w0:w0 + 512], in_=T2[:, w0:w0 + 512]),
                             reads=[r_T2], writes=[r_T2])
                        P.op("dve", lambda e, cs=cs, w0=w0: e.tensor_tensor(
                            out=T2[:, w0:w0 + 512], in0=T2[:, w0:w0 + 512], in1=o_h[:, cs], op=ALU.mult),
                            reads=[r_T2] + r_oh[tc * 4:(tc + 1) * 4], writes=[r_T2])
                        grow = 2048 + h * 128
                        P.op("sp", lambda e, grow=grow, cs=cs, w0=w0: e.dma_start(
                            out=T3[:, w0:w0 + 512], in_=projT_d[grow:grow + 128, cs]),
                            reads=[r_projT[grow]], writes=[r_T3], dma=True)
                        P.op("act", lambda e, w0=w0: e.activation(out=T3[:, w0:w0 + 512], in_=T3[:, w0:w0 + 512],
                                                                  func=AF.Silu), reads=[r_T3], writes=[r_T3])
                        P.op("dve", lambda e, h=h, cs=cs, w0=w0: e.scalar_tensor_tensor(
                            out=hgT[:, h, cs], in0=T2[:, w0:w0 + 512], scalar=ngf[:, h:h + 1],
                            in1=T3[:, w0:w0 + 512], op0=ALU.mult, op1=ALU.mult),
                            reads=[r_T2, r_T3, r_lb], writes=[r_hgT[h]])
                P.flush()

            cvT = sb(stA, "cvT", [128, 4, S], BF16)
            r_cvT = [Res() for _ in range(4)]
            with ExitStack() as st:
                dww = sb(st, "dww", [128, 4, 31]); dwb = sb(st, "dwb", [128, 4])
                lng = sb(st, "lng", [128, 4]); lnb = sb(st, "lnb", [128, 4]); r_cp = Res()
                for cc in range(4):
                    P.op("sp", lambda e, cc=cc: e.dma_start(out=dww[:, cc, :],
                                                            in_=dw_w[:, cc * 128:(cc + 1) * 128].rearrange("j p -> p j")),
                         writes=[r_cp], dma=True)
                for t_, src_ in ((dwb, dw_b), (lng, cv_ln_g), (lnb, cv_ln_b)):
                    P.op("sp", lambda e, t_=t_, src_=src_: e.dma_start(out=t_[:], in_=src_.rearrange("(c p) -> p c", p=128)),
                         writes=[r_cp], dma=True)
                ub = sb(st, "ub", [128, 4, S + 30], BF16); r_ub = [Res() for _ in range(4)]
                for cc in range(4):
                    P.op("pool", lambda e, cc=cc: e.memset(ub[:, cc, 0:15], 0.0), writes=[r_ub[cc]])
                    P.op("pool", lambda e, cc=cc: e.memset(ub[:, cc, S + 15:S + 30], 0.0), writes=[r_ub[cc]])
                HH = 2048
                with ExitStack() as st2:
                    vt = [sb(st2, "vt%d" % i, [128, HH]) for i in range(2)]; r_vt4 = [Res(), Res()]
                    gt = [sb(st2, "gt%d" % i, [128, HH]) for i in range(2)]; r_gt4 = [Res(), Res()]
                    n_it = 0
                    for cc in range(4):
                        vrow = 2560 + cc * 128
                        grow = 3072 + cc * 128
                        for hf in range(2):
                            b2 = n_it % 2; n_it += 1
                            c0 = hf * HH
                            P.op("sp", lambda e, vrow=vrow, c0=c0, b2=b2: e.dma_start(
                                out=vt[b2][:], in_=projT_d[vrow:vrow + 128, c0:c0 + HH]),
                                reads=[r_projT[vrow]], writes=[r_vt4[b2]], dma=True)
                            P.op("sp", lambda e, grow=grow, c0=c0, b2=b2: e.dma_start(
                                out=gt[b2][:], in_=projT_d[grow:grow + 128, c0:c0 + HH]),
                                reads=[r_projT[grow]], writes=[r_gt4[b2]], dma=True)
                            P.op("act", lambda e, b2=b2: e.activation(out=gt[b2][:], in_=gt[b2][:], func=AF.Sigmoid),
                                 reads=[r_gt4[b2]], writes=[r_gt4[b2]])
                            P.op("dve", lambda e, cc=cc, c0=c0, b2=b2: e.tensor_tensor(
                                out=ub[:, cc, 15 + c0:15 + c0 + HH], in0=vt[b2][:], in1=gt[b2][:], op=ALU.mult),
                                reads=[r_vt4[b2], r_gt4[b2]], writes=[r_ub[cc]])
                    P.flush()
                dg = sb(st, "dg", [128, 4, 31, 128], BF16); r_dg = [Res() for _ in range(4)]
                n_it = 0
                for cc in range(4):
                    for j in range(31):
                        if n_it % 2 == 0:
                            P.op("dve", lambda e, cc=cc, j=j: e.tensor_scalar_mul(
                                out=dg[:, cc, j, :], in0=ident_f[:], scalar1=dww[:, cc, j:j + 1]),
                                reads=[r_ident, r_cp], writes=[r_dg[cc]])
                        else:
                            P.op("act", lambda e, cc=cc, j=j: e.activation(
                                out=dg[:, cc, j, :], in_=ident_f[:], func=AF.Copy, scale=dww[:, cc, j:j + 1]),
                                reads=[r_ident, r_cp], writes=[r_dg[cc]])
                        n_it += 1
                ones_b = sb(st, "ones_b", [128, 128], BF16); r_ob = Res()
                P.op("dve", lambda e: e.tensor_copy(out=ones_b[:], in_=ones_f[:]), reads=[r_ones], writes=[r_ob])
                uc = [sb(st, "uc%d" % i, [128, 4, 512], BF16) for i in range(2)]; r_uc = [[Res() for _ in range(4)] for _ in range(2)]
                usq = sb(st, "usq", [128, 4, 512], BF16); r_usq = Res()
                p_cv = [ps(st, "p_cv%d" % i, [128, 512]) for i in range(4)]; r_pcv = [Res() for _ in range(4)]
                p_s1 = ps(st, "p_s1", [128, 512]); p_s2 = ps(st, "p_s2", [128, 512]); r_ps1 = Res(); r_ps2 = Res()
                mean = sb(st, "mean", [128, 512]); msq = sb(st, "msq", [128, 512]); rstd = sb(st, "rstd", [128, 512])
                r_mean = Res(); r_rstd = Res()
                tt = [sb(st, "tt%d" % i, [128, 512]) for i in range(2)]; r_tt = [Res(), Res()]
                for tc in range(NCH):
                    cs = slice(tc * 512, (tc + 1) * 512)
                    ub_ = tc % 2
                    for cc in range(4):
                        for j in range(31):
                            P.op("pe", lambda e, cc=cc, j=j, tc=tc: e.matmul(
                                p_cv[cc][:], lhsT=dg[:, cc, j, :], rhs=ub[:, cc, tc * 512 + j:tc * 512 + j + 512],
                                start=(j == 0), stop=(j == 30)), reads=[r_dg[cc], r_ub[cc]], writes=[r_pcv[cc]])
                        P.op("act", lambda e, cc=cc, ub_=ub_: e.activation(
                            out=uc[ub_][:, cc, :], in_=p_cv[cc][:], func=AF.Identity, bias=dwb[:, cc:cc + 1]),
                            reads=[r_pcv[cc], r_cp], writes=[r_uc[ub_][cc]])
                    for cc in range(4):
                        P.op("act", lambda e, cc=cc, ub_=ub_: e.activation(out=usq[:, cc, :], in_=uc[ub_][:, cc, :],
                                                                          func=AF.Square),
                             reads=[r_uc[ub_][cc]], writes=[r_usq])
                    for cc in range(4):
                        P.op("pe", lambda e, cc=cc, ub_=ub_: e.matmul(p_s1[:], lhsT=ones_b[:], rhs=uc[ub_][:, cc, :],
                                                                      start=(cc == 0), stop=(cc == 3)),
                             reads=[r_uc[ub_][cc], r_ob], writes=[r_ps1])
                    for cc in range(4):
                        P.op("pe", lambda e, cc=cc: e.matmul(p_s2[:], lhsT=ones_b[:], rhs=usq[:, cc, :],
                                                             start=(cc == 0), stop=(cc == 3)),
                             reads=[r_usq, r_ob], writes=[r_ps2])
                    P.op("act", lambda e: e.activation(out=mean[:], in_=p_s1[:], func=AF.Copy, scale=1.0 / 512),
                         reads=[r_ps1], writes=[r_mean])
                    P.op("dve", lambda e: e.tensor_tensor(out=msq[:], in0=mean[:], in1=mean[:], op=ALU.mult),
                         reads=[r_mean], writes=[r_rstd])
                    P.op("dve", lambda e: e.scalar_tensor_tensor(out=rstd[:], in0=p_s2[:], scalar=1.0 / 512, in1=msq[:],
                                                                 op0=ALU.mult, op1=ALU.subtract),
                         reads=[r_ps2, r_rstd], writes=[r_rstd])
                    P.op("dve", lambda e: e.tensor_scalar_max(out=rstd[:], in0=rstd[:], scalar1=0.0),
                         reads=[r_rstd], writes=[r_rstd])
                    P.op("act", lambda e: e.activation(out=rstd[:], in_=rstd[:], func=AF.Sqrt, bias=epsb[:]),
                         reads=[r_rstd, r_const], writes=[r_rstd])
                    P.op("dve", lambda e: e.reciprocal(out=rstd[:], in_=rstd[:]), reads=[r_rstd], writes=[r_rstd])
                    for cc in range(4):
                        b2 = cc % 2
                        P.op("dve", lambda e, cc=cc, ub_=ub_, b2=b2: e.tensor_tensor(
                            out=tt[b2][:], in0=uc[ub_][:, cc, :], in1=mean[:], op=ALU.subtract),
                            reads=[r_uc[ub_][cc], r_mean], writes=[r_tt[b2]])
                        P.op("dve", lambda e, b2=b2: e.tensor_tensor(out=tt[b2][:], in0=tt[b2][:], in1=rstd[:],
                                                                     op=ALU.mult),
                             reads=[r_tt[b2], r_rstd], writes=[r_tt[b2]])
                        P.op("act", lambda e, cc=cc, cs=cs, b2=b2: e.activation(
                            out=cvT[:, cc, cs], in_=tt[b2][:], func=AF.Silu, scale=lng[:, cc:cc + 1],
                            bias=lnb[:, cc:cc + 1]), reads=[r_tt[b2], r_cp], writes=[r_cvT[cc]])
                P.flush()

            with ExitStack() as st:
                wohg = sb(st, "wohg", [128, 4, D], BF16); wocv = sb(st, "wocv", [128, 4, D], BF16)
                wo = sb(st, "wo", [128, 8, D], BF16); r_w5 = Res()
                rw = sb(st, "rw", [128, 8, E]); rbb = sb(st, "rbb", [128, E]); bocv = sb(st, "bocv", [128, 8])
                g1b = sb(st, "g1b", [128, D]); a2b = sb(st, "a2b", [128, D]); sh2b = sb(st, "sh2b", [128, D])
                r_bt = Res()
                P.op("pool", lambda e: e.dma_start(out=wohg[:], in_=w_o_hg.rearrange("(c p) n -> p c n", p=128)),
                     writes=[r_w5], dma=True)
                P.op("pool", lambda e: e.dma_start(out=wocv[:], in_=w_o_cv.rearrange("(c p) n -> p c n", p=128)),
                     writes=[r_w5], dma=True)
                P.op("pool", lambda e: e.dma_start(out=wo[:], in_=w_out.rearrange("(c p) n -> p c n", p=128)),
                     writes=[r_w5], dma=True)
                P.op("sp", lambda e: e.dma_start(out=rw[:], in_=router_w.rearrange("(c p) n -> p c n", p=128)),
                     writes=[r_w5], dma=True)
                P.op("sp", lambda e: e.dma_start(out=rbb[:], in_=router_b.partition_broadcast(128)),
                     writes=[r_w5], dma=True)
                P.op("sp", lambda e: e.dma_start(out=bocv[:], in_=b_o_cv.rearrange("(c p) -> p c", p=128)),
                     writes=[r_w5], dma=True)
                P.op("sp", lambda e: e.dma_start(out=g1b[:], in_=mod_d[2, :].partition_broadcast(128)),
                     reads=[r_mod], writes=[r_bt], dma=True)
                P.op("sp", lambda e: e.dma_start(out=sh2b[:], in_=mod_d[3, :].partition_broadcast(128)),
                     reads=[r_mod], writes=[r_bt], dma=True)
                P.op("sp", lambda e: e.dma_start(out=a2b[:], in_=mod_d[4, :].partition_broadcast(128)),
                     reads=[r_mod], writes=[r_bt], dma=True)
                P.op("sp", lambda e: e.dma_start(out=g2b[:], in_=mod_d[5, :].partition_broadcast(128)),
                     reads=[r_mod], writes=[r_g2b], dma=True)
                P.op("sp", lambda e: e.dma_start(out=fgb[:], in_=fin_g.partition_broadcast(128)),
                     writes=[r_fgb], dma=True)

                gh = [sb(st, "gh%d" % i, [128, 512]) for i in range(2)]; r_gh = [Res(), Res()]
                gc = [sb(st, "gc%d" % i, [128, 512]) for i in range(2)]; r_gc = [Res(), Res()]
                mT = sb(st, "mT", [128, 8, 512], BF16); r_mT = Res()
                m1 = [sb(st, "m1_%d" % i, [128, 512]) for i in range(2)]
                m2 = [sb(st, "m2_%d" % i, [128, 512]) for i in range(2)]
                r_m1 = [Res(), Res()]; r_m2 = [Res(), Res()]
                p_yh = [ps(st, "p_yh%d" % i, [128, 512]) for i in range(2)]; r_pyh = [Res(), Res()]
                p_yc = [ps(st, "p_yc%d" % i, [128, 512]) for i in range(2)]; r_pyc = [Res(), Res()]
                p_o5 = [ps(st, "p_o5%d" % i, [128, 512]) for i in range(2)]; r_po5 = [Res(), Res()]
                p_tr = ps(st, "p_tr", [128, 8, 128], BF16); r_ptr5 = Res()
                p_lg = ps(st, "p_lg", [128, 16, E]); r_plg = Res()
                xr = [sb(st, "xr%d" % i, [128, D]) for i in range(2)]; r_xr = [Res(), Res()]
                hr = [sb(st, "hr%d" % i, [128, D]) for i in range(2)]; r_hr = [Res(), Res()]
                h2f = [sb(st, "h2f%d" % i, [128, D]) for i in range(2)]; r_h2f = [Res(), Res()]
                P.op("sp", lambda e: e.dma_start(out=h2f[0][:], in_=norm2_g.partition_broadcast(128)),
                     writes=[r_h2f[0]], dma=True)
                P.op("dve", lambda e: e.scalar_tensor_tensor(out=a2b[:], in0=a2b[:], scalar=1.0, in1=h2f[0][:],
                                                             op0=ALU.add, op1=ALU.mult), reads=[r_bt, r_h2f[0]], writes=[r_bt])
                h2b = [sb(st, "h2b%d" % i, [128, D], BF16) for i in range(3)]; r_h2b = [Res(), Res(), Res()]
                junk = sb(st, "junk", [128, D], BF16); r_junk = Res()
                ss2 = sb(st, "ss2", [128, NT]); r_ss2 = [Res() for _ in range(NT)]
                h2Th = sb(st, "h2Th", [128, 8, 128], BF16); r_h2Th = Res()
                h2Tl = sb(st, "h2Tl", [128, 8, 128], BF16); r_h2Tl = Res()
                h2l = [sb(st, "h2l%d" % i, [128, D], BF16) for i in range(3)]; r_h2l = [Res(), Res(), Res()]
                rwh = sb(st, "rwh", [128, 8, E], BF16); rwl = sb(st, "rwl", [128, 8, E], BF16)
                P.op("dve", lambda e: e.tensor_copy(out=rwh[:], in_=rw[:]), reads=[r_w5], writes=[r_w5])
                P.op("dve", lambda e: e.tensor_tensor(out=rwl[:], in0=rw[:], in1=rwh[:], op=ALU.subtract),
                     reads=[r_w5], writes=[r_w5])
                lg_all = sb(st, "lg_all", [128, NT, E]); r_lgall = [Res() for _ in range(NT)]
                m8a = sb(st, "m8a", [128, NT, 8]); r_m8a = Res()
                den_all = sb(st, "den_all", [128, NT]); r_den = Res(); r_gwall = Res()
                P.op("pool", lambda e: e.memset(junk[:], 0.0), writes=[r_junk])
                P.op("sp", lambda e: e.dma_start(out=h2_d[S:S + 128, :], in_=junk[:]), reads=[r_junk], writes=[r_h2d[NT]],
                     dma=True)
                pending_b = []
                for tc in range(NCH):
                    cs = slice(tc * 512, (tc + 1) * 512)
                    for dch in range(8):
                        b2 = dch % 2
                        rh = 3584 + dch * 128
                        rc = 4608 + dch * 128
                        P.op("sp", lambda e, cs=cs, rh=rh, b2=b2: e.dma_start(out=gh[b2][:], in_=projT_d[rh:rh + 128, cs]),
                             reads=[r_projT[rh]], writes=[r_gh[b2]], dma=True)
                        P.op("sp", lambda e, cs=cs, rc=rc, b2=b2: e.dma_start(out=gc[b2][:], in_=projT_d[rc:rc + 128, cs]),
                             reads=[r_projT[rc]], writes=[r_gc[b2]], dma=True)
                        P.op("act", lambda e, b2=b2: e.activation(out=gh[b2][:], in_=gh[b2][:], func=AF.Sigmoid),
                             reads=[r_gh[b2]], writes=[r_gh[b2]])
                        P.op("act", lambda e, b2=b2: e.activation(out=gc[b2][:], in_=gc[b2][:], func=AF.Sigmoid),
                             reads=[r_gc[b2]], writes=[r_gc[b2]])
                        for kc in range(4):
                            P.op("pe", lambda e, dch=dch, kc=kc, b2=b2, cs=cs: e.matmul(
                                p_yh[b2][:], lhsT=wohg[:, kc, dch * 128:(dch + 1) * 128], rhs=hgT[:, kc, cs],
                                start=(kc == 0), stop=(kc == 3)), reads=[r_w5, r_hgT[kc]], writes=[r_pyh[b2]])
                        for kc in range(4):
                            P.op("pe", lambda e, dch=dch, kc=kc, b2=b2, cs=cs: e.matmul(
                                p_yc[b2][:], lhsT=wocv[:, kc, dch * 128:(dch + 1) * 128], rhs=cvT[:, kc, cs],
                                start=(kc == 0), stop=(kc == 3)), reads=[r_w5, r_cvT[kc]], writes=[r_pyc[b2]])
                        P.op("dve", lambda e, dch=dch, b2=b2: e.tensor_tensor(
                            out=m1[b2][:], in0=p_yh[b2][:], in1=gh[b2][:], op=ALU.mult),
                            reads=[r_pyh[b2], r_gh[b2]], writes=[r_m1[b2]])
                        P.op("dve", lambda e, dch=dch, b2=b2: e.scalar_tensor_tensor(
                            out=m2[b2][:], in0=p_yc[b2][:], scalar=bocv[:, dch:dch + 1], in1=gc[b2][:],
                            op0=ALU.add, op1=ALU.mult), reads=[r_pyc[b2], r_gc[b2], r_w5], writes=[r_m2[b2]])
                        P.op("dve", lambda e, dch=dch, b2=b2: e.tensor_tensor(
                            out=mT[:, dch, :], in0=m1[b2][:], in1=m2[b2][:], op=ALU.add),
                            reads=[r_m1[b2], r_m2[b2]], writes=[r_mT])
                    for q in range(4):
                        i = tc * 4 + q
                        b2 = i % 2
                        P.op("sp", lambda e, i=i, b2=b2: e.dma_start(out=xr[b2][:], in_=x[i * 128:(i + 1) * 128, :]),
                             writes=[r_xr[b2]], dma=True)
                        for dh in range(2):
                            for kc in range(8):
                                P.op("pe", lambda e, q=q, dh=dh, kc=kc: e.matmul(
                                    p_o5[dh][:], lhsT=mT[:, kc, q * 128:(q + 1) * 128],
                                    rhs=wo[:, kc, dh * 512:(dh + 1) * 512], start=(kc == 0), stop=(kc == 7)),
                                    reads=[r_mT, r_w5], writes=[r_po5[dh]])
                            ds_ = slice(dh * 512, (dh + 1) * 512)
                            P.op("dve", lambda e, dh=dh, ds_=ds_, b2=b2: e.tensor_tensor(
                                out=hr[b2][:, ds_], in0=p_o5[dh][:], in1=g1b[:, ds_], op=ALU.mult),
                                reads=[r_po5[dh], r_bt], writes=[r_hr[b2]])
                        P.op("dve", lambda e, b2=b2: e.tensor_tensor(out=hr[b2][:], in0=hr[b2][:], in1=xr[b2][:],
                                                                     op=ALU.add),
                             reads=[r_hr[b2], r_xr[b2]], writes=[r_hr[b2]])
                        P.op("sp", lambda e, i=i, b2=b2: e.dma_start(out=hres_d[i * 128:(i + 1) * 128, :], in_=hr[b2][:]),
                             reads=[r_hr[b2]], writes=[r_hres[i]], dma=True)
                        P.op("act", lambda e, i=i, b2=b2: e.activation(out=junk[:], in_=hr[b2][:], func=AF.Square,
                                                                        accum_out=ss2[:, i:i + 1]),
                             reads=[r_hr[b2]], writes=[r_junk, r_ss2[i]])
                        P.op("act", lambda e, i=i: e.activation(out=ss2[:, i:i + 1], in_=ss2[:, i:i + 1], func=AF.Sqrt,
                                                                scale=1.0 / D, bias=epsb[:]),
                             reads=[r_ss2[i], r_const], writes=[r_ss2[i]])
                        P.op("dve", lambda e, i=i: e.reciprocal(out=ss2[:, i:i + 1], in_=ss2[:, i:i + 1]),
                             reads=[r_ss2[i]], writes=[r_ss2[i]])
                        P.op("dve", lambda e, i=i, b2=b2: e.scalar_tensor_tensor(
                            out=h2f[b2][:], in0=hr[b2][:], scalar=ss2[:, i:i + 1], in1=a2b[:],
                            op0=ALU.mult, op1=ALU.mult), reads=[r_hr[b2], r_ss2[i], r_bt], writes=[r_h2f[b2]])
                        P.op("dve", lambda e, b2=b2: e.tensor_tensor(out=h2f[b2][:], in0=h2f[b2][:], in1=sh2b[:],
                                                                     op=ALU.add),
                             reads=[r_h2f[b2], r_bt], writes=[r_h2f[b2]])
                        b3 = i % 3
                        P.op("act", lambda e, b2=b2, b3=b3: e.copy(out=h2b[b3][:], in_=h2f[b2][:]),
                             reads=[r_h2f[b2]], writes=[r_h2b[b3]])
                        P.op("sp", lambda e, i=i, b3=b3: e.dma_start(out=h2_d[i * 128:(i + 1) * 128, :], in_=h2b[b3][:]),
                             reads=[r_h2b[b3]], writes=[r_h2d[i]], dma=True)
                        P.op("dve", lambda e, b2=b2, b3=b3: e.tensor_tensor(out=h2l[b3][:], in0=h2f[b2][:], in1=h2b[b3][:],
                                                                             op=ALU.subtract),
                             reads=[r_h2f[b2], r_h2b[b3]], writes=[r_h2l[b3]])
                        def stage_b(i=i, b2=b3):
                            for part, (srcT, r_src, dstT, r_dst) in enumerate(((h2b[b2], r_h2b[b2], h2Th, r_h2Th),
                                                                              (h2l[b2], r_h2l[b2], h2Tl, r_h2Tl))):
                                for kc in range(8):
                                    P.op("pe", lambda e, kc=kc, srcT=srcT: e.transpose(
                                        out=p_tr[:, kc, :], in_=srcT[:, kc * 128:(kc + 1) * 128], identity=ident_b[:]),
                                        reads=[r_src, r_const], writes=[r_ptr5])
                                if part == 0:
                                    P.op("act", lambda e, dstT=dstT: e.copy(out=dstT[:], in_=p_tr[:]),
                                         reads=[r_ptr5], writes=[r_dst])
                                else:
                                    P.op("dve", lambda e, dstT=dstT: e.tensor_copy(out=dstT[:], in_=p_tr[:]),
                                         reads=[r_ptr5], writes=[r_dst])
                            n_acc = 0
                            for (aT, r_a, wpart) in ((h2Th, r_h2Th, rwh), (h2Tl, r_h2Tl, rwh), (h2Th, r_h2Th, rwl)):
                                for kc in range(8):
                                    P.op("pe", lambda e, kc=kc, aT=aT, wpart=wpart, n_acc=n_acc: e.matmul(
                                        p_lg[:, 0, :], lhsT=aT[:, kc, :], rhs=wpart[:, kc, :],
                                        start=(n_acc == 0), stop=(n_acc == 23)),
                                        reads=[r_a, r_w5], writes=[r_plg])
                                    n_acc += 1
                            P.op("dve", lambda e, i=i: e.tensor_tensor(out=lg_all[:, i, :], in0=p_lg[:, 0, :], in1=rbb[:],
                                                                        op=ALU.add),
                                 reads=[r_plg, r_w5], writes=[r_lgall[i]])

                        if len(pending_b) >= 2:
                            pending_b.pop(0)()
                        pending_b.append(stage_b)
                while pending_b:
                    pending_b.pop(0)()
                tot_all = xr[0][:].rearrange("p (a b) -> p a b", a=NT); r_tot = r_xr[0]
                cum_all = xr[1][:].rearrange("p (a b) -> p a b", a=NT); r_cumall = r_xr[1]
                for i in range(NT):
                    P.op("dve", lambda e, i=i: e.max(out=m8a[:, i, :], in_=lg_all[:, i, :]),
                         reads=[r_lgall[i]], writes=[r_m8a])
                P.op("dve", lambda e: e.tensor_tensor(out=mask_all[:], in0=lg_all[:],
                                                      in1=bc(m8a[:, :, 3:4], [128, NT, E]), op=ALU.is_ge),
                     reads=r_lgall + [r_m8a], writes=r_maskall)
                P.op("dve", lambda e: e.tensor_tensor(out=lg_all[:], in0=lg_all[:],
                                                      in1=bc(m8a[:, :, 0:1], [128, NT, E]), op=ALU.subtract),
                     reads=r_lgall + [r_m8a], writes=r_lgall)
                P.op("act", lambda e: e.activation(out=lg_all[:], in_=lg_all[:], func=AF.Exp),
                     reads=r_lgall, writes=r_lgall)
                P.op("dve", lambda e: e.tensor_tensor(out=lg_all[:], in0=lg_all[:], in1=mask_all[:], op=ALU.mult),
                     reads=r_lgall + r_maskall, writes=r_lgall)
                P.op("dve", lambda e: e.reduce_sum(out=den_all[:], in_=lg_all[:], axis=AX.X), reads=r_lgall, writes=[r_den])
                P.op("dve", lambda e: e.reciprocal(out=den_all[:], in_=den_all[:]), reads=[r_den], writes=[r_den])
                P.op("dve", lambda e: e.tensor_tensor(out=gw_all[:], in0=lg_all[:],
                                                      in1=bc(den_all[:, :].unsqueeze(2), [128, NT, E]), op=ALU.mult),
                     reads=r_lgall + [r_den], writes=[r_gwall])
                for i in range(NT):
                    P.op("pe", lambda e, i=i: e.matmul(p_yh[i // 16][:, (i % 16) * E:(i % 16 + 1) * E], lhsT=ones_f[:],
                                                       rhs=mask_all[:, i, :], start=True, stop=True),
                         reads=[r_maskall[i], r_ones], writes=[r_pyh[i // 16]])
                    P.op("pe", lambda e, i=i: e.matmul(p_yc[i // 16][:, (i % 16) * E:(i % 16 + 1) * E], lhsT=lstrict[:],
                                                       rhs=mask_all[:, i, :], start=True, stop=True),
                         reads=[r_maskall[i], r_const], writes=[r_pyc[i // 16]])
                for hf in range(2):
                    P.op("act", lambda e, hf=hf: e.copy(out=tot_all[:, hf * 16:(hf + 1) * 16, :].rearrange("p a b -> p (a b)"),
                                                        in_=p_yh[hf][:]), reads=[r_pyh[hf]], writes=[r_tot])
                P.op("pool", lambda e: e.memset(cum_all[:, 0, :], 0.0), writes=[r_cumall])
                for i in range(1, NT):
                    P.op("dve", lambda e, i=i: e.tensor_tensor(out=cum_all[:, i, :], in0=cum_all[:, i - 1, :],
                                                                in1=tot_all[:, i - 1, :], op=ALU.add),
                         reads=[r_cumall, r_tot], writes=[r_cumall])
                P.op("dve", lambda e: e.tensor_tensor(out=cnt_b[:], in0=cum_all[:, NT - 1, :], in1=tot_all[:, NT - 1, :],
                                                      op=ALU.add), reads=[r_cumall, r_tot], writes=[r_cnt])
                for hf in range(2):
                    P.op("dve", lambda e, hf=hf: e.tensor_tensor(
                        out=rank_all[:, hf * 16:(hf + 1) * 16, :].rearrange("p a b -> p (a b)"), in0=p_yc[hf][:],
                        in1=cum_all[:, hf * 16:(hf + 1) * 16, :].rearrange("p a b -> p (a b)"), op=ALU.add),
                        reads=[r_pyc[hf], r_cumall], writes=r_rankall)
                P.flush()

        with ExitStack() as st:
            padb = sb(st, "padb", [128, E]); endb = sb(st, "endb", [128, E]); startb = sb(st, "startb", [128, E])
            r_rt = Res()
            P.op("pool", lambda e: e.memset(padb[:], 0.0), writes=[r_rt])
            for k in range(8):
                P.op("dve", lambda e, k=k: e.scalar_tensor_tensor(out=padb[:], in0=cnt_b[:], scalar=float(512 * k + 1),
                                                                  in1=padb[:], op0=ALU.is_ge, op1=ALU.add),
                     reads=[r_cnt, r_rt], writes=[r_rt])
            P.op("dve", lambda e: e.tensor_scalar_mul(out=padb[:], in0=padb[:], scalar1=512.0), reads=[r_rt], writes=[r_rt])
            P.op("dve", lambda e: e.tensor_tensor_scan(out=endb[:], data0=ones_f[:, 0:E], data1=padb[:], initial=0.0,
                                                       op0=ALU.mult, op1=ALU.add), reads=[r_rt, r_ones], writes=[r_rt])
            P.op("dve", lambda e: e.tensor_tensor(out=startb[:], in0=endb[:], in1=padb[:], op=ALU.subtract),
                 reads=[r_rt], writes=[r_rt])
            dsel = sb(st, "dsel", [128, NT, E]); r_dsel = Res()
            P.op("dve", lambda e: e.tensor_tensor(out=dsel[:], in0=rank_all[:], in1=bc(startb[:, :].unsqueeze(1), [128, NT, E]),
                                                  op=ALU.add), reads=r_rankall + [r_rt], writes=[r_dsel])
            P.op("dve", lambda e: e.scalar_tensor_tensor(out=dsel[:], in0=dsel[:], scalar=1.0, in1=mask_all[:],
                                                         op0=ALU.add, op1=ALU.mult),
                 reads=[r_dsel] + r_maskall, writes=[r_dsel])
            t8 = sb(st, "t8", [128, NT, 8]); r_t8 = Res()
            oh = sb(st, "oh", [128, E]); r_oh_ = Res()
            d4f = sb(st, "d4f", [128, NT, 4]); r_d4f = Res()
            junk2 = sb(st, "junk2", [128, E])
            fill = sb(st, "fill", [128, NBLK * 4], I32); r_fill = Res()
            tokid = sb(st, "tokid", [128, NT], I32); r_tok = Res()
            P.op("pool", lambda e: e.iota(fill[:], pattern=[[0, NBLK * 4]], base=S, channel_multiplier=0), writes=[r_fill])
            P.op("pool", lambda e: e.iota(tokid[:], pattern=[[128, NT]], base=0, channel_multiplier=1), writes=[r_tok])
            P.op("sp", lambda e: e.dma_start(out=slot_d.rearrange("(p c) o -> p (c o)", p=128), in_=fill[:]),
                 reads=[r_fill], writes=[r_slotd], dma=True)
            r_sc = [Res() for _ in range(NT * 4)]
            for i in range(NT):
                P.op("dve", lambda e, i=i: e.max(out=t8[:, i, :], in_=dsel[:, i, :]), reads=[r_dsel], writes=[r_t8])
                P.op("dve", lambda e, i=i: e.tensor_scalar_add(out=d4f[:, i, :], in0=t8[:, i, 0:4], scalar1=-1.0),
                     reads=[r_t8], writes=[r_d4f])
                P.op("dve", lambda e, i=i: e.tensor_copy(out=dest4i[:, i, :], in_=d4f[:, i, :]),
                     reads=[r_d4f], writes=[r_d4[i]])
                for k in range(4):
                    P.op("pool", lambda e, i=i, k=k: e.indirect_dma_start(
                        out=slot_d[:, :], out_offset=bass.IndirectOffsetOnAxis(ap=dest4i[:, i, k:k + 1], axis=0),
                        in_=tokid[:, i:i + 1], in_offset=None),
                        reads=[r_d4[i], r_tok, r_slotd], writes=[r_sc[i * 4 + k]], dma=True)
                for k in range(4):
                    P.op("dve", lambda e, i=i, k=k: e.tensor_scalar(out=oh[:], in0=dsel[:, i, :], scalar1=t8[:, i, k:k + 1],
                                                                     scalar2=None, op0=ALU.is_equal),
                         reads=[r_dsel, r_t8], writes=[r_oh_])
                    P.op("dve", lambda e, i=i: e.tensor_tensor(out=junk2[:], in0=oh[:], in1=gw_all[:, i, :], op=ALU.mult),
                         reads=[r_oh_, r_gwall], writes=[r_oh_])
                    P.op("dve", lambda e, i=i, k=k: e.reduce_sum(out=w4[:, i, k:k + 1], in_=junk2[:], axis=AX.X),
                         reads=[r_oh_], writes=[r_w4])
            for g in range(8):
                P.op("sp", lambda e, g=g: e.dma_start(
                    out=slot_sb[:, g * 32:(g + 1) * 32],
                    in_=slot_d[g * 4096:(g + 1) * 4096, :].rearrange("(c p) o -> p (c o)", p=128)),
                    reads=[r_slotd] + r_sc, writes=[r_slot_sb], dma=True)
            thr = sb(st, "thr", [128, NBLK]); cmp = sb(st, "cmp", [128, NBLK, E]); beb = sb(st, "beb", [128, NBLK])
            iokc = sb(st, "iokc", [128, 8]); wif = sb(st, "wif", [128, NBLK, 8]); r_be = Res()
            P.op("pool", lambda e: e.iota(thr[:], pattern=[[512, NBLK]], base=0, channel_multiplier=0,
                                          allow_small_or_imprecise_dtypes=True), writes=[r_be])
            P.op("pool", lambda e: e.iota(iokc[:], pattern=[[128, 8]], base=0, channel_multiplier=1,
                                          allow_small_or_imprecise_dtypes=True), writes=[r_be])
            P.op("dve", lambda e: e.tensor_tensor(out=cmp[:], in0=bc(thr[:, :].unsqueeze(2), [128, NBLK, E]),
                                                  in1=bc(endb[:, :].unsqueeze(1), [128, NBLK, E]), op=ALU.is_ge),
                 reads=[r_rt, r_be], writes=[r_be])
            P.op("dve", lambda e: e.reduce_sum(out=beb[:], in_=cmp[:], axis=AX.X), reads=[r_be], writes=[r_be])
            P.op("dve", lambda e: e.tensor_scalar_min(out=beb[:], in0=beb[:], scalar1=float(E - 1)), reads=[r_be], writes=[r_be])
            P.op("dve", lambda e: e.tensor_scalar(out=ohb[:], in0=beb[0:32, :], scalar1=iota_p[0:32, 0:1], scalar2=None,
                                                   op0=ALU.is_equal), reads=[r_be, r_const], writes=[r_ohb])
            P.op("dve", lambda e: e.scalar_tensor_tensor(out=wif[:], in0=bc(beb[:, :].unsqueeze(2), [128, NBLK, 8]),
                                                         scalar=1024.0, in1=bc(iokc[:, :].unsqueeze(1), [128, NBLK, 8]),
                                                         op0=ALU.mult, op1=ALU.add), reads=[r_be], writes=[r_be])
            same = sb(st, "same", [128, NBLK]); pm1 = sb(st, "pm1", [128, 1])
            P.op("dve", lambda e: e.memset(same[:, 0:1], 0.0), reads=[r_be], writes=[r_be])
            P.op("dve", lambda e: e.tensor_tensor(out=same[:, 1:NBLK], in0=beb[:, 1:NBLK], in1=beb[:, 0:NBLK - 1],
                                                  op=ALU.is_equal), reads=[r_be], writes=[r_be])
            P.op("dve", lambda e: e.tensor_scalar(out=pm1[:], in0=iota_p[:], scalar1=1.0, scalar2=1048576.0,
                                                   op0=ALU.min, op1=ALU.mult), reads=[r_be, r_const], writes=[r_be])
            P.op("dve", lambda e: e.tensor_scalar_mul(out=same[:], in0=same[:], scalar1=pm1[:, 0:1]),
                 reads=[r_be], writes=[r_be])
            P.op("dve", lambda e: e.tensor_tensor(out=wif[:], in0=wif[:], in1=bc(same[:, :].unsqueeze(2), [128, NBLK, 8]),
                                                  op=ALU.add), reads=[r_be], writes=[r_be])
            P.op("dve", lambda e: e.tensor_copy(out=widx[:], in_=wif[:]), reads=[r_be], writes=[r_widx])
            if debug:
                P.op("sp", lambda e: e.dma_start(out=dbg_d[:, 0:NT * E], in_=gw_all[:].rearrange("p a b -> p (a b)")),
                     reads=r_maskall, writes=[Res()], dma=True)
                P.op("sp", lambda e: e.dma_start(out=dbg_d[:, NT * E:NT * E + NBLK], in_=beb[:]),
                     reads=[r_be], writes=[Res()], dma=True)
                P.op("sp", lambda e: e.dma_start(out=dbg_d[:, NT * E + NBLK:NT * E + NBLK + NT * 4],
                                                 in_=d4f[:].rearrange("p a b -> p (a b)")),
                     reads=[r_d4f], writes=[Res()], dma=True)
            P.flush()
        stR.close()

        with ExitStack() as st:
            bgu = sb(st, "bgu", [E, 2 * D], BF16); bdn = sb(st, "bdn", [E, D], BF16); r_bias = Res()
            P.op("pool", lambda e: e.dma_start(out=bgu[:], in_=b_gu[:, :]), writes=[r_bias], dma=True)
            P.op("pool", lambda e: e.dma_start(out=bdn[:], in_=b_dn[:, :]), writes=[r_bias], dma=True)
            wgu = [sb(st, "wgu%d" % i, [128, 8, 2 * D], BF16) for i in range(2)]; r_wgu = [[Res() for _ in range(8)] for _ in range(2)]
            wdn = [sb(st, "wdn%d" % i, [128, 8, D], BF16) for i in range(2)]; r_wdn = [[Res() for _ in range(8)] for _ in range(2)]
            xg = [sb(st, "xg%d" % i, [128, 4, D], BF16) for i in range(2)]; r_xg = [[Res() for _ in range(4)] for _ in range(2)]
            xT = sb(st, "xT", [128, 8, 512], BF16); r_xT = Res()
            aT = sb(st, "aT", [128, 8, 512], BF16); r_aT = Res()
            ohj = [sb(st, "ohj%d" % i, [E, 512], BF16) for i in range(2)]; r_ohj = [Res(), Res()]
            yst = sb(st, "yst", [128, 4, D]); r_yst = Res()
            gp = [sb(st, "gp%d" % i, [128, 512]) for i in range(2)]; r_gp = [Res(), Res()]
            sg_ = [sb(st, "sg%d" % i, [128, 512]) for i in range(2)]; r_sg = [Res(), Res()]
            up = [sb(st, "up%d" % i, [128, 512]) for i in range(2)]; r_up7 = [Res(), Res()]
            p_x = [ps(st, "p_x%d" % i, [128, 8, 128], BF16) for i in range(2)]; r_px = [Res(), Res()]
            p_g = [ps(st, "p_g%d" % i, [128, 512]) for i in range(2)]; r_pg = [Res(), Res()]
            p_u = [ps(st, "p_u%d" % i, [128, 512]) for i in range(2)]; r_pu = [Res(), Res()]
            p_y = [ps(st, "p_y%d" % i, [128, 512]) for i in range(2)]; r_py = [Res(), Res()]

            bnd_reg = []

            def get_bnd(e):
                if not bnd_reg:
                    r = e.alloc_register("bnd_reg")
                    e.reg_mov(r, E * D - 1)
                    bnd_reg.append(r)
                return bnd_reg[0]

            def load_block(j):
                bi = j % 2
                if j >= 1:
                    P.op("dve", lambda e, bi=bi: e.tensor_copy(out=wgu[bi][:], in_=wgu[1 - bi][:]),
                         reads=r_wgu[1 - bi], writes=r_wgu[bi])
                    P.op("dve", lambda e, bi=bi: e.tensor_copy(out=wdn[bi][:], in_=wdn[1 - bi][:]),
                         reads=r_wdn[1 - bi], writes=r_wdn[bi])
                for kc in range(8):
                    P.op("pool", lambda e, j=j, kc=kc, bi=bi: e.indirect_dma_start(
                        out=wgu[bi][:, kc, :], out_offset=None, in_=w_gu[:, :],
                        in_offset=bass.IndirectOffsetOnAxis(ap=widx[:, j, kc:kc + 1], axis=0),
                        bounds_check=get_bnd(e), oob_is_err=False),
                        reads=[r_widx], writes=[r_wgu[bi][kc]], dma=True)
                for kc in range(8):
                    P.op("pool", lambda e, j=j, kc=kc, bi=bi: e.indirect_dma_start(
                        out=wdn[bi][:, kc, :], out_offset=None, in_=w_dn[:, :],
                        in_offset=bass.IndirectOffsetOnAxis(ap=widx[:, j, kc:kc + 1], axis=0),
                        bounds_check=get_bnd(e), oob_is_err=False),
                        reads=[r_widx], writes=[r_wdn[bi][kc]], dma=True)
                for q in range(4):
                    P.op("pool", lambda e, j=j, q=q, bi=bi: e.indirect_dma_start(
                        out=xg[bi][:, q, :], out_offset=None, in_=h2_d[:, :],
                        in_offset=bass.IndirectOffsetOnAxis(ap=slot_sb[:, j * 4 + q:j * 4 + q + 1], axis=0)),
                        reads=[r_slot_sb] + r_h2d, writes=[r_xg[bi][q]], dma=True)
                P.op("dve", lambda e, j=j, bi=bi: e.tensor_copy(out=ohj[bi][:], in_=bc(ohb[:, j:j + 1], [E, 512])),
                     reads=[r_ohb], writes=[r_ohj[bi]])

            load_block(0)
            pxi = 0
            for j in range(NBLK):
                bi = j % 2
                if j + 1 < NBLK:
                    load_block(j + 1)
                for kc in range(8):
                    pb = pxi % 2; pxi += 1
                    for q in range(4):
                        P.op("pe", lambda e, kc=kc, q=q, pb=pb, bi=bi: e.transpose(
                            out=p_x[pb][:, q, :], in_=xg[bi][:, q, kc * 128:(kc + 1) * 128], identity=ident_b[:]),
                            reads=[r_xg[bi][q], r_const], writes=[r_px[pb]])
                    P.op("act", lambda e, kc=kc, pb=pb: e.copy(out=xT[:, kc, :].rearrange("p (a b) -> p a b", a=4), in_=p_x[pb][:, 0:4, :]),
                         reads=[r_px[pb]], writes=[r_xT])
                for m in range(8):
                    b2 = m % 2
                    for (pt_, rp, col0) in ((p_g[b2], r_pg[b2], m * 128), (p_u[b2], r_pu[b2], D + m * 128)):
                        for kc in range(8):
                            P.op("pe", lambda e, pt_=pt_, kc=kc, col0=col0, bi=bi: e.matmul(
                                pt_[:], lhsT=wgu[bi][:, kc, col0:col0 + 128], rhs=xT[:, kc, :],
                                start=(kc == 0), stop=False), reads=[r_wgu[bi][kc], r_xT], writes=[rp])
                        P.op("pe", lambda e, pt_=pt_, col0=col0, bi=bi: e.matmul(
                            pt_[:], lhsT=bgu[:, col0:col0 + 128], rhs=ohj[bi][:], start=False, stop=True),
                            reads=[r_bias, r_ohj[bi]], writes=[rp])
                    P.op("dve", lambda e, b2=b2: e.tensor_scalar_min(out=gp[b2][:], in0=p_g[b2][:], scalar1=7.0),
                         reads=[r_pg[b2]], writes=[r_gp[b2]])
                    P.op("act", lambda e, b2=b2: e.activation(out=sg_[b2][:], in_=gp[b2][:], func=AF.Sigmoid, scale=1.702),
                         reads=[r_gp[b2]], writes=[r_sg[b2]])
                    P.op("dve", lambda e, b2=b2: e.tensor_scalar(out=up[b2][:], in0=p_u[b2][:], scalar1=-7.0, scalar2=7.0,
                                                                  op0=ALU.max, op1=ALU.min),
                         reads=[r_pu[b2]], writes=[r_up7[b2]])
                    P.op("dve", lambda e, b2=b2: e.tensor_tensor(out=gp[b2][:], in0=gp[b2][:], in1=sg_[b2][:], op=ALU.mult),
                         reads=[r_gp[b2], r_sg[b2]], writes=[r_gp[b2]])
                    P.op("dve", lambda e, b2=b2, m=m: e.scalar_tensor_tensor(
                        out=aT[:, m, :], in0=up[b2][:], scalar=1.0, in1=gp[b2][:], op0=ALU.add, op1=ALU.mult),
                        reads=[r_up7[b2], r_gp[b2]], writes=[r_aT])
                for q in range(4):
                    for dh in range(2):
                        pb = (q * 2 + dh) % 2
                        for m in range(8):
                            P.op("pe", lambda e, q=q, dh=dh, m=m, pb=pb, bi=bi: e.matmul(
                                p_y[pb][:], lhsT=aT[:, m, q * 128:(q + 1) * 128], rhs=wdn[bi][:, m, dh * 512:(dh + 1) * 512],
                                start=(m == 0), stop=False), reads=[r_aT, r_wdn[bi][m]], writes=[r_py[pb]])
                        P.op("pe", lambda e, dh=dh, pb=pb, bi=bi: e.matmul(
                            p_y[pb][:], lhsT=ohj[bi][:, 0:128], rhs=bdn[:, dh * 512:(dh + 1) * 512], start=False, stop=True),
                            reads=[r_ohj[bi], r_bias], writes=[r_py[pb]])
                        if dh == 0:
                            P.op("act", lambda e, q=q, pb=pb: e.copy(out=yst[:, q, 0:512], in_=p_y[pb][:]),
                                 reads=[r_py[pb]], writes=[r_yst])
                        else:
                            P.op("dve", lambda e, q=q, pb=pb: e.tensor_copy(out=yst[:, q, 512:1024], in_=p_y[pb][:]),
                                 reads=[r_py[pb]], writes=[r_yst])
                P.op("sp", lambda e, j=j: e.dma_start(
                    out=ys_d[j * 512:(j + 1) * 512, :].rearrange("(q p) d -> p q d", p=128), in_=yst[:]),
                    reads=[r_yst], writes=[r_ysd], dma=True)
            P.flush()

        with ExitStack() as st:
            G = [[sb(st, "G%d_%d" % (b_, k), [128, D]) for k in range(4)] for b_ in range(2)]
            r_G = [[Res() for _ in range(4)] for _ in range(2)]
            hx = [sb(st, "hx%d" % i, [128, D]) for i in range(2)]; r_hx = [Res(), Res()]
            ac = [sb(st, "ac%d" % i, [128, D]) for i in range(2)]; r_ac = [Res(), Res()]
            ot = [sb(st, "ot%d" % i, [128, D]) for i in range(2)]; r_ot = [Res(), Res()]
            junk3 = sb(st, "junk3", [128, D], BF16); r_j3 = Res()
            ss3 = sb(st, "ss3", [128, NT]); r_ss3 = [Res() for _ in range(NT)]
            r_out = Res()
            for i in range(NT):
                b2 = i % 2
                for k in range(4):
                    P.op("pool", lambda e, i=i, k=k, b2=b2: e.indirect_dma_start(
                        out=G[b2][k][:], out_offset=None, in_=ys_d[:, :],
                        in_offset=bass.IndirectOffsetOnAxis(ap=dest4i[:, i, k:k + 1], axis=0)),
                        reads=[r_d4[i], r_ysd], writes=[r_G[b2][k]], dma=True)
                P.op("sp", lambda e, i=i, b2=b2: e.dma_start(out=hx[b2][:], in_=hres_d[i * 128:(i + 1) * 128, :]),
                     reads=[r_hres[i]], writes=[r_hx[b2]], dma=True)
                P.op("dve", lambda e, i=i, b2=b2: e.tensor_scalar_mul(out=ac[b2][:], in0=G[b2][0][:], scalar1=w4[:, i, 0:1]),
                     reads=[r_G[b2][0], r_w4], writes=[r_ac[b2]])
                for k in range(1, 4):
                    P.op("dve", lambda e, i=i, k=k, b2=b2: e.scalar_tensor_tensor(
                        out=ac[b2][:], in0=G[b2][k][:], scalar=w4[:, i, k:k + 1], in1=ac[b2][:], op0=ALU.mult, op1=ALU.add),
                        reads=[r_G[b2][k], r_w4, r_ac[b2]], writes=[r_ac[b2]])
                P.op("dve", lambda e, b2=b2: e.tensor_tensor(out=ac[b2][:], in0=ac[b2][:], in1=g2b[:], op=ALU.mult),
                     reads=[r_ac[b2], r_g2b], writes=[r_ac[b2]])
                P.op("dve", lambda e, b2=b2: e.tensor_tensor(out=ac[b2][:], in0=ac[b2][:], in1=hx[b2][:], op=ALU.add),
                     reads=[r_ac[b2], r_hx[b2]], writes=[r_ac[b2]])
                P.op("act", lambda e, i=i, b2=b2: e.activation(out=junk3[:], in_=ac[b2][:], func=AF.Square,
                                                                accum_out=ss3[:, i:i + 1]),
                     reads=[r_ac[b2]], writes=[r_j3, r_ss3[i]])
                P.op("act", lambda e, i=i: e.activation(out=ss3[:, i:i + 1], in_=ss3[:, i:i + 1], func=AF.Sqrt,
                                                        scale=1.0 / D, bias=epsb[:]),
                     reads=[r_ss3[i], r_const], writes=[r_ss3[i]])
                P.op("dve", lambda e, i=i: e.reciprocal(out=ss3[:, i:i + 1], in_=ss3[:, i:i + 1]),
                     reads=[r_ss3[i]], writes=[r_ss3[i]])
                P.op("dve", lambda e, i=i, b2=b2: e.scalar_tensor_tensor(
                    out=ot[b2][:], in0=ac[b2][:], scalar=ss3[:, i:i + 1], in1=fgb[:], op0=ALU.mult, op1=ALU.mult),
                    reads=[r_ac[b2], r_ss3[i], r_fgb], writes=[r_ot[b2]])
                P.op("sp", lambda e, i=i, b2=b2: e.dma_start(out=out[i * 128:(i + 1) * 128, :], in_=ot[b2][:]),
                     reads=[r_ot[b2]], writes=[r_out], dma=True)
            P.flush()
    return nc


_NC_CACHE = {}


def _get_nc(debug=False):
    if debug not in _NC_CACHE:
        _NC_CACHE[debug] = build_nc(debug)
    return _NC_CACHE[debug]


def make_in_maps(inputs, cores):
    g = lambda k: np.ascontiguousarray(np.asarray(inputs[k], dtype=np.float32))
    shared = {
        "ada_w": g("ada_w")[0], "ada_b": g("ada_b")[0], "norm1_g": g("norm1_g")[0], "w_in": g("w_in")[0],
        "lb_table": g("lb_table"), "hg_norm_g": g("hg_norm_g")[0], "w_o_hg": g("w_o_hg")[0],
        "dw_w": g("dw_w")[0], "dw_b": g("dw_b")[0], "cv_ln_g": g("cv_ln_g")[0], "cv_ln_b": g("cv_ln_b")[0],
        "w_o_cv": g("w_o_cv")[0], "b_o_cv": g("b_o_cv")[0], "w_out": g("w_out")[0], "norm2_g": g("norm2_g")[0],
        "router_w": g("router_w")[0], "router_b": g("router_b")[0],
        "w_gate_up": g("w_gate_up")[0].reshape(E * D, 2 * D), "b_gate_up": g("b_gate_up")[0],
        "w_down": g("w_down")[0].reshape(E * D, D), "b_down": g("b_down")[0],
        "final_norm_g": g("final_norm_g"),
    }
    xs = g("x")
    cs = g("c")
    maps = []
    for b in cores:
        m = dict(shared)
        m["x"] = xs[b]
        m["c"] = cs[b]
        maps.append(m)
    return maps


def kernel(**inputs):
    nc = _get_nc(False)
    maps = make_in_maps(inputs, list(range(8)))
    res = run_bass_kernel_spmd(nc, maps, core_ids=list(range(8)))
    return np.stack([np.asarray(r["out"], dtype=np.float32) for r in res.results], axis=0)
```
